# Optimizing a Trainium2 kernel written in Bass

```python
import math
import jax, jax.numpy as jnp
from jax import lax
import numpy as np

D_MODEL = 1024
BATCH = 4
SEQ = 8192
DEPTH = 4

N_A_LAYERS = DEPTH // 2
N_B_LAYERS = DEPTH - N_A_LAYERS
A_HEADS = D_MODEL // 128
A_HEAD_DIM = 64
A_VALUE_DIM = 2 * A_HEAD_DIM
D_ATTN_A = A_HEADS * A_VALUE_DIM
B_HEADS = D_MODEL // 64
B_HEAD_DIM = 64
D_ATTN_B = B_HEADS * B_HEAD_DIM
N_EXPERTS = 32
TOP_K = 4
D_EXPERT = D_MODEL
SWIGLU_ALPHA = 1.702
SWIGLU_LIMIT = 7.0
Q_BLOCK = 128
EXPERT_BLOCK = 128
EPS = 1e-6
NEG_INF = -1e30

kernel_name = 'yoco_diffattn_stickbreaking_moe_adaln'


def rmsnorm(x, gain):
    xf = x.astype(jnp.float32)
    y = xf * lax.rsqrt(jnp.mean(xf * xf, axis=-1, keepdims=True) + EPS)
    return (y * gain.astype(jnp.float32)).astype(x.dtype)


def adaln(x, gain, shift, scale):
    return rmsnorm(x, gain) * (1 + scale) + shift


def alibi_slopes(n_heads):
    return jnp.asarray(np.array([2.0 ** (-8.0 * (i + 1) / n_heads) for i in range(n_heads)], dtype=np.float32))


def to_query_blocks(q):
    b, s = q.shape[0], q.shape[1]
    return jnp.moveaxis(q.reshape((b, s // Q_BLOCK, Q_BLOCK) + q.shape[2:]), 1, 0)


def from_query_blocks(o):
    o = jnp.moveaxis(o, 0, 1)
    return o.reshape((o.shape[0], o.shape[1] * o.shape[2]) + o.shape[3:])


def diff_attention(h, wqkv, wo, lam_vecs, subln, layer_idx):
    b, s, _ = h.shape
    qkv = h @ wqkv
    q, k, v = jnp.split(qkv, 3, axis=-1)
    q = q.reshape(b, s, A_HEADS, 2, A_HEAD_DIM) * (A_HEAD_DIM ** -0.5)
    k = k.reshape(b, s, A_HEADS, 2, A_HEAD_DIM)
    v = v.reshape(b, s, A_HEADS, A_VALUE_DIM)
    lam_init = 0.8 - 0.6 * math.exp(-0.3 * layer_idx)
    lv = lam_vecs.astype(jnp.float32)
    lam = jnp.exp(jnp.sum(lv[0] * lv[1])) - jnp.exp(jnp.sum(lv[2] * lv[3])) + lam_init
    slopes = alibi_slopes(A_HEADS)[:, None, None, None]
    kpos = jnp.arange(s)
    n_blk = s // Q_BLOCK

    def block(args):
        i, qi = args
        qpos = i * Q_BLOCK + jnp.arange(Q_BLOCK)
        dist = (qpos[:, None] - kpos[None, :]).astype(jnp.float32)
        sc = jnp.einsum('bqhrd,bkhrd->bhrqk', qi, k).astype(jnp.float32)
        sc = jnp.where(dist >= 0, sc - slopes * dist, NEG_INF)
        p = jax.nn.softmax(sc, axis=-1)
        attn = p[:, :, 0] - lam * p[:, :, 1]
        return jnp.einsum('bhqk,bkhe->bqhe', attn.astype(v.dtype), v)

    o = from_query_blocks(lax.map(block, (jnp.arange(n_blk), to_query_blocks(q))))
    o = rmsnorm(o, subln) * (1 - lam_init)
    return o.reshape(b, s, D_ATTN_A) @ wo


def stick_breaking_attention(h, wq, wo, k, v):
    b, s, _ = h.shape
    q = (h @ wq).reshape(b, s, B_HEADS, B_HEAD_DIM) * (B_HEAD_DIM ** -0.5)
    kpos = jnp.arange(s)
    n_blk = s // Q_BLOCK

    def block(args):
        i, qi = args
        qpos = i * Q_BLOCK + jnp.arange(Q_BLOCK)
        strict = kpos[None, :] < qpos[:, None]
        z = jnp.einsum('bqhd,bkhd->bhqk', qi, k).astype(jnp.float32)
        log_beta = jax.nn.log_sigmoid(z)
        log_1mb = jnp.where(strict, jax.nn.log_sigmoid(-z), 0.0)
        log_rem = lax.cumsum(log_1mb, axis=3, reverse=True) - log_1mb
        a = jnp.where(strict, jnp.exp(log_beta + log_rem), 0.0)
        return jnp.einsum('bhqk,bkhd->bqhd', a.astype(v.dtype), v)

    o = from_query_blocks(lax.map(block, (jnp.arange(n_blk), to_query_blocks(q))))
    return o.reshape(b, s, D_ATTN_B) @ wo


def clamped_swiglu(g, u):
    g = jnp.minimum(g, SWIGLU_LIMIT)
    u = jnp.clip(u, -SWIGLU_LIMIT, SWIGLU_LIMIT)
    return g * jax.nn.sigmoid(SWIGLU_ALPHA * g) * (u + 1)


def moe(h, rw, rb, wgu, bgu, wd, bd):
    b, s, d = h.shape
    t = b * s
    xt = h.reshape(t, d)
    logits = (xt @ rw + rb).astype(jnp.float32)
    top_vals, top_idx = lax.top_k(logits, TOP_K)
    gates = jax.nn.softmax(top_vals, axis=-1)
    n_assign = t * TOP_K
    e_flat = top_idx.reshape(n_assign)
    order = jnp.argsort(e_flat)
    sorted_e = e_flat[order]
    sorted_tok = (order // TOP_K).astype(jnp.int32)
    sorted_gate = gates.reshape(n_assign)[order]
    counts = jnp.bincount(e_flat, length=N_EXPERTS)
    padded = (counts + EXPERT_BLOCK - 1) // EXPERT_BLOCK * EXPERT_BLOCK
    start = jnp.cumsum(counts) - counts
    pend = jnp.cumsum(padded)
    pstart = pend - padded
    dest = pstart[sorted_e] + (jnp.arange(n_assign) - start[sorted_e])
    n_blocks = (n_assign + N_EXPERTS * (EXPERT_BLOCK - 1) + EXPERT_BLOCK - 1) // EXPERT_BLOCK
    n_rows = n_blocks * EXPERT_BLOCK
    row_tok = jnp.zeros((n_rows,), jnp.int32).at[dest].set(sorted_tok)
    row_gate = jnp.zeros((n_rows,), jnp.float32).at[dest].set(sorted_gate)
    block_e = jnp.minimum(jnp.searchsorted(pend, jnp.arange(n_blocks) * EXPERT_BLOCK, side='right'), N_EXPERTS - 1)

    def expert_block(args):
        tok, e = args
        gu = xt[tok] @ wgu[e] + bgu[e]
        return clamped_swiglu(gu[:, :D_EXPERT], gu[:, D_EXPERT:]) @ wd[e] + bd[e]

    ys = lax.map(expert_block, (row_tok.reshape(n_blocks, EXPERT_BLOCK), block_e)).reshape(n_rows, d)
    ys = ys * row_gate[:, None].astype(ys.dtype)
    return jax.ops.segment_sum(ys, row_tok, num_segments=t).reshape(b, s, d)


def setup_inputs(seed: int = 0) -> dict:
    key = jax.random.key(seed)
    ks = jax.random.split(key, 24)
    f32 = jnp.float32
    D = D_MODEL
    def nrm(k, shape, scale):
        return jax.random.normal(k, shape, f32) * scale
    return {
        'x': nrm(ks[0], (BATCH, SEQ, D), 1.0),
        'c': nrm(ks[1], (BATCH, D), 1.0),
        'ada_w': nrm(ks[2], (DEPTH, D, 6 * D), 0.5 * D ** -0.5),
        'ada_b': nrm(ks[3], (DEPTH, 6 * D), 0.02),
        'norm_mix': 1.0 + nrm(ks[4], (DEPTH, D), 0.02),
        'norm_moe': 1.0 + nrm(ks[5], (DEPTH, D), 0.02),
        'a_wqkv': nrm(ks[6], (N_A_LAYERS, D, 3 * D_ATTN_A), D ** -0.5),
        'a_wo': nrm(ks[7], (N_A_LAYERS, D_ATTN_A, D), D_ATTN_A ** -0.5),
        'a_lambda': nrm(ks[8], (N_A_LAYERS, 4, A_HEAD_DIM), 0.1),
        'a_subln': 1.0 + nrm(ks[9], (N_A_LAYERS, A_VALUE_DIM), 0.02),
        'kv_norm': 1.0 + nrm(ks[10], (D,), 0.02),
        'kv_ada_w': nrm(ks[11], (D, 2 * D), 0.5 * D ** -0.5),
        'kv_ada_b': nrm(ks[12], (2 * D,), 0.02),
        'kv_w': nrm(ks[13], (D, 2 * D_ATTN_B), D ** -0.5),
        'b_wq': nrm(ks[14], (N_B_LAYERS, D, D_ATTN_B), D ** -0.5),
        'b_wo': nrm(ks[15], (N_B_LAYERS, D_ATTN_B, D), D_ATTN_B ** -0.5),
        'router_w': nrm(ks[16], (DEPTH, D, N_EXPERTS), D ** -0.5),
        'router_b': nrm(ks[17], (DEPTH, N_EXPERTS), 0.01),
        'w_gate_up': nrm(ks[18], (DEPTH, N_EXPERTS, D, 2 * D_EXPERT), D ** -0.5),
        'b_gate_up': nrm(ks[19], (DEPTH, N_EXPERTS, 2 * D_EXPERT), 0.02),
        'w_down': nrm(ks[20], (DEPTH, N_EXPERTS, D_EXPERT, D), D_EXPERT ** -0.5),
        'b_down': nrm(ks[21], (DEPTH, N_EXPERTS, D), 0.02),
        'final_norm': 1.0 + nrm(ks[22], (D,), 0.02),
    }


def reference(x, c, ada_w, ada_b, norm_mix, norm_moe, a_wqkv, a_wo, a_lambda, a_subln,
              kv_norm, kv_ada_w, kv_ada_b, kv_w, b_wq, b_wo, router_w, router_b,
              w_gate_up, b_gate_up, w_down, b_down, final_norm):
    b, s, _ = x.shape
    c_act = jax.nn.silu(c)
    k_sh = None
    v_sh = None
    for l in range(DEPTH):
        mod = (c_act @ ada_w[l] + ada_b[l])[:, None, :]
        sh1, sc1, g1, sh2, sc2, g2 = jnp.split(mod, 6, axis=-1)
        h = adaln(x, norm_mix[l], sh1, sc1)
        if l < N_A_LAYERS:
            y = diff_attention(h, a_wqkv[l], a_wo[l], a_lambda[l], a_subln[l], l)
        else:
            if l == N_A_LAYERS:
                kv_mod = (c_act @ kv_ada_w + kv_ada_b)[:, None, :]
                kv_sh, kv_sc = jnp.split(kv_mod, 2, axis=-1)
                kv = adaln(x, kv_norm, kv_sh, kv_sc) @ kv_w
                k_sh, v_sh = jnp.split(kv, 2, axis=-1)
                k_sh = k_sh.reshape(b, s, B_HEADS, B_HEAD_DIM)
                v_sh = v_sh.reshape(b, s, B_HEADS, B_HEAD_DIM)
            j = l - N_A_LAYERS
            y = stick_breaking_attention(h, b_wq[j], b_wo[j], k_sh, v_sh)
        x = x + g1 * y
        h = adaln(x, norm_moe[l], sh2, sc2)
        x = x + g2 * moe(h, router_w[l], router_b[l], w_gate_up[l], b_gate_up[l], w_down[l], b_down[l])
    return rmsnorm(x, final_norm)
```

```python
import math
import numpy as np
import ml_dtypes
import concourse.bass as bass
import concourse.mybir as mybir
from concourse.bass_utils import run_bass_kernel_spmd

F32 = mybir.dt.float32
BF16 = mybir.dt.bfloat16
AF = mybir.ActivationFunctionType
ALU = mybir.AluOpType
NPBF = ml_dtypes.bfloat16

D = 1024
SEQ = 8192
BATCH = 4
DEPTH = 4
NA = 2
T = 4096
NG = 8
E = 32
EPS = 1e-6
NEG = -32768.0
ENGS = ("pe", "dve", "act", "pool", "sp")


class Op:
    __slots__ = ("eng", "fn", "reads", "writes", "is_dma", "deps", "signal", "sem", "semval",
                 "idx", "is_out", "prev_on_sem", "bar")

    def __init__(self, eng, fn, reads, writes, is_dma, is_out):
        self.eng = eng
        self.fn = fn
        self.reads = reads
        self.writes = writes
        self.is_dma = is_dma
        self.deps = []
        self.signal = False
        self.sem = None
        self.semval = 0
        self.is_out = is_out
        self.prev_on_sem = 0


class Prog:
    def __init__(self, nc, n_dma_sems=20):
        self.nc = nc
        self.ops = []
        self.state = {}
        self.n_dma_sems = n_dma_sems

    def op(self, eng, fn, reads=(), writes=(), dma=False, out=False):
        o = Op(eng, fn, tuple(reads), tuple(writes), dma, out)
        o.idx = len(self.ops)
        deps = set()
        for k in o.reads:
            st = self.state.get(k)
            if st is not None and st[0] is not None:
                deps.add(st[0])
        for k in o.writes:
            st = self.state.get(k)
            if st is not None:
                if st[0] is not None:
                    deps.add(st[0])
                for r in st[1]:
                    deps.add(r)
        for k in o.reads:
            if isinstance(k, str) and k.startswith("c:"):
                continue
            st = self.state.setdefault(k, [None, []])
            st[1].append(o)
        for k in o.writes:
            self.state[k] = [o, []]
        deps.discard(o)
        o.deps = sorted(deps, key=lambda d: d.idx)
        self.ops.append(o)
        return o

    def dma(self, eng, out_ap, in_ap, reads=(), writes=(), out=False):
        return self.op(eng, lambda e: e.dma_start(out=out_ap, in_=in_ap), reads, writes, dma=True, out=out)

    def barrier(self):
        self.ops.append(None)

    def emit(self):
        nc = self.nc
        ops = self.ops
        real = [o for o in ops if o is not None]
        for o in real:
            for d in o.deps:
                if d.is_dma:
                    continue
                if d.eng == o.eng and d.eng == "pe" and not o.is_dma:
                    continue
                d.signal = True
        last = {}
        for o in ops:
            if o is None:
                for e, lo in last.items():
                    lo.signal = True
            elif not o.is_dma:
                last[o.eng] = o
        esem = {e: nc.alloc_semaphore(f"s_{e}") for e in ENGS}
        dq = ("sp", "act", "pool")
        dsem = {e: [nc.alloc_semaphore(f"d_{e}_{i}") for i in range(self.n_dma_sems)] for e in dq}
        dcount = {e: [0] * self.n_dma_sems for e in dq}
        rr = {e: 0 for e in dq}
        tick = {e: 0 for e in ENGS}
        bars = []
        per = {e: [] for e in ENGS}
        nbar = 0
        for o in ops:
            if o is None:
                w = {}
                for e in ENGS:
                    if tick[e] > 0:
                        w[esem[e]] = tick[e]
                for e in dq:
                    for i in range(self.n_dma_sems):
                        if dcount[e][i] > 0:
                            w[dsem[e][i]] = dcount[e][i] * 16
                bars.append(w)
                nbar += 1
                continue
            e = o.eng
            o.bar = nbar
            per[e].append(o)
            if o.is_dma:
                k = rr[e]
                rr[e] = (k + 1) % self.n_dma_sems
                o.prev_on_sem = dcount[e][k] * 16
                dcount[e][k] += 1
                o.sem = dsem[e][k]
                o.semval = dcount[e][k] * 16
            elif o.signal:
                tick[e] += 1
                o.sem = esem[e]
                o.semval = tick[e]
        out_waits = {}
        for o in real:
            if o.is_dma and o.is_out:
                out_waits[o.sem] = max(out_waits.get(o.sem, 0), o.semval)
        self.stats = {e: len(per[e]) for e in ENGS}

        def run(e, engobj):
            seen = {}
            curbar = 0
            for o in per[e]:
                waits = {}
                if o.bar > curbar:
                    curbar = o.bar
                    waits.update(bars[curbar - 1])
                for d in o.deps:
                    if (not d.is_dma) and d.eng == e and e == "pe" and not o.is_dma:
                        continue
                    if d.sem is None:
                        continue
                    if waits.get(d.sem, 0) < d.semval:
                        waits[d.sem] = d.semval
                if o.is_dma and o.prev_on_sem > 0 and waits.get(o.sem, 0) < o.prev_on_sem:
                    waits[o.sem] = o.prev_on_sem
                for s, v in waits.items():
                    if seen.get(s, 0) >= v:
                        continue
                    seen[s] = v
                    engobj.wait_ge(s, v)
                ins = o.fn(engobj)
                if o.is_dma:
                    ins.then_inc(o.sem, 16)
                elif o.signal:
                    ins.then_inc(o.sem, 1)
            if e == "sp":
                for s, v in out_waits.items():
                    engobj.wait_ge(s, v)

        with nc.Block() as block:
            @block.tensor
            def _(t):
                run("pe", t)

            @block.vector
            def _(v):
                run("dve", v)

            @block.scalar
            def _(s):
                run("act", s)

            @block.gpsimd
            def _(g):
                run("pool", g)

            @block.sync
            def _(s):
                run("sp", s)


class SbufAlloc:
    def __init__(self, nc, base=16512, limit=229312, prog=None):
        self.nc = nc
        self.prog = prog
        self.off = base
        self.limit = limit
        self.n = 0

    def mark(self):
        return self.off

    def release(self, m):
        self.off = m
        if self.prog is not None:
            self.prog.barrier()

    def alloc(self, shape, dtype, name="t"):
        esz = 4 if dtype == F32 else 2
        nbytes = int(np.prod(shape[1:])) * esz
        nbytes = (nbytes + 63) // 64 * 64
        assert self.off + nbytes <= self.limit, f"SBUF overflow {name} {self.off}+{nbytes}"
        self.n += 1
        t = self.nc.alloc_sbuf_tensor_at(f"{name}_{self.n}", list(shape), dtype, offset=self.off)
        self.off += nbytes
        return t


class Builder:
    def __init__(self, nc):
        self.nc = nc
        self.P = Prog(nc)
        self.sb = SbufAlloc(nc, prog=self.P)
        self.banks = [nc.alloc_psum_tensor(f"pb{i}", [128, 512], F32) for i in range(8)]
        self.uid = 0
        self.dram = {}

    def din(self, name, shape, dtype=F32):
        t = self.nc.dram_tensor(name, list(shape), dtype, kind="ExternalInput").ap()
        self.dram[name] = t
        return t

    def dout(self, name, shape, dtype=F32):
        t = self.nc.dram_tensor(name, list(shape), dtype, kind="ExternalOutput").ap()
        self.dram[name] = t
        return t

    def dint(self, name, shape, dtype=F32):
        t = self.nc.dram_tensor(name, list(shape), dtype).ap()
        self.dram[name] = t
        return t

    def mm(self, out, lhsT, rhs, start, stop, reads, writes):
        self.P.op("pe", lambda e: e.matmul(out, lhsT, rhs, start=start, stop=stop), reads, writes)

    def act(self, out, in_, func, reads, writes, bias=0.0, scale=1.0):
        self.P.op("act", lambda e: e.activation(out=out, in_=in_, func=func, bias=bias, scale=scale),
                  reads, writes)

    def tt(self, eng, out, in0, in1, op, reads, writes):
        self.P.op(eng, lambda e: e.tensor_tensor(out=out, in0=in0, in1=in1, op=op), reads, writes)

    def ts(self, eng, out, in0, s1, s2, op0, op1, reads, writes):
        if s2 is None:
            self.P.op(eng, lambda e: e.tensor_single_scalar(out=out, in_=in0, scalar=s1, op=op0), reads, writes)
        else:
            self.P.op(eng, lambda e: e.tensor_scalar(out=out, in0=in0, scalar1=s1, scalar2=s2, op0=op0, op1=op1),
                      reads, writes)

    def stt(self, eng, out, in0, scalar, in1, op0, op1, reads, writes):
        self.P.op(eng, lambda e: e.scalar_tensor_tensor(out=out, in0=in0, scalar=scalar, in1=in1, op0=op0, op1=op1),
                  reads, writes)

    def copy(self, eng, out, in_, reads, writes):
        self.P.op(eng, lambda e: e.tensor_copy(out=out, in_=in_), reads, writes)

    def load_consts(self):
        sb, P = self.sb, self.P
        C = {}
        specs = [("ident", [128, 128], F32), ("onesf", [128, 128], F32), ("onesb", [128, 128], BF16),
                 ("negtri", [128, 128], BF16), ("negones", [128, 128], BF16), ("sel", [32, E * 128], BF16),
                 ("identb", [128, 128], BF16),
                 ("flagcol", [128, 1], F32), ("cT", [128, 8], F32), ("zcol", [128, 1], F32)]
        for name, shape, dt in specs:
            d = self.din("k_" + name, shape, dt)
            t = sb.alloc(shape, dt, name)
            P.dma("sp", t[:], d, writes=["c:" + name])
            C[name] = t
        self.C = C
        sig = sb.alloc([128, 8], F32, "csig")
        cact = sb.alloc([128, 8], F32, "cact")
        self.act(sig[:], C["cT"][:], AF.Sigmoid, ["c:cT"], ["csig"])
        self.tt("dve", cact[:], C["cT"][:], sig[:], ALU.mult, ["c:cT", "csig"], ["c:cact"])
        C["cact"] = cact

    def mod_cols(self, w_ap, bT_ap, ncols, tag):
        sb, P, C = self.sb, self.P, self.C
        nch = ncols // 128
        out = sb.alloc([128, nch], F32, "mod" + tag)
        bt = sb.alloc([128, nch], F32, "modb" + tag)
        P.dma("sp", bt[:], bT_ap, writes=["modb" + tag])
        m = sb.mark()
        slabw = 768 if ncols % 768 == 0 else 512
        nslab = ncols // slabw
        slabs = [sb.alloc([128, 8, slabw], F32, "slab") for _ in range(2)]
        wv = w_ap.rearrange("(kc p) n -> p kc n", p=128)
        ps = self.banks[7]
        for s in range(nslab):
            sl = slabs[s % 2]
            key = f"slab{s % 2}"
            P.dma("sp", sl[:], wv[:, :, s * slabw:(s + 1) * slabw], writes=[key])
            for n in range(slabw // 128):
                nn = s * (slabw // 128) + n
                for kc in range(8):
                    self.mm(ps[:, nn:nn + 1], sl[:, kc, n * 128:(n + 1) * 128], C["cact"][:, kc:kc + 1],
                            kc == 0, kc == 7, [key, "c:cact"], ["pb7"])
        self.tt("dve", out[:], ps[:, 0:nch], bt[:], ALU.add, ["pb7", "modb" + tag], ["mod" + tag])
        sb.release(m)
        return out, "mod" + tag

    def layer_mods(self, l):
        sb, P = self.sb, self.P
        ada_w = self.din(f"ada_w{l}", [D, 6 * D])
        ada_bT = self.din(f"ada_bT{l}", [128, 48])
        nmT = self.din(f"nmT{l}", [128, 16])
        mod, mk = self.mod_cols(ada_w, ada_bT, 6 * D, f"L{l}")
        nm = sb.alloc([128, 16], F32, "nm")
        P.dma("sp", nm[:], nmT, writes=[f"nm{l}"])
        A = sb.alloc([128, 16], F32, "Acols")
        self.stt("dve", A[:, 0:8], mod[:, 8:16], 1.0, nm[:, 0:8], ALU.add, ALU.mult, [mk, f"nm{l}"], [f"A1_{l}"])
        self.stt("dve", A[:, 8:16], mod[:, 32:40], 1.0, nm[:, 8:16], ALU.add, ALU.mult, [mk, f"nm{l}"], [f"A2_{l}"])
        return dict(A1=A[:, 0:8], B1=mod[:, 0:8], G1=mod[:, 16:24], A2=A[:, 8:16], B2=mod[:, 24:32],
                    G2=mod[:, 40:48], kA1=f"A1_{l}", kA2=f"A2_{l}", kmod=mk)

    def rstd_of(self, x, xkeys, n, sq, rstd, tag, bank, dim=1024.0, nk=8):
        C = self.C
        ps = self.banks[bank]
        for kc in range(nk):
            q = sq[kc % 2]
            self.act(q[:, 0:n], x[:, kc, :], AF.Square, xkeys, [f"sq{tag}{kc % 2}"])
            self.mm(ps[:, 0:n], C["onesb"][:], q[:, 0:n], kc == 0, kc == nk - 1,
                    [f"sq{tag}{kc % 2}", "c:onesb"], [f"pb{bank}"])
        self.ts("dve", rstd, ps[:, 0:n], 1.0 / dim, EPS, ALU.mult, ALU.add, [f"pb{bank}"], ["rstd" + tag])
        self.act(rstd, rstd, AF.Ln, ["rstd" + tag], ["rstd" + tag])
        self.act(rstd, rstd, AF.Exp, ["rstd" + tag], ["rstd" + tag], scale=-0.5)

    def phase_proj(self, l, XT, mods, outs):
        sb, P, C = self.sb, self.P, self.C
        m0 = sb.mark()
        isA = l < NA
        wts = {}
        if isA:
            w = self.din(f"wqkv{l}", [D, 3 * D]).rearrange("(kc p) n -> p kc n", p=128)
            for i, nm in enumerate(("Q", "K", "V")):
                t = sb.alloc([128, 8, D], BF16, "w" + nm)
                P.dma("pool", t[:], w[:, :, i * D:(i + 1) * D], writes=["w" + nm])
                wts[nm] = t
        else:
            w = self.din(f"wq{l}", [D, D]).rearrange("(kc p) n -> p kc n", p=128)
            t = sb.alloc([128, 8, D], BF16, "wQ")
            P.dma("pool", t[:], w, writes=["wQ"])
            wts["Q"] = t
            if "KB" in outs:
                w = self.din("kvw", [D, 2 * D]).rearrange("(kc p) n -> p kc n", p=128)
                for i, nm in enumerate(("KB", "VB")):
                    t = sb.alloc([128, 8, D], BF16, "w" + nm)
                    P.dma("pool", t[:], w[:, :, i * D:(i + 1) * D], writes=["w" + nm])
                    wts[nm] = t
                kvmod, kvk = self.mod_cols(self.din("kv_ada_w", [D, 2 * D]), self.din("kv_ada_bT", [128, 16]),
                                           2 * D, "KV")
                kvn = sb.alloc([128, 8], F32, "kvn")
                P.dma("sp", kvn[:], self.din("kvnT", [128, 8]), writes=["kvn"])
                Akv = sb.alloc([128, 8], F32, "Akv")
                self.stt("dve", Akv[:], kvmod[:, 8:16], 1.0, kvn[:], ALU.add, ALU.mult, [kvk, "kvn"], ["Akv"])
        XTv = XT.rearrange("(kc p) t -> p kc t", p=128)
        xs = [sb.alloc([128, 8, 512], F32, "x") for _ in range(2)]
        sqs = [sb.alloc([128, 512], BF16, "sq") for _ in range(2)]
        rstd = sb.alloc([128, 512], F32, "rstd")
        tmp = [sb.alloc([128, 512], F32, "tmp") for _ in range(2)]
        hT = [sb.alloc([128, 8, 512], BF16, "hT") for _ in range(2)]
        hK = sb.alloc([128, 8, 512], BF16, "hK") if "KB" in outs else None
        ev = [sb.alloc([128, 512], BF16, "ev") for _ in range(4)]
        vst = [sb.alloc([128, 4, D], BF16, "vst") for _ in range(2)]
        evi = 0
        pbi = 0
        for g in range(NG):
            x = xs[g % 2]
            xk = f"x{g % 2}"
            P.dma("sp", x[:], XTv[:, :, g * 512:(g + 1) * 512], reads=[("XT", g)], writes=[xk])
            self.rstd_of(x, [xk], 512, sqs, rstd[:], "p", 6)
            h = hT[g % 2]
            hk = f"hT{g % 2}"
            for kc in range(8):
                tm = tmp[kc % 2]
                self.stt("dve", tm[:], x[:, kc, :], mods["A1"][:, kc:kc + 1], rstd[:], ALU.mult, ALU.mult,
                         [xk, mods["kA1"], "rstdp"], [f"tmp{kc % 2}"])
                self.act(h[:, kc, :], tm[:], AF.Identity, [f"tmp{kc % 2}", mods["kmod"]], [hk],
                         bias=mods["B1"][:, kc:kc + 1])
            if hK is not None:
                for kc in range(8):
                    tm = tmp[kc % 2]
                    self.stt("dve", tm[:], x[:, kc, :], Akv[:, kc:kc + 1], rstd[:], ALU.mult, ALU.mult,
                             [xk, "Akv", "rstdp"], [f"tmp{kc % 2}"])
                    self.act(hK[:, kc, :], tm[:], AF.Identity, [f"tmp{kc % 2}", kvk], ["hK"],
                             bias=kvmod[:, kc:kc + 1])
            if getattr(self, "debug", False) and g == 0:
                P.dma("sp", self.dout("d_rstd", [128, 512]), rstd[:], reads=["rstdp"], out=True)
                P.dma("sp", self.dout("d_hT", [128, 8, 512], BF16), h[:], reads=[hk], out=True)
                P.dma("sp", self.dout("d_x", [128, 8, 512]), x[:], reads=[xk], out=True)
            for nm, src, srck, scale in (("Q", h, hk, 0.125), ("K", h, hk, 1.0), ("KB", hK, "hK", 1.0)):
                if nm not in outs or nm not in wts:
                    continue
                for j in range(8):
                    pb = pbi % 4
                    pbi += 1
                    ps = self.banks[pb]
                    for kc in range(8):
                        self.mm(ps[:], wts[nm][:, kc, j * 128:(j + 1) * 128], src[:, kc, :], kc == 0, kc == 7,
                                ["w" + nm, srck], [f"pb{pb}"])
                    e = ev[evi % 4]
                    ek = f"ev{evi % 4}"
                    evi += 1
                    if j % 2 == 0:
                        self.act(e[:], ps[:], AF.Identity, [f"pb{pb}"], [ek], scale=scale)
                    else:
                        self.ts("dve", e[:], ps[:], scale, None, ALU.mult, None, [f"pb{pb}"], [ek])
                    P.dma("sp", outs[nm][j * 128:(j + 1) * 128, g * 512:(g + 1) * 512], e[:], reads=[ek],
                          writes=[(nm, j, g)])
            for nm, src, srck in (("V", h, hk), ("VB", hK, "hK")):
                if nm not in outs or nm not in wts:
                    continue
                vs = vst[g % 2]
                vk = f"vst{g % 2}"
                for tt_ in range(4):
                    for half in range(2):
                        pb = pbi % 4
                        pbi += 1
                        ps = self.banks[pb]
                        for kc in range(8):
                            self.mm(ps[:], src[:, kc, tt_ * 128:(tt_ + 1) * 128],
                                    wts[nm][:, kc, half * 512:(half + 1) * 512], kc == 0, kc == 7,
                                    ["w" + nm, srck], [f"pb{pb}"])
                        if half == 0:
                            self.act(vs[:, tt_, 0:512], ps[:], AF.Identity, [f"pb{pb}"], [vk])
                        else:
                            self.copy("dve", vs[:, tt_, 512:1024], ps[:], [f"pb{pb}"], [vk])
                P.dma("sp", outs[nm].rearrange("(n p) c -> p n c", p=128)[:, g * 4:(g + 1) * 4, :], vs[:],
                      reads=[vk], writes=[(nm, g)])
        sb.release(m0)

    def phase_att_a(self, l, QT, KTo, KTp, Vo, Vp, OT, kprev_ready, qaugD, kaugD):
        sb, P, C = self.sb, self.P, self.C
        m0 = sb.mark()
        mt = sb.alloc([128, 4 * 512], BF16, "maskA")
        P.dma("sp", mt[:], self.din("k_maskA", [128, 4 * 512], BF16), writes=["c:maskA"])
        C["maskA"] = mt
        lam_init = 0.8 - 0.6 * math.exp(-0.3 * l)
        lamr = sb.alloc([128, 256], F32, "lamr")
        P.dma("sp", lamr[:], self.din(f"lamrep{l}", [128, 256]), writes=["lamr"])
        lprod = sb.alloc([128, 2, 64], F32, "lprod")
        lsum = sb.alloc([128, 2], F32, "lsum")
        lexp = sb.alloc([128, 2], F32, "lexp")
        neglam = sb.alloc([128, 1], F32, "neglam")
        self.tt("dve", lprod[:, 0, :], lamr[:, 0:64], lamr[:, 64:128], ALU.mult, ["lamr"], ["lprod"])
        self.tt("dve", lprod[:, 1, :], lamr[:, 128:192], lamr[:, 192:256], ALU.mult, ["lamr"], ["lprod"])
        P.op("dve", lambda e: e.reduce_sum(out=lsum[:], in_=lprod[:], axis=mybir.AxisListType.X), ["lprod"], ["lsum"])
        self.act(lexp[:], lsum[:], AF.Exp, ["lsum"], ["lexp"])
        self.tt("dve", neglam[:], lexp[:, 1:2], lexp[:, 0:1], ALU.subtract, ["lexp"], ["neglam"])
        self.ts("dve", neglam[:], neglam[:], -lam_init, None, ALU.add, None, ["neglam"], ["neglam"])
        subc = sb.alloc([128, 1], F32, "subc")
        P.dma("sp", subc[:], self.din(f"sublnT{l}", [128, 1]), writes=["subc"])
        self.ts("dve", subc[:], subc[:], 1.0 - lam_init, None, ALU.mult, None, ["subc"], ["subc"])

        kaug = [[sb.alloc([68, 2 * T], BF16, "kaug") for r in range(2)] for _ in range(2)]
        qaug = [[sb.alloc([68, T], BF16, "qaug") for r in range(2)] for _ in range(2)]
        vh = [sb.alloc([128, 64, 128], BF16, "vh") for _ in range(2)]
        pbuf = [sb.alloc([128, 512], BF16, "pbuf") for _ in range(3)]
        rl = sb.alloc([128, 512], F32, "rl")
        o0 = sb.alloc([128, 512], F32, "o0")
        o1 = sb.alloc([128, 512], F32, "o1")
        osq = sb.alloc([128, 512], F32, "osq")
        orstd = sb.alloc([128, 512], F32, "orstd")
        onb = [sb.alloc([128, 512], BF16, "onb") for _ in range(2)]
        Vov = Vo.rearrange("(n p) c -> p n c", p=128)
        Vpv = Vp.rearrange("(n p) c -> p n c", p=128)
        sbank = 0
        pcount = 0
        ocount = 0
        for h in range(8):
            hb = h % 2
            for r in range(2):
                row0 = h * 128 + r * 64
                ka, qa = kaug[hb][r], qaug[hb][r]
                P.dma("sp", ka[0:64, 0:T], KTp[row0:row0 + 64, :], reads=[kprev_ready], writes=[f"ka{hb}{r}"])
                P.dma("sp", ka[0:64, T:2 * T], KTo[row0:row0 + 64, :],
                      reads=[("K", h, g) for g in range(NG)], writes=[f"ka{hb}{r}"])
                P.dma("sp", ka[64:68, :], kaugD[h], writes=[f"ka{hb}{r}"])
                P.dma("sp", qa[0:64, :], QT[row0:row0 + 64, :], reads=[("Q", h, g) for g in range(NG)],
                      writes=[f"qa{hb}{r}"])
                P.dma("sp", qa[64:68, :], qaugD[h], writes=[f"qa{hb}{r}"])
            v = vh[hb]
            P.dma("sp", v[:, 0:32, :], Vpv[:, :, h * 128:(h + 1) * 128], reads=[kprev_ready], writes=[f"vh{hb}"])
            P.dma("sp", v[:, 32:64, :], Vov[:, :, h * 128:(h + 1) * 128], reads=[("V", g) for g in range(NG)],
                  writes=[f"vh{hb}"])
            for g in range(NG):
                for r in range(2):
                    ka, qa = kaug[hb][r], qaug[hb][r]
                    kak, qak = f"ka{hb}{r}", f"qa{hb}{r}"
                    ob, lb = 4 + r, 6
                    blocks = [(kb, None, True) for kb in range(32)]
                    blocks += [(32 + j, None, False) for j in range(4 * g)]
                    blocks += [(32 + 4 * g + d, d, False) for d in range(4)]
                    nb = len(blocks)
                    pend = []

                    def do_pv(item, first, last, v=v, hb=hb, ob=ob, lb=lb):
                        kb, pk, pt = item
                        self.mm(self.banks[ob][:], v[:, kb, :], pt[:], first, last, [f"vh{hb}", pk], [f"pb{ob}"])
                        self.mm(self.banks[lb][:], C["onesb"][:], pt[:], first, last, ["c:onesb", pk], [f"pb{lb}"])

                    done = 0
                    for bi, (kb, d, isprev) in enumerate(blocks):
                        sbk = sbank % 3
                        sbank += 1
                        ps = self.banks[sbk]
                        self.mm(ps[:], ka[:, kb * 128:(kb + 1) * 128], qa[:, g * 512:(g + 1) * 512], True, d is None,
                                [kak, qak], [f"pb{sbk}"])
                        if d is not None:
                            self.mm(ps[:], C["identb"][:], C["maskA"][:, d * 512:(d + 1) * 512], False, True,
                                    ["c:identb", "c:maskA"], [f"pb{sbk}"])
                        pt = pbuf[pcount % 3]
                        pk = f"pbuf{pcount % 3}"
                        pcount += 1
                        self.act(pt[:], ps[:], AF.Exp, [f"pb{sbk}", "c:flagcol"], [pk],
                                 bias=(C["flagcol"][:, 0:1] if isprev else C["zcol"][:, 0:1]))
                        pend.append((kb, pk, pt))
                        if len(pend) > 1:
                            do_pv(pend.pop(0), done == 0, False)
                            done += 1
                    do_pv(pend.pop(0), done == 0, True)
                    P.op("dve", lambda e: e.reciprocal(out=rl[:], in_=self.banks[lb][:]), [f"pb{lb}"], ["rl"])
                    if r == 0:
                        self.tt("dve", o0[:], self.banks[ob][:], rl[:], ALU.mult, [f"pb{ob}", "rl"], ["o0"])
                    else:
                        self.tt("dve", o1[:], self.banks[ob][:], rl[:], ALU.mult, [f"pb{ob}", "rl"], ["o1"])
                self.stt("dve", o0[:], o1[:], neglam[:, 0:1], o0[:], ALU.mult, ALU.add, ["o1", "neglam", "o0"], ["o0"])
                self.tt("pool", osq[:], o0[:], o0[:], ALU.mult, ["o0"], ["osq"])
                ps = self.banks[7]
                self.mm(ps[:], C["onesf"][:], osq[:], True, True, ["c:onesf", "osq"], ["pb7"])
                self.ts("dve", orstd[:], ps[:], 1.0 / 128.0, EPS, ALU.mult, ALU.add, ["pb7"], ["orstd"])
                self.act(orstd[:], orstd[:], AF.Ln, ["orstd"], ["orstd"])
                self.act(orstd[:], orstd[:], AF.Exp, ["orstd"], ["orstd"], scale=-0.5)
                ob_ = onb[ocount % 2]
                obk = f"onb{ocount % 2}"
                ocount += 1
                self.stt("dve", ob_[:], o0[:], subc[:, 0:1], orstd[:], ALU.mult, ALU.mult, ["o0", "subc", "orstd"], [obk])
                P.dma("sp", OT[h * 128:(h + 1) * 128, g * 512:(g + 1) * 512], ob_[:], reads=[obk], writes=[("O", h, g)])
        sb.release(m0)

    def phase_att_b(self, l, QT, KTo, KTp, Vo, Vp, OT, kprev_ready, kown_keys, vown_keys):
        sb, P, C = self.sb, self.P, self.C
        m0 = sb.mark()
        mt = sb.alloc([128, 4 * 512], BF16, "maskB")
        P.dma("sp", mt[:], self.din("k_maskB", [128, 4 * 512], BF16), writes=["c:maskB"])
        C["maskB"] = mt
        kt = [sb.alloc([128, 2 * T], BF16, "ktb") for _ in range(2)]
        qt = [sb.alloc([128, T], BF16, "qtb") for _ in range(2)]
        vh = [sb.alloc([128, 64, 128], BF16, "vhb") for _ in range(2)]
        ebuf = [sb.alloc([128, 512], F32, "ebuf") for _ in range(2)]
        spb = [sb.alloc([128, 512], BF16, "spb") for _ in range(4)]
        abuf = [sb.alloc([128, 512], BF16, "abuf") for _ in range(4)]
        ssum = sb.alloc([128, 512], F32, "ssum")
        ssb = [sb.alloc([128, 512], BF16, "ssb") for _ in range(2)]
        ost = [sb.alloc([128, 512], BF16, "ost") for _ in range(2)]
        Vov = Vo.rearrange("(n p) c -> p n c", p=128)
        Vpv = Vp.rearrange("(n p) c -> p n c", p=128)
        ssum2 = [ssum, sb.alloc([128, 512], F32, "ssum1")]
        cnt = 0
        for hp in range(8):
            hb = hp % 2
            k_, q_, v = kt[hb], qt[hb], vh[hb]
            kk, qk, vk = f"ktb{hb}", f"qtb{hb}", f"vhb{hb}"
            P.dma("sp", k_[:, 0:T], KTp[hp * 128:(hp + 1) * 128, :], reads=[kprev_ready], writes=[kk])
            P.dma("sp", k_[:, T:2 * T], KTo[hp * 128:(hp + 1) * 128, :], reads=[kk_ for kk_ in kown_keys(hp)],
                  writes=[kk])
            P.dma("sp", q_[:], QT[hp * 128:(hp + 1) * 128, :], reads=[("Q", hp, g) for g in range(NG)], writes=[qk])
            P.dma("sp", v[:, 0:32, :], Vpv[:, :, hp * 128:(hp + 1) * 128], reads=[kprev_ready], writes=[vk])
            P.dma("sp", v[:, 32:64, :], Vov[:, :, hp * 128:(hp + 1) * 128], reads=vown_keys, writes=[vk])
            for g in range(NG):
                os_ = ost[(hp * NG + g) % 2]
                osk = f"ost{(hp * NG + g) % 2}"
                blocks = [(32 + 4 * g + d, d, False) for d in (3, 2, 1, 0)]
                blocks += [(32 + j, None, False) for j in range(4 * g - 1, -1, -1)]
                blocks += [(kb, None, True) for kb in range(31, -1, -1)]
                nb = len(blocks)
                for bi, (kb, d, isprev) in enumerate(blocks):
                    bias = C["flagcol"][:, 0:1] if isprev else C["zcol"][:, 0:1]
                    mk = C["maskB"][:, d * 512:(d + 1) * 512] if d is not None else None
                    par = bi % 2
                    st = []
                    for hh in range(2):
                        r0 = hh * 64
                        kblk = k_[r0:r0 + 64, kb * 128:(kb + 1) * 128]
                        qblk = q_[r0:r0 + 64, g * 512:(g + 1) * 512]
                        zb = hh
                        zp = self.banks[zb]
                        self.mm(zp[:], kblk, qblk, True, d is None, [kk, qk], [f"pb{zb}"])
                        if d is not None:
                            self.mm(zp[:], C["identb"][:], mk, False, True, ["c:identb", "c:maskB"], [f"pb{zb}"])
                        st.append((kblk, qblk))
                    for hh in range(2):
                        zb = hh
                        eb, ek = ebuf[hh], f"ebuf{hh}"
                        self.act(eb[:], self.banks[zb][:], AF.Exp, [f"pb{zb}", "c:flagcol"], [ek], bias=bias)
                        sp_, spk = spb[hh * 2 + par], f"spb{hh * 2 + par}"
                        self.act(sp_[:], eb[:], AF.Ln, [ek], [spk], bias=1.0)
                    for hh in range(2):
                        kblk, qblk = st[hh]
                        rb = 2 + hh
                        rp = self.banks[rb]
                        sp_, spk = spb[hh * 2 + par], f"spb{hh * 2 + par}"
                        self.mm(rp[:], kblk, qblk, True, False, [kk, qk], [f"pb{rb}"])
                        if d is not None:
                            self.mm(rp[:], C["identb"][:], mk, False, False, ["c:identb", "c:maskB"], [f"pb{rb}"])
                        if bi > 0:
                            self.mm(rp[:], C["negones"][:], ssb[hh][:], False, False, ["c:negones", f"ssb{hh}"],
                                    [f"pb{rb}"])
                        self.mm(rp[:], C["negtri"][:], sp_[:], False, True, ["c:negtri", spk], [f"pb{rb}"])
                    for hh in range(2):
                        rb = 2 + hh
                        at, ak = abuf[hh * 2 + par], f"abuf{hh * 2 + par}"
                        self.act(at[:], self.banks[rb][:], AF.Exp, [f"pb{rb}", "c:flagcol"], [ak], bias=bias)
                    for hh in range(2):
                        r0 = hh * 64
                        ob = 4 + hh
                        at, ak = abuf[hh * 2 + par], f"abuf{hh * 2 + par}"
                        sp_, spk = spb[hh * 2 + par], f"spb{hh * 2 + par}"
                        self.mm(self.banks[ob][0:64, :], v[:, kb, r0:r0 + 64], at[:], bi == 0, bi == nb - 1,
                                [vk, ak], [f"pb{ob}"])
                        if bi < nb - 1:
                            if bi == 0:
                                self.copy("pool", ssum2[hh][:], sp_[:], [spk], [f"ssum{hh}"])
                            else:
                                self.tt("pool", ssum2[hh][:], ssum2[hh][:], sp_[:], ALU.add, [f"ssum{hh}", spk],
                                        [f"ssum{hh}"])
                            self.copy("dve", ssb[hh][:], ssum2[hh][:], [f"ssum{hh}"], [f"ssb{hh}"])
                for hh in range(2):
                    ob = 4 + hh
                    self.copy("dve", os_[hh * 64:(hh + 1) * 64, :], self.banks[ob][0:64, :], [f"pb{ob}"], [osk])
                P.dma("sp", OT[hp * 128:(hp + 1) * 128, g * 512:(g + 1) * 512], os_[:], reads=[osk],
                      writes=[("O", hp, g)])
        sb.release(m0)

    def phase_res_moe(self, l, XT, OT, mods, XOUT, final=False):
        sb, P, C = self.sb, self.P, self.C
        m0 = sb.mark()
        NTG = 1024
        wo = sb.alloc([128, 8, D], BF16, "wo")
        P.dma("pool", wo[:], self.din(f"wo{l}", [D, D]).rearrange("(kc p) n -> p kc n", p=128), writes=["wo"])
        wr = sb.alloc([128, 8, E], F32, "wr")
        P.dma("sp", wr[:], self.din(f"rw{l}", [D, E]).rearrange("(kc p) n -> p kc n", p=128), writes=["wr"])
        rbrow = sb.alloc([1, E], F32, "rbrow")
        P.dma("sp", rbrow[:], self.din(f"rb{l}", [1, E]), writes=["rbrow"])
        bd = sb.alloc([E, D], F32, "bd")
        P.dma("sp", bd[:], self.din(f"bd{l}", [E, D]), writes=["bd"])
        bgu = sb.alloc([128, E, 16], F32, "bgu")
        P.dma("sp", bgu[:], self.din(f"bguT{l}", [128, E, 16]), writes=["bgu"])
        wguD = self.din(f"wgu{l}", [E, 8, 128, 8 * 256])
        wdD = self.din(f"wd{l}", [E, 8, 128, 8 * 128])
        if final:
            fnc = sb.alloc([128, 8], F32, "fnc")
            P.dma("sp", fnc[:], self.din("fnT", [128, 8]), writes=["fnc"])
        XTv = XT.rearrange("(kc p) t -> p kc t", p=128)
        OTv = OT.rearrange("(kc p) t -> p kc t", p=128)
        XOv = XOUT.rearrange("(kc p) t -> p kc t", p=128)
        x = sb.alloc([128, 8, NTG], F32, "xg")
        hT = sb.alloc([128, 8, NTG], BF16, "hTg")
        h32 = sb.alloc([128, 8, NTG], F32, "h32acc")
        actT = sb.alloc([128, 8, NTG], BF16, "actT")
        oT = actT
        sq = [sb.alloc([128, 512], BF16, "sqm") for _ in range(2)]
        rstd = sb.alloc([128, 512], F32, "rstdm")
        tmp = [sb.alloc([128, 512], F32, "tmpm") for _ in range(2)]
        lg = sb.alloc([128, E], F32, "lg")
        m8 = sb.alloc([128, 8], F32, "m8")
        negm = sb.alloc([128, 1], F32, "negm")
        msk = sb.alloc([128, E], F32, "msk")
        ee = sb.alloc([128, E], F32, "ee")
        ssum = sb.alloc([128, 1], F32, "gsum")
        gts = sb.alloc([128, E], F32, "gts")
        gT = sb.alloc([E, NTG], F32, "gT")
        gTh = sb.alloc([E, NTG], BF16, "gTh")
        gTl = sb.alloc([E, NTG], BF16, "gTl")
        gTr = sb.alloc([E, NTG], F32, "gTr")
        gbs = [sb.alloc([128, 512], F32, "gbs") for _ in range(2)]
        wgu = [sb.alloc([128, 8, 256], BF16, "wgu") for _ in range(4)]
        wdn = [sb.alloc([128, 8, 128], BF16, "wdn") for _ in range(4)]
        gc = [sb.alloc([128, 512], F32, "gc") for _ in range(2)]
        sg = [sb.alloc([128, 512], F32, "sg") for _ in range(2)]
        uc = [sb.alloc([128, 512], F32, "uc") for _ in range(2)]
        t1 = [sb.alloc([128, 512], F32, "t1") for _ in range(2)]
        wcnt = 0
        dcnt = 0
        ecnt = 0
        for tg in range(T // NTG):
            gl = [2 * tg, 2 * tg + 1]
            tsl = slice(tg * NTG, (tg + 1) * NTG)
            P.dma("sp", x[:], XTv[:, :, tsl], reads=[("XT", g) for g in gl], writes=["xg"])
            P.dma("sp", oT[:], OTv[:, :, tsl], reads=[("O", h, g) for h in range(8) for g in gl], writes=["actT"])
            for s in range(2):
                ss = slice(s * 512, (s + 1) * 512)
                for j in range(8):
                    pb = (s * 8 + j) % 2
                    ps = self.banks[pb]
                    for kc in range(8):
                        self.mm(ps[:], wo[:, kc, j * 128:(j + 1) * 128], oT[:, kc, ss], kc == 0, kc == 7,
                                ["wo", "actT"], [f"pb{pb}"])
                    self.stt("dve", x[:, j, ss], ps[:], mods["G1"][:, j:j + 1], x[:, j, ss], ALU.mult, ALU.add,
                             [f"pb{pb}", mods["kmod"], "xg"], ["xg"])
            if getattr(self, "debug", False):
                if tg == 0:
                    self.dbg_x1 = self.dout("d_x1", [D, T])
                P.dma("sp", self.dbg_x1.rearrange("(kc p) t -> p kc t", p=128)[:, :, tsl], x[:], reads=["xg"], out=True)
            for s in range(2):
                ss = slice(s * 512, (s + 1) * 512)
                self.rstd_of(x[:, :, ss], ["xg"], 512, sq, rstd[:], "m", 2)
                for kc in range(8):
                    tm = tmp[kc % 2]
                    self.stt("dve", tm[:], x[:, kc, ss], mods["A2"][:, kc:kc + 1], rstd[:], ALU.mult, ALU.mult,
                             ["xg", mods["kA2"], "rstdm"], [f"tmpm{kc % 2}"])
                    self.act(h32[:, kc, ss], tm[:], AF.Identity, [f"tmpm{kc % 2}", mods["kmod"]], ["h32"],
                             bias=mods["B2"][:, kc:kc + 1])
                    self.copy("pool", hT[:, kc, ss], h32[:, kc, ss], ["h32"], ["hTg"])
            for tt_ in range(NTG // 128):
                ts_ = slice(tt_ * 128, (tt_ + 1) * 128)
                ps = self.banks[3]
                for kc in range(8):
                    self.mm(ps[:, 0:E], h32[:, kc, ts_], wr[:, kc, :], kc == 0, False, ["h32", "wr"], ["pb3"])
                self.mm(ps[:, 0:E], C["onesf"][0:1, :], rbrow[:], False, True, ["c:onesf", "rbrow"], ["pb3"])
                self.copy("dve", lg[:], ps[:, 0:E], ["pb3"], ["lg"])
                P.op("dve", lambda e: e.max(out=m8[:], in_=lg[:]), ["lg"], ["m8"])
                self.ts("dve", negm[:], m8[:, 0:1], -1.0, None, ALU.mult, None, ["m8"], ["negm"])
                self.ts("dve", msk[:], lg[:], m8[:, 3:4], None, ALU.is_ge, None, ["lg", "m8"], ["msk"])
                self.act(ee[:], lg[:], AF.Exp, ["lg", "negm"], ["ee"], bias=negm[:, 0:1])
                self.tt("dve", ee[:], ee[:], msk[:], ALU.mult, ["ee", "msk"], ["ee"])
                P.op("dve", lambda e: e.reduce_sum(out=ssum[:], in_=ee[:], axis=mybir.AxisListType.X), ["ee"], ["gsum"])
                P.op("dve", lambda e: e.reciprocal(out=ssum[:], in_=ssum[:]), ["gsum"], ["gsum"])
                self.ts("dve", gts[:], ee[:], ssum[:, 0:1], None, ALU.mult, None, ["ee", "gsum"], ["gts"])
                ps2 = self.banks[4]
                self.mm(ps2[0:E, 0:128], gts[:], C["ident"][:], True, True, ["gts", "c:ident"], ["pb4"])
                self.copy("dve", gT[:, ts_], ps2[0:E, 0:128], ["pb4"], ["gT"])
            self.copy("dve", gTh[:], gT[:], ["gT"], ["gTh"])
            self.tt("dve", gTr[:], gT[:], gTh[:], ALU.subtract, ["gT", "gTh"], ["gTr"])
            self.copy("dve", gTl[:], gTr[:], ["gTr"], ["gTl"])
            acc = h32
            for s in range(2):
                ss = slice(s * 512, (s + 1) * 512)
                for dj in range(8):
                    pb = dj % 2
                    ps = self.banks[pb]
                    self.mm(ps[:], bd[:, dj * 128:(dj + 1) * 128], gT[:, ss], True, True, ["bd", "gT"], [f"pb{pb}"])
                    self.copy("dve", acc[:, dj, ss], ps[:], [f"pb{pb}"], ["h32"])
            for e in range(E):
                for s in range(2):
                    ss = slice(s * 512, (s + 1) * 512)
                    ps = self.banks[6]
                    self.mm(ps[:], C["sel"][:, e * 128:(e + 1) * 128], gTh[:, ss], True, False, ["c:sel", "gTh"], ["pb6"])
                    self.mm(ps[:], C["sel"][:, e * 128:(e + 1) * 128], gTl[:, ss], False, True, ["c:sel", "gTl"], ["pb6"])
                    self.act(gbs[s][:], ps[:], AF.Identity, ["pb6"], [f"gbs{s}"])
                for j in range(8):
                    w = wgu[wcnt % 4]
                    wk = f"wgu{wcnt % 4}"
                    wcnt += 1
                    P.dma("pool", w[:], wguD[e, j].rearrange("p (kc c) -> p kc c", kc=8), writes=[wk])
                    for s in range(2):
                        ss = slice(s * 512, (s + 1) * 512)
                        i2 = ecnt % 2
                        ecnt += 1
                        gb_, ub_ = self.banks[i2], self.banks[2 + i2]
                        for kc in range(8):
                            self.mm(gb_[:], w[:, kc, 0:128], hT[:, kc, ss], kc == 0, kc == 7, [wk, "hTg"], [f"pb{i2}"])
                        for kc in range(8):
                            self.mm(ub_[:], w[:, kc, 128:256], hT[:, kc, ss], kc == 0, kc == 7, [wk, "hTg"],
                                    [f"pb{2 + i2}"])
                        self.ts("dve", gc[i2][:], gb_[:], bgu[:, e, j:j + 1], 7.0, ALU.add, ALU.min,
                                [f"pb{i2}", "bgu"], [f"gc{i2}"])
                        self.act(sg[i2][:], gc[i2][:], AF.Sigmoid, [f"gc{i2}"], [f"sg{i2}"], scale=1.702)
                        self.ts("dve", uc[i2][:], ub_[:], bgu[:, e, 8 + j:9 + j], 7.0, ALU.add, ALU.min,
                                [f"pb{2 + i2}", "bgu"], [f"uc{i2}"])
                        self.ts("pool", uc[i2][:], uc[i2][:], -7.0, 1.0, ALU.max, ALU.add, [f"uc{i2}"], [f"uc{i2}"])
                        self.tt("pool", t1[i2][:], gc[i2][:], gbs[s][:], ALU.mult, [f"gc{i2}", f"gbs{s}"], [f"t1{i2}"])
                        self.tt("pool", uc[i2][:], uc[i2][:], t1[i2][:], ALU.mult, [f"uc{i2}", f"t1{i2}"], [f"uc{i2}"])
                        self.tt("dve", actT[:, j, ss], uc[i2][:], sg[i2][:], ALU.mult, [f"uc{i2}", f"sg{i2}"], ["actT"])
                for dj in range(8):
                    w = wdn[dcnt % 4]
                    wk = f"wdn{dcnt % 4}"
                    dcnt += 1
                    P.dma("pool", w[:], wdD[e, dj].rearrange("p (j c) -> p j c", j=8), writes=[wk])
                    for s in range(2):
                        ss = slice(s * 512, (s + 1) * 512)
                        pb = 4 + (dj * 2 + s) % 2
                        ps = self.banks[pb]
                        for j in range(8):
                            self.mm(ps[:], w[:, j, :], actT[:, j, ss], j == 0, j == 7, [wk, "actT"], [f"pb{pb}"])
                        self.tt("dve", acc[:, dj, ss], acc[:, dj, ss], ps[:], ALU.add, ["h32", f"pb{pb}"], ["h32"])
            if getattr(self, "debug", False):
                if tg == 0:
                    self.dbg_m = self.dout("d_m", [D, T])
                    self.dbg_g = self.dout("d_g", [E, T])
                P.dma("sp", self.dbg_m.rearrange("(kc p) t -> p kc t", p=128)[:, :, tsl], acc[:], reads=["h32"], out=True)
                P.dma("sp", self.dbg_g[:, tsl], gT[:], reads=["gT"], out=True)
            for j in range(8):
                self.stt("dve", x[:, j, :], acc[:, j, :], mods["G2"][:, j:j + 1], x[:, j, :], ALU.mult, ALU.add,
                         ["h32", mods["kmod"], "xg"], ["xg"])
            if final:
                for s in range(2):
                    ss = slice(s * 512, (s + 1) * 512)
                    self.rstd_of(x[:, :, ss], ["xg"], 512, sq, rstd[:], "m", 2)
                    for kc in range(8):
                        self.stt("dve", x[:, kc, ss], x[:, kc, ss], fnc[:, kc:kc + 1], rstd[:], ALU.mult, ALU.mult,
                                 ["xg", "fnc", "rstdm"], ["xg"])
            P.dma("sp", XOv[:, :, tsl], x[:], reads=["xg"], writes=[("XT", g) for g in gl], out=True)
        sb.release(m0)


def _cols(v, n):
    return np.ascontiguousarray(np.asarray(v, np.float32).reshape(n, 128).T)


def core_consts(inputs, core):
    b, hf = core // 2, core % 2
    p = np.arange(128)
    i = np.arange(512)
    k = {}
    k["k_ident"] = np.eye(128, dtype=np.float32)
    k["k_onesf"] = np.ones((128, 128), np.float32)
    k["k_onesb"] = np.ones((128, 128), NPBF)
    k["k_identb"] = np.eye(128, dtype=np.float32).astype(NPBF)
    k["k_negtri"] = (-(p[:, None] >= p[None, :]).astype(np.float32)).astype(NPBF)
    k["k_negones"] = (-np.ones((128, 128), np.float32)).astype(NPBF)
    sel = np.zeros((E, E, 128), np.float32)
    for e in range(E):
        sel[e, e, :] = 1.0
    k["k_sel"] = sel.reshape(E, E * 128).astype(NPBF)
    mA = np.zeros((128, 4, 512), np.float32)
    mB = np.zeros((128, 4, 512), np.float32)
    for d in range(4):
        kp = 128 * d + p[:, None]
        mA[:, d, :] = np.where(kp <= i[None, :], 0.0, NEG)
        mB[:, d, :] = np.where(kp < i[None, :], 0.0, NEG)
    k["k_maskA"] = mA.reshape(128, 2048).astype(NPBF)
    k["k_maskB"] = mB.reshape(128, 2048).astype(NPBF)
    k["k_flagcol"] = np.full((128, 1), 0.0 if hf == 1 else NEG, np.float32)
    k["k_zcol"] = np.zeros((128, 1), np.float32)
    k["k_cT"] = _cols(inputs["c"][b], 8)
    return k


def alibi_tables(hf):
    qp = hf * T + np.arange(T)
    kp = np.concatenate([(1 - hf) * T + np.arange(T), hf * T + np.arange(T)])
    qa = np.zeros((8, 4, T), np.float32)
    ka = np.zeros((8, 4, 2 * T), np.float32)
    for h in range(8):
        s = 2.0 ** (-(h + 1))
        qa[h, 0] = -s * 256.0 * (qp // 256)
        qa[h, 1] = -s * (qp % 256)
        qa[h, 2] = 1.0
        qa[h, 3] = 1.0
        ka[h, 0] = 1.0
        ka[h, 1] = 1.0
        ka[h, 2] = s * 256.0 * (kp // 256)
        ka[h, 3] = s * (kp % 256)
    return qa.astype(NPBF), ka.astype(NPBF)


def layer_shared(inputs, l, cache):
    if l in cache:
        return cache[l]
    f = np.float32
    w = {}
    w[f"ada_w{l}"] = np.ascontiguousarray(inputs["ada_w"][l], f)
    w[f"ada_bT{l}"] = _cols(inputs["ada_b"][l], 48)
    w[f"nmT{l}"] = np.concatenate([_cols(inputs["norm_mix"][l], 8), _cols(inputs["norm_moe"][l], 8)], axis=1)
    if l < NA:
        w[f"wqkv{l}"] = np.ascontiguousarray(inputs["a_wqkv"][l], f)
        w[f"wo{l}"] = np.ascontiguousarray(inputs["a_wo"][l], f)
        w[f"lamrep{l}"] = np.ascontiguousarray(np.tile(np.asarray(inputs["a_lambda"][l], f).reshape(1, 256), (128, 1)))
        w[f"sublnT{l}"] = np.asarray(inputs["a_subln"][l], f).reshape(128, 1).copy()
    else:
        w[f"wq{l}"] = np.ascontiguousarray(inputs["b_wq"][l - NA], f)
        w[f"wo{l}"] = np.ascontiguousarray(inputs["b_wo"][l - NA], f)
    if l == NA:
        w["kvw"] = np.ascontiguousarray(inputs["kv_w"], f)
        w["kv_ada_w"] = np.ascontiguousarray(inputs["kv_ada_w"], f)
        w["kv_ada_bT"] = _cols(inputs["kv_ada_b"], 16)
        w["kvnT"] = _cols(inputs["kv_norm"], 8)
    w[f"rw{l}"] = np.ascontiguousarray(inputs["router_w"][l], f)
    w[f"rb{l}"] = np.asarray(inputs["router_b"][l], f).reshape(1, E).copy()
    w[f"bd{l}"] = np.ascontiguousarray(inputs["b_down"][l], f)
    w[f"bguT{l}"] = np.ascontiguousarray(np.asarray(inputs["b_gate_up"][l], f).reshape(E, 16, 128).transpose(2, 0, 1))
    gu = np.asarray(inputs["w_gate_up"][l], f).reshape(E, 8, 128, 2, 8, 128)
    w[f"wgu{l}"] = np.ascontiguousarray(gu.transpose(0, 4, 2, 1, 3, 5)).reshape(E, 8, 128, 2048)
    dn = np.asarray(inputs["w_down"][l], f).reshape(E, 8, 128, 8, 128)
    w[f"wd{l}"] = np.ascontiguousarray(dn.transpose(0, 3, 2, 1, 4)).reshape(E, 8, 128, 1024)
    if l == DEPTH - 1:
        w["fnT"] = _cols(inputs["final_norm"], 8)
    cache[l] = w
    return w


def build_proj(l):
    nc = bass.Bass("TRN2", target_bir_lowering=False)
    B = Builder(nc)
    B.load_consts()
    XT = B.din("XT", [D, T])
    mods = B.layer_mods(l)
    outs = {"Q": B.dout("Q", [D, T], BF16)}
    if l < NA:
        outs["K"] = B.dout("K", [D, T], BF16)
        outs["V"] = B.dout("V", [T, D], BF16)
    elif l == NA:
        outs["KB"] = B.dout("KB", [D, T], BF16)
        outs["VB"] = B.dout("VB", [T, D], BF16)
    B.phase_proj(l, XT, mods, outs)
    for o in B.P.ops:
        if o is not None and o.is_dma and any(isinstance(k, tuple) and k[0] in ("Q", "K", "V", "KB", "VB") for k in o.writes):
            o.is_out = True
    B.P.emit()
    return nc, B


def build_res(l, debug=False):
    nc = bass.Bass("TRN2", target_bir_lowering=False)
    B = Builder(nc)
    B.debug = debug
    B.load_consts()
    XT = B.din("XT", [D, T])
    mods = B.layer_mods(l)
    QT = B.din("Q", [D, T], BF16)
    Ko = B.din("Ko", [D, T], BF16)
    Kp = B.din("Kp", [D, T], BF16)
    Vo = B.din("Vo", [T, D], BF16)
    Vp = B.din("Vp", [T, D], BF16)
    OT = B.dint("OT", [D, T], BF16)
    XO = B.dout("XO", [D, T])
    if l < NA:
        qa = B.din("qaug", [8, 4, T], BF16)
        ka = B.din("kaug", [8, 4, 2 * T], BF16)
        B.phase_att_a(l, QT, Ko, Kp, Vo, Vp, OT, "none", qa, ka)
    else:
        B.phase_att_b(l, QT, Ko, Kp, Vo, Vp, OT, "none", lambda hp: [], [])
    B.P.barrier()
    B.phase_res_moe(l, XT, OT, mods, XO, final=(l == DEPTH - 1))
    B.P.emit()
    return nc, B


def _pick(d, names):
    return {n: d[n] for n in names}


def kernel(**inputs):
    ncores = 8
    cache = {}
    x = np.asarray(inputs["x"], np.float32)
    XTs = [np.ascontiguousarray(x[c // 2, (c % 2) * T:(c % 2 + 1) * T, :].T) for c in range(ncores)]
    consts = [core_consts(inputs, c) for c in range(ncores)]
    aug = [alibi_tables(c % 2) for c in range(ncores)]
    KB = VB = None
    for l in range(DEPTH):
        w = layer_shared(inputs, l, cache)
        nc, B = build_proj(l)
        names = [n for n in B.dram if n in w]
        maps = []
        for c in range(ncores):
            m = dict(consts[c])
            m["XT"] = XTs[c]
            m.update(_pick(w, names))
            maps.append(m)
        res = run_bass_kernel_spmd(nc, maps, core_ids=list(range(ncores))).results
        Q = [r["Q"] for r in res]
        if l < NA:
            K = [r["K"] for r in res]
            V = [r["V"] for r in res]
        else:
            if l == NA:
                KB = [r["KB"] for r in res]
                VB = [r["VB"] for r in res]
            K, V = KB, VB
        nc, B = build_res(l)
        names = [n for n in B.dram if n in w]
        maps = []
        for c in range(ncores):
            m = dict(consts[c])
            m["XT"] = XTs[c]
            m.update(_pick(w, names))
            m.update({"Q": Q[c], "Ko": K[c], "Kp": K[c ^ 1], "Vo": V[c], "Vp": V[c ^ 1]})
            if l < NA:
                m["qaug"], m["kaug"] = aug[c]
            maps.append(m)
        res = run_bass_kernel_spmd(nc, maps, core_ids=list(range(ncores))).results
        XTs = [r["XO"] for r in res]
        cache.pop(l, None)
    out = np.empty((BATCH, SEQ, D), np.float32)
    for c in range(ncores):
        out[c // 2, (c % 2) * T:(c % 2 + 1) * T, :] = XTs[c].T
    return out
```

```python
import math
import numpy as np
import ml_dtypes
import concourse.bass as bass
import concourse.mybir as mybir
from concourse.bass_utils import run_bass_kernel_spmd

F32 = mybir.dt.float32
BF16 = mybir.dt.bfloat16
AF = mybir.ActivationFunctionType
ALU = mybir.AluOpType
NPBF = ml_dtypes.bfloat16

D = 1024
SEQ = 8192
BATCH = 4
DEPTH = 4
NA = 2
T = 4096
NG = 8
E = 32
EPS = 1e-6
NEG = -32768.0
ENGS = ("pe", "dve", "act", "pool", "sp")


class Op:
    __slots__ = ("eng", "fn", "reads", "writes", "is_dma", "deps", "signal", "sem", "semval",
                 "idx", "is_out", "prev_on_sem", "bar", "is_cc")

    def __init__(self, eng, fn, reads, writes, is_dma, is_out):
        self.eng = eng
        self.fn = fn
        self.reads = reads
        self.writes = writes
        self.is_dma = is_dma
        self.deps = []
        self.signal = False
        self.sem = None
        self.semval = 0
        self.is_out = is_out
        self.prev_on_sem = 0
        self.is_cc = False


class Prog:
    def __init__(self, nc, n_dma_sems=20):
        self.nc = nc
        self.ops = []
        self.state = {}
        self.n_dma_sems = n_dma_sems

    def op(self, eng, fn, reads=(), writes=(), dma=False, out=False):
        o = Op(eng, fn, tuple(reads), tuple(writes), dma, out)
        o.idx = len(self.ops)
        deps = set()
        for k in o.reads:
            st = self.state.get(k)
            if st is not None and st[0] is not None:
                deps.add(st[0])
        for k in o.writes:
            st = self.state.get(k)
            if st is not None:
                if st[0] is not None:
                    deps.add(st[0])
                for r in st[1]:
                    deps.add(r)
        for k in o.reads:
            if isinstance(k, str) and k.startswith("c:"):
                continue
            st = self.state.setdefault(k, [None, []])
            st[1].append(o)
        for k in o.writes:
            self.state[k] = [o, []]
        deps.discard(o)
        o.deps = sorted(deps, key=lambda d: d.idx)
        self.ops.append(o)
        return o

    def dma(self, eng, out_ap, in_ap, reads=(), writes=(), out=False):
        return self.op(eng, lambda e: e.dma_start(out=out_ap, in_=in_ap), reads, writes, dma=True, out=out)

    def cc(self, fn, reads=(), writes=()):
        o = self.op("pool", fn, reads, writes, dma=True)
        o.is_cc = True
        return o

    def barrier(self):
        self.ops.append(None)

    def emit(self):
        nc = self.nc
        ops = self.ops
        real = [o for o in ops if o is not None]
        for o in real:
            for d in o.deps:
                if d.is_dma:
                    continue
                if d.eng == o.eng and d.eng == "pe" and not o.is_dma:
                    continue
                d.signal = True
        last = {}
        for o in ops:
            if o is None:
                for e, lo in last.items():
                    lo.signal = True
            elif not o.is_dma:
                last[o.eng] = o
        esem = {e: nc.alloc_semaphore(f"s_{e}") for e in ENGS}
        dq = ("sp", "act", "pool")
        dsem = {e: [nc.alloc_semaphore(f"d_{e}_{i}") for i in range(self.n_dma_sems)] for e in dq}
        dcount = {e: [0] * self.n_dma_sems for e in dq}
        rr = {e: 0 for e in dq}
        tick = {e: 0 for e in ENGS}
        bars = []
        ccs = []
        per = {e: [] for e in ENGS}
        nbar = 0
        for o in ops:
            if o is None:
                w = {}
                for e in ENGS:
                    if tick[e] > 0:
                        w[esem[e]] = tick[e]
                for e in dq:
                    for i in range(self.n_dma_sems):
                        if dcount[e][i] > 0:
                            w[dsem[e][i]] = dcount[e][i] * 16
                for co in ccs:
                    w[co.sem] = 1
                bars.append(w)
                nbar += 1
                continue
            e = o.eng
            o.bar = nbar
            per[e].append(o)
            if o.is_cc:
                o.sem = nc.alloc_semaphore(f"cc_{o.idx}")
                o.semval = 1
                ccs.append(o)
            elif o.is_dma:
                k = rr[e]
                rr[e] = (k + 1) % self.n_dma_sems
                o.prev_on_sem = dcount[e][k] * 16
                dcount[e][k] += 1
                o.sem = dsem[e][k]
                o.semval = dcount[e][k] * 16
            elif o.signal:
                tick[e] += 1
                o.sem = esem[e]
                o.semval = tick[e]
        out_waits = {}
        for o in real:
            if o.is_dma and o.is_out:
                out_waits[o.sem] = max(out_waits.get(o.sem, 0), o.semval)
        self.stats = {e: len(per[e]) for e in ENGS}

        def run(e, engobj):
            seen = {}
            curbar = 0
            for o in per[e]:
                waits = {}
                if o.bar > curbar:
                    curbar = o.bar
                    waits.update(bars[curbar - 1])
                for d in o.deps:
                    if (not d.is_dma) and d.eng == e and e == "pe" and not o.is_dma:
                        continue
                    if d.sem is None:
                        continue
                    if waits.get(d.sem, 0) < d.semval:
                        waits[d.sem] = d.semval
                if o.is_dma and o.prev_on_sem > 0 and waits.get(o.sem, 0) < o.prev_on_sem:
                    waits[o.sem] = o.prev_on_sem
                for s, v in waits.items():
                    if seen.get(s, 0) >= v:
                        continue
                    seen[s] = v
                    engobj.wait_ge(s, v)
                ins = o.fn(engobj)
                if o.is_cc:
                    ins.then_inc(o.sem)
                elif o.is_dma:
                    ins.then_inc(o.sem, 16)
                elif o.signal:
                    ins.then_inc(o.sem, 1)
            if e == "sp":
                for s, v in out_waits.items():
                    engobj.wait_ge(s, v)

        with nc.Block() as block:
            @block.tensor
            def _(t):
                run("pe", t)

            @block.vector
            def _(v):
                run("dve", v)

            @block.scalar
            def _(s):
                run("act", s)

            @block.gpsimd
            def _(g):
                run("pool", g)

            @block.sync
            def _(s):
                run("sp", s)


class SbufAlloc:
    def __init__(self, nc, base=16512, limit=229312, prog=None):
        self.nc = nc
        self.prog = prog
        self.off = base
        self.limit = limit
        self.n = 0

    def mark(self):
        return self.off

    def release(self, m):
        self.off = m
        if self.prog is not None:
            self.prog.barrier()

    def alloc(self, shape, dtype, name="t"):
        esz = 4 if dtype == F32 else 2
        nbytes = int(np.prod(shape[1:])) * esz
        nbytes = (nbytes + 63) // 64 * 64
        assert self.off + nbytes <= self.limit, f"SBUF overflow {name} {self.off}+{nbytes}"
        self.n += 1
        t = self.nc.alloc_sbuf_tensor_at(f"{name}_{self.n}", list(shape), dtype, offset=self.off)
        self.off += nbytes
        return t


class Builder:
    def __init__(self, nc):
        self.nc = nc
        self.P = Prog(nc)
        self.sb = SbufAlloc(nc, prog=self.P)
        self.banks = [nc.alloc_psum_tensor(f"pb{i}", [128, 512], F32) for i in range(8)]
        self.uid = 0
        self.dram = {}

    def din(self, name, shape, dtype=F32):
        if name in self.dram:
            return self.dram[name]
        t = self.nc.dram_tensor(name, list(shape), dtype, kind="ExternalInput").ap()
        self.dram[name] = t
        return t

    def dout(self, name, shape, dtype=F32):
        t = self.nc.dram_tensor(name, list(shape), dtype, kind="ExternalOutput").ap()
        self.dram[name] = t
        return t

    def dint(self, name, shape, dtype=F32):
        t = self.nc.dram_tensor(name, list(shape), dtype).ap()
        self.dram[name] = t
        return t

    def mm(self, out, lhsT, rhs, start, stop, reads, writes):
        self.P.op("pe", lambda e: e.matmul(out, lhsT, rhs, start=start, stop=stop), reads, writes)

    def act(self, out, in_, func, reads, writes, bias=0.0, scale=1.0):
        self.P.op("act", lambda e: e.activation(out=out, in_=in_, func=func, bias=bias, scale=scale),
                  reads, writes)

    def tt(self, eng, out, in0, in1, op, reads, writes):
        self.P.op(eng, lambda e: e.tensor_tensor(out=out, in0=in0, in1=in1, op=op), reads, writes)

    def ts(self, eng, out, in0, s1, s2, op0, op1, reads, writes):
        if s2 is None:
            self.P.op(eng, lambda e: e.tensor_single_scalar(out=out, in_=in0, scalar=s1, op=op0), reads, writes)
        else:
            self.P.op(eng, lambda e: e.tensor_scalar(out=out, in0=in0, scalar1=s1, scalar2=s2, op0=op0, op1=op1),
                      reads, writes)

    def stt(self, eng, out, in0, scalar, in1, op0, op1, reads, writes):
        self.P.op(eng, lambda e: e.scalar_tensor_tensor(out=out, in0=in0, scalar=scalar, in1=in1, op0=op0, op1=op1),
                  reads, writes)

    def copy(self, eng, out, in_, reads, writes):
        self.P.op(eng, lambda e: e.tensor_copy(out=out, in_=in_), reads, writes)

    def load_consts(self):
        sb, P = self.sb, self.P
        C = {}
        specs = [("ident", [128, 128], F32), ("onesf", [128, 128], F32), ("onesb", [128, 128], BF16),
                 ("negtri", [128, 128], BF16), ("negones", [128, 128], BF16), ("sel", [32, E * 128], BF16),
                 ("identb", [128, 128], BF16),
                 ("flagcol", [128, 1], F32), ("cT", [128, 8], F32), ("zcol", [128, 1], F32)]
        for name, shape, dt in specs:
            d = self.din("k_" + name, shape, dt)
            t = sb.alloc(shape, dt, name)
            P.dma("sp", t[:], d, writes=["c:" + name])
            C[name] = t
        self.C = C
        sig = sb.alloc([128, 8], F32, "csig")
        cact = sb.alloc([128, 8], F32, "cact")
        self.act(sig[:], C["cT"][:], AF.Sigmoid, ["c:cT"], ["csig"])
        self.tt("dve", cact[:], C["cT"][:], sig[:], ALU.mult, ["c:cT", "csig"], ["c:cact"])
        C["cact"] = cact

    def mod_cols(self, w_ap, bT_ap, ncols, tag):
        sb, P, C = self.sb, self.P, self.C
        nch = ncols // 128
        out = sb.alloc([128, nch], F32, "mod" + tag)
        bt = sb.alloc([128, nch], F32, "modb" + tag)
        P.dma("sp", bt[:], bT_ap, writes=["modb" + tag])
        m = sb.mark()
        slabw = 768 if ncols % 768 == 0 else 512
        nslab = ncols // slabw
        slabs = [sb.alloc([128, 8, slabw], F32, "slab") for _ in range(2)]
        wv = w_ap.rearrange("(kc p) n -> p kc n", p=128)
        ps = self.banks[7]
        for s in range(nslab):
            sl = slabs[s % 2]
            key = f"slab{s % 2}"
            P.dma("sp", sl[:], wv[:, :, s * slabw:(s + 1) * slabw], writes=[key])
            for n in range(slabw // 128):
                nn = s * (slabw // 128) + n
                for kc in range(8):
                    self.mm(ps[:, nn:nn + 1], sl[:, kc, n * 128:(n + 1) * 128], C["cact"][:, kc:kc + 1],
                            kc == 0, kc == 7, [key, "c:cact"], ["pb7"])
        self.tt("dve", out[:], ps[:, 0:nch], bt[:], ALU.add, ["pb7", "modb" + tag], ["mod" + tag])
        sb.release(m)
        return out, "mod" + tag

    def layer_mods(self, l):
        sb, P = self.sb, self.P
        ada_w = self.din(f"ada_w{l}", [D, 6 * D])
        ada_bT = self.din(f"ada_bT{l}", [128, 48])
        nmT = self.din(f"nmT{l}", [128, 16])
        mod, mk = self.mod_cols(ada_w, ada_bT, 6 * D, f"L{l}")
        nm = sb.alloc([128, 16], F32, "nm")
        P.dma("sp", nm[:], nmT, writes=[f"nm{l}"])
        A = sb.alloc([128, 16], F32, "Acols")
        self.stt("dve", A[:, 0:8], mod[:, 8:16], 1.0, nm[:, 0:8], ALU.add, ALU.mult, [mk, f"nm{l}"], [f"A1_{l}"])
        self.stt("dve", A[:, 8:16], mod[:, 32:40], 1.0, nm[:, 8:16], ALU.add, ALU.mult, [mk, f"nm{l}"], [f"A2_{l}"])
        return dict(A1=A[:, 0:8], B1=mod[:, 0:8], G1=mod[:, 16:24], A2=A[:, 8:16], B2=mod[:, 24:32],
                    G2=mod[:, 40:48], kA1=f"A1_{l}", kA2=f"A2_{l}", kmod=mk)

    def rstd_of(self, x, xkeys, n, sq, rstd, tag, bank, dim=1024.0, nk=8):
        C = self.C
        ps = self.banks[bank]
        for kc in range(nk):
            q = sq[kc % 2]
            self.act(q[:, 0:n], x[:, kc, :], AF.Square, xkeys, [f"sq{tag}{kc % 2}"])
            self.mm(ps[:, 0:n], C["onesb"][:], q[:, 0:n], kc == 0, kc == nk - 1,
                    [f"sq{tag}{kc % 2}", "c:onesb"], [f"pb{bank}"])
        self.ts("dve", rstd, ps[:, 0:n], 1.0 / dim, EPS, ALU.mult, ALU.add, [f"pb{bank}"], ["rstd" + tag])
        self.act(rstd, rstd, AF.Ln, ["rstd" + tag], ["rstd" + tag])
        self.act(rstd, rstd, AF.Exp, ["rstd" + tag], ["rstd" + tag], scale=-0.5)

    def phase_proj(self, l, XT, mods, outs):
        sb, P, C = self.sb, self.P, self.C
        m0 = sb.mark()
        isA = l < NA
        wts = {}
        if isA:
            w = self.din(f"wqkv{l}", [D, 3 * D]).rearrange("(kc p) n -> p kc n", p=128)
            for i, nm in enumerate(("Q", "K", "V")):
                t = sb.alloc([128, 8, D], BF16, "w" + nm)
                P.dma("pool", t[:], w[:, :, i * D:(i + 1) * D], writes=["w" + nm])
                wts[nm] = t
        else:
            w = self.din(f"wq{l}", [D, D]).rearrange("(kc p) n -> p kc n", p=128)
            t = sb.alloc([128, 8, D], BF16, "wQ")
            P.dma("pool", t[:], w, writes=["wQ"])
            wts["Q"] = t
            if "KB" in outs:
                w = self.din("kvw", [D, 2 * D]).rearrange("(kc p) n -> p kc n", p=128)
                for i, nm in enumerate(("KB", "VB")):
                    t = sb.alloc([128, 8, D], BF16, "w" + nm)
                    P.dma("pool", t[:], w[:, :, i * D:(i + 1) * D], writes=["w" + nm])
                    wts[nm] = t
                kvmod, kvk = self.mod_cols(self.din("kv_ada_w", [D, 2 * D]), self.din("kv_ada_bT", [128, 16]),
                                           2 * D, "KV")
                kvn = sb.alloc([128, 8], F32, "kvn")
                P.dma("sp", kvn[:], self.din("kvnT", [128, 8]), writes=["kvn"])
                Akv = sb.alloc([128, 8], F32, "Akv")
                self.stt("dve", Akv[:], kvmod[:, 8:16], 1.0, kvn[:], ALU.add, ALU.mult, [kvk, "kvn"], ["Akv"])
        XTv = XT.rearrange("(kc p) t -> p kc t", p=128)
        xs = [sb.alloc([128, 8, 512], F32, "x") for _ in range(2)]
        sqs = [sb.alloc([128, 512], BF16, "sq") for _ in range(2)]
        rstd = sb.alloc([128, 512], F32, "rstd")
        tmp = [sb.alloc([128, 512], F32, "tmp") for _ in range(2)]
        hT = [sb.alloc([128, 8, 512], BF16, "hT") for _ in range(2)]
        hK = sb.alloc([128, 8, 512], BF16, "hK") if "KB" in outs else None
        ev = [sb.alloc([128, 512], BF16, "ev") for _ in range(4)]
        vst = [sb.alloc([128, 4, D], BF16, "vst") for _ in range(2)]
        evi = 0
        pbi = 0
        for g in range(NG):
            x = xs[g % 2]
            xk = f"x{g % 2}"
            P.dma("sp", x[:], XTv[:, :, g * 512:(g + 1) * 512], reads=[("XT", g)], writes=[xk])
            self.rstd_of(x, [xk], 512, sqs, rstd[:], "p", 6)
            h = hT[g % 2]
            hk = f"hT{g % 2}"
            for kc in range(8):
                tm = tmp[kc % 2]
                self.stt("dve", tm[:], x[:, kc, :], mods["A1"][:, kc:kc + 1], rstd[:], ALU.mult, ALU.mult,
                         [xk, mods["kA1"], "rstdp"], [f"tmp{kc % 2}"])
                self.act(h[:, kc, :], tm[:], AF.Identity, [f"tmp{kc % 2}", mods["kmod"]], [hk],
                         bias=mods["B1"][:, kc:kc + 1])
            if hK is not None:
                for kc in range(8):
                    tm = tmp[kc % 2]
                    self.stt("dve", tm[:], x[:, kc, :], Akv[:, kc:kc + 1], rstd[:], ALU.mult, ALU.mult,
                             [xk, "Akv", "rstdp"], [f"tmp{kc % 2}"])
                    self.act(hK[:, kc, :], tm[:], AF.Identity, [f"tmp{kc % 2}", kvk], ["hK"],
                             bias=kvmod[:, kc:kc + 1])
            if getattr(self, "debug", False) and g == 0:
                P.dma("sp", self.dout("d_rstd", [128, 512]), rstd[:], reads=["rstdp"], out=True)
                P.dma("sp", self.dout("d_hT", [128, 8, 512], BF16), h[:], reads=[hk], out=True)
                P.dma("sp", self.dout("d_x", [128, 8, 512]), x[:], reads=[xk], out=True)
            for nm, src, srck, scale in (("Q", h, hk, 0.125), ("K", h, hk, 1.0), ("KB", hK, "hK", 1.0)):
                if nm not in outs or nm not in wts:
                    continue
                for j in range(8):
                    pb = pbi % 4
                    pbi += 1
                    ps = self.banks[pb]
                    for kc in range(8):
                        self.mm(ps[:], wts[nm][:, kc, j * 128:(j + 1) * 128], src[:, kc, :], kc == 0, kc == 7,
                                ["w" + nm, srck], [f"pb{pb}"])
                    e = ev[evi % 4]
                    ek = f"ev{evi % 4}"
                    evi += 1
                    if j % 2 == 0:
                        self.act(e[:], ps[:], AF.Identity, [f"pb{pb}"], [ek], scale=scale)
                    else:
                        self.ts("dve", e[:], ps[:], scale, None, ALU.mult, None, [f"pb{pb}"], [ek])
                    P.dma("sp", outs[nm][j * 128:(j + 1) * 128, g * 512:(g + 1) * 512], e[:], reads=[ek],
                          writes=[(nm, j, g)])
            for nm, src, srck in (("V", h, hk), ("VB", hK, "hK")):
                if nm not in outs or nm not in wts:
                    continue
                vs = vst[g % 2]
                vk = f"vst{g % 2}"
                for tt_ in range(4):
                    for half in range(2):
                        pb = pbi % 4
                        pbi += 1
                        ps = self.banks[pb]
                        for kc in range(8):
                            self.mm(ps[:], src[:, kc, tt_ * 128:(tt_ + 1) * 128],
                                    wts[nm][:, kc, half * 512:(half + 1) * 512], kc == 0, kc == 7,
                                    ["w" + nm, srck], [f"pb{pb}"])
                        if half == 0:
                            self.act(vs[:, tt_, 0:512], ps[:], AF.Identity, [f"pb{pb}"], [vk])
                        else:
                            self.copy("dve", vs[:, tt_, 512:1024], ps[:], [f"pb{pb}"], [vk])
                P.dma("sp", outs[nm].rearrange("(n p) c -> p n c", p=128)[:, g * 4:(g + 1) * 4, :], vs[:],
                      reads=[vk], writes=[(nm, g)])
        sb.release(m0)

    def phase_att_a(self, l, QT, KTo, KTp, Vo, Vp, OT, kprev_ready, qaugD, kaugD):
        sb, P, C = self.sb, self.P, self.C
        m0 = sb.mark()
        mt = sb.alloc([128, 4 * 512], BF16, "maskA")
        P.dma("sp", mt[:], self.din("k_maskA", [128, 4 * 512], BF16), writes=["c:maskA"])
        C["maskA"] = mt
        lam_init = 0.8 - 0.6 * math.exp(-0.3 * l)
        lamr = sb.alloc([128, 256], F32, "lamr")
        P.dma("sp", lamr[:], self.din(f"lamrep{l}", [128, 256]), writes=["lamr"])
        lprod = sb.alloc([128, 2, 64], F32, "lprod")
        lsum = sb.alloc([128, 2], F32, "lsum")
        lexp = sb.alloc([128, 2], F32, "lexp")
        neglam = sb.alloc([128, 1], F32, "neglam")
        self.tt("dve", lprod[:, 0, :], lamr[:, 0:64], lamr[:, 64:128], ALU.mult, ["lamr"], ["lprod"])
        self.tt("dve", lprod[:, 1, :], lamr[:, 128:192], lamr[:, 192:256], ALU.mult, ["lamr"], ["lprod"])
        P.op("dve", lambda e: e.reduce_sum(out=lsum[:], in_=lprod[:], axis=mybir.AxisListType.X), ["lprod"], ["lsum"])
        self.act(lexp[:], lsum[:], AF.Exp, ["lsum"], ["lexp"])
        self.tt("dve", neglam[:], lexp[:, 1:2], lexp[:, 0:1], ALU.subtract, ["lexp"], ["neglam"])
        self.ts("dve", neglam[:], neglam[:], -lam_init, None, ALU.add, None, ["neglam"], ["neglam"])
        subc = sb.alloc([128, 1], F32, "subc")
        P.dma("sp", subc[:], self.din(f"sublnT{l}", [128, 1]), writes=["subc"])
        self.ts("dve", subc[:], subc[:], 1.0 - lam_init, None, ALU.mult, None, ["subc"], ["subc"])

        kaug = [[sb.alloc([68, 2 * T], BF16, "kaug") for r in range(2)] for _ in range(2)]
        qaug = [[sb.alloc([68, T], BF16, "qaug") for r in range(2)] for _ in range(2)]
        vh = [sb.alloc([128, 64, 128], BF16, "vh") for _ in range(2)]
        pbuf = [sb.alloc([128, 512], BF16, "pbuf") for _ in range(3)]
        rl = sb.alloc([128, 512], F32, "rl")
        o0 = sb.alloc([128, 512], F32, "o0")
        o1 = sb.alloc([128, 512], F32, "o1")
        osq = sb.alloc([128, 512], F32, "osq")
        orstd = sb.alloc([128, 512], F32, "orstd")
        onb = [sb.alloc([128, 512], BF16, "onb") for _ in range(2)]
        Vov = Vo.rearrange("(n p) c -> p n c", p=128)
        Vpv = Vp.rearrange("(n p) c -> p n c", p=128)
        sbank = 0
        pcount = 0
        ocount = 0
        for h in range(8):
            hb = h % 2
            for r in range(2):
                row0 = h * 128 + r * 64
                ka, qa = kaug[hb][r], qaug[hb][r]
                P.dma("sp", ka[0:64, 0:T], KTp[row0:row0 + 64, :], reads=kprev_ready, writes=[f"ka{hb}{r}"])
                P.dma("sp", ka[0:64, T:2 * T], KTo[row0:row0 + 64, :],
                      reads=[("K", h, g) for g in range(NG)], writes=[f"ka{hb}{r}"])
                P.dma("sp", ka[64:68, :], kaugD[h], writes=[f"ka{hb}{r}"])
                P.dma("sp", qa[0:64, :], QT[row0:row0 + 64, :], reads=[("Q", h, g) for g in range(NG)],
                      writes=[f"qa{hb}{r}"])
                P.dma("sp", qa[64:68, :], qaugD[h], writes=[f"qa{hb}{r}"])
            v = vh[hb]
            P.dma("sp", v[:, 0:32, :], Vpv[:, :, h * 128:(h + 1) * 128], reads=kprev_ready, writes=[f"vh{hb}"])
            P.dma("sp", v[:, 32:64, :], Vov[:, :, h * 128:(h + 1) * 128], reads=[("V", g) for g in range(NG)],
                  writes=[f"vh{hb}"])
            for g in range(NG):
                for r in range(2):
                    ka, qa = kaug[hb][r], qaug[hb][r]
                    kak, qak = f"ka{hb}{r}", f"qa{hb}{r}"
                    ob, lb = 4 + r, 6
                    blocks = [(kb, None, True) for kb in range(32)]
                    blocks += [(32 + j, None, False) for j in range(4 * g)]
                    blocks += [(32 + 4 * g + d, d, False) for d in range(4)]
                    nb = len(blocks)
                    pend = []

                    def do_pv(item, first, last, v=v, hb=hb, ob=ob, lb=lb):
                        kb, pk, pt = item
                        self.mm(self.banks[ob][:], v[:, kb, :], pt[:], first, last, [f"vh{hb}", pk], [f"pb{ob}"])
                        self.mm(self.banks[lb][:], C["onesb"][:], pt[:], first, last, ["c:onesb", pk], [f"pb{lb}"])

                    done = 0
                    for bi, (kb, d, isprev) in enumerate(blocks):
                        sbk = sbank % 3
                        sbank += 1
                        ps = self.banks[sbk]
                        self.mm(ps[:], ka[:, kb * 128:(kb + 1) * 128], qa[:, g * 512:(g + 1) * 512], True, d is None,
                                [kak, qak], [f"pb{sbk}"])
                        if d is not None:
                            self.mm(ps[:], C["identb"][:], C["maskA"][:, d * 512:(d + 1) * 512], False, True,
                                    ["c:identb", "c:maskA"], [f"pb{sbk}"])
                        pt = pbuf[pcount % 3]
                        pk = f"pbuf{pcount % 3}"
                        pcount += 1
                        self.act(pt[:], ps[:], AF.Exp, [f"pb{sbk}", "c:flagcol"], [pk],
                                 bias=(C["flagcol"][:, 0:1] if isprev else C["zcol"][:, 0:1]))
                        pend.append((kb, pk, pt))
                        if len(pend) > 1:
                            do_pv(pend.pop(0), done == 0, False)
                            done += 1
                    do_pv(pend.pop(0), done == 0, True)
                    P.op("dve", lambda e: e.reciprocal(out=rl[:], in_=self.banks[lb][:]), [f"pb{lb}"], ["rl"])
                    if r == 0:
                        self.tt("dve", o0[:], self.banks[ob][:], rl[:], ALU.mult, [f"pb{ob}", "rl"], ["o0"])
                    else:
                        self.tt("dve", o1[:], self.banks[ob][:], rl[:], ALU.mult, [f"pb{ob}", "rl"], ["o1"])
                self.stt("dve", o0[:], o1[:], neglam[:, 0:1], o0[:], ALU.mult, ALU.add, ["o1", "neglam", "o0"], ["o0"])
                self.tt("pool", osq[:], o0[:], o0[:], ALU.mult, ["o0"], ["osq"])
                ps = self.banks[7]
                self.mm(ps[:], C["onesf"][:], osq[:], True, True, ["c:onesf", "osq"], ["pb7"])
                self.ts("dve", orstd[:], ps[:], 1.0 / 128.0, EPS, ALU.mult, ALU.add, ["pb7"], ["orstd"])
                self.act(orstd[:], orstd[:], AF.Ln, ["orstd"], ["orstd"])
                self.act(orstd[:], orstd[:], AF.Exp, ["orstd"], ["orstd"], scale=-0.5)
                ob_ = onb[ocount % 2]
                obk = f"onb{ocount % 2}"
                ocount += 1
                self.stt("dve", ob_[:], o0[:], subc[:, 0:1], orstd[:], ALU.mult, ALU.mult, ["o0", "subc", "orstd"], [obk])
                P.dma("sp", OT[h * 128:(h + 1) * 128, g * 512:(g + 1) * 512], ob_[:], reads=[obk], writes=[("O", h, g)])
        sb.release(m0)

    def phase_att_b(self, l, QT, KTo, KTp, Vo, Vp, OT, kprev_ready, kown_keys, vown_keys):
        sb, P, C = self.sb, self.P, self.C
        m0 = sb.mark()
        mt = sb.alloc([128, 4 * 512], BF16, "maskB")
        P.dma("sp", mt[:], self.din("k_maskB", [128, 4 * 512], BF16), writes=["c:maskB"])
        C["maskB"] = mt
        kt = [sb.alloc([128, 2 * T], BF16, "ktb") for _ in range(2)]
        qt = [sb.alloc([128, T], BF16, "qtb") for _ in range(2)]
        vh = [sb.alloc([128, 64, 128], BF16, "vhb") for _ in range(2)]
        ebuf = [sb.alloc([128, 512], F32, "ebuf") for _ in range(2)]
        spb = [sb.alloc([128, 512], BF16, "spb") for _ in range(4)]
        abuf = [sb.alloc([128, 512], BF16, "abuf") for _ in range(4)]
        ssum = sb.alloc([128, 512], F32, "ssum")
        ssb = [sb.alloc([128, 512], BF16, "ssb") for _ in range(2)]
        ost = [sb.alloc([128, 512], BF16, "ost") for _ in range(2)]
        Vov = Vo.rearrange("(n p) c -> p n c", p=128)
        Vpv = Vp.rearrange("(n p) c -> p n c", p=128)
        ssum2 = [ssum, sb.alloc([128, 512], F32, "ssum1")]
        cnt = 0
        for hp in range(8):
            hb = hp % 2
            k_, q_, v = kt[hb], qt[hb], vh[hb]
            kk, qk, vk = f"ktb{hb}", f"qtb{hb}", f"vhb{hb}"
            P.dma("sp", k_[:, 0:T], KTp[hp * 128:(hp + 1) * 128, :], reads=kprev_ready, writes=[kk])
            P.dma("sp", k_[:, T:2 * T], KTo[hp * 128:(hp + 1) * 128, :], reads=[kk_ for kk_ in kown_keys(hp)],
                  writes=[kk])
            P.dma("sp", q_[:], QT[hp * 128:(hp + 1) * 128, :], reads=[("Q", hp, g) for g in range(NG)], writes=[qk])
            P.dma("sp", v[:, 0:32, :], Vpv[:, :, hp * 128:(hp + 1) * 128], reads=kprev_ready, writes=[vk])
            P.dma("sp", v[:, 32:64, :], Vov[:, :, hp * 128:(hp + 1) * 128], reads=vown_keys, writes=[vk])
            for g in range(NG):
                os_ = ost[(hp * NG + g) % 2]
                osk = f"ost{(hp * NG + g) % 2}"
                blocks = [(32 + 4 * g + d, d, False) for d in (3, 2, 1, 0)]
                blocks += [(32 + j, None, False) for j in range(4 * g - 1, -1, -1)]
                blocks += [(kb, None, True) for kb in range(31, -1, -1)]
                nb = len(blocks)
                for bi, (kb, d, isprev) in enumerate(blocks):
                    bias = C["flagcol"][:, 0:1] if isprev else C["zcol"][:, 0:1]
                    mk = C["maskB"][:, d * 512:(d + 1) * 512] if d is not None else None
                    par = bi % 2
                    st = []
                    for hh in range(2):
                        r0 = hh * 64
                        kblk = k_[r0:r0 + 64, kb * 128:(kb + 1) * 128]
                        qblk = q_[r0:r0 + 64, g * 512:(g + 1) * 512]
                        zb = hh
                        zp = self.banks[zb]
                        self.mm(zp[:], kblk, qblk, True, d is None, [kk, qk], [f"pb{zb}"])
                        if d is not None:
                            self.mm(zp[:], C["identb"][:], mk, False, True, ["c:identb", "c:maskB"], [f"pb{zb}"])
                        st.append((kblk, qblk))
                    for hh in range(2):
                        zb = hh
                        eb, ek = ebuf[hh], f"ebuf{hh}"
                        self.act(eb[:], self.banks[zb][:], AF.Exp, [f"pb{zb}", "c:flagcol"], [ek], bias=bias)
                        sp_, spk = spb[hh * 2 + par], f"spb{hh * 2 + par}"
                        self.act(sp_[:], eb[:], AF.Ln, [ek], [spk], bias=1.0)
                    for hh in range(2):
                        kblk, qblk = st[hh]
                        rb = 2 + hh
                        rp = self.banks[rb]
                        sp_, spk = spb[hh * 2 + par], f"spb{hh * 2 + par}"
                        self.mm(rp[:], kblk, qblk, True, False, [kk, qk], [f"pb{rb}"])
                        if d is not None:
                            self.mm(rp[:], C["identb"][:], mk, False, False, ["c:identb", "c:maskB"], [f"pb{rb}"])
                        if bi > 0:
                            self.mm(rp[:], C["negones"][:], ssb[hh][:], False, False, ["c:negones", f"ssb{hh}"],
                                    [f"pb{rb}"])
                        self.mm(rp[:], C["negtri"][:], sp_[:], False, True, ["c:negtri", spk], [f"pb{rb}"])
                    for hh in range(2):
                        rb = 2 + hh
                        at, ak = abuf[hh * 2 + par], f"abuf{hh * 2 + par}"
                        self.act(at[:], self.banks[rb][:], AF.Exp, [f"pb{rb}", "c:flagcol"], [ak], bias=bias)
                    for hh in range(2):
                        r0 = hh * 64
                        ob = 4 + hh
                        at, ak = abuf[hh * 2 + par], f"abuf{hh * 2 + par}"
                        sp_, spk = spb[hh * 2 + par], f"spb{hh * 2 + par}"
                        self.mm(self.banks[ob][0:64, :], v[:, kb, r0:r0 + 64], at[:], bi == 0, bi == nb - 1,
                                [vk, ak], [f"pb{ob}"])
                        if bi < nb - 1:
                            if bi == 0:
                                self.copy("pool", ssum2[hh][:], sp_[:], [spk], [f"ssum{hh}"])
                            else:
                                self.tt("pool", ssum2[hh][:], ssum2[hh][:], sp_[:], ALU.add, [f"ssum{hh}", spk],
                                        [f"ssum{hh}"])
                            self.copy("dve", ssb[hh][:], ssum2[hh][:], [f"ssum{hh}"], [f"ssb{hh}"])
                for hh in range(2):
                    ob = 4 + hh
                    self.copy("dve", os_[hh * 64:(hh + 1) * 64, :], self.banks[ob][0:64, :], [f"pb{ob}"], [osk])
                P.dma("sp", OT[hp * 128:(hp + 1) * 128, g * 512:(g + 1) * 512], os_[:], reads=[osk],
                      writes=[("O", hp, g)])
        sb.release(m0)

    def phase_res_moe(self, l, XT, OT, mods, XOUT, final=False):
        sb, P, C = self.sb, self.P, self.C
        m0 = sb.mark()
        NTG = 1024
        wo = sb.alloc([128, 8, D], BF16, "wo")
        P.dma("pool", wo[:], self.din(f"wo{l}", [D, D]).rearrange("(kc p) n -> p kc n", p=128), writes=["wo"])
        wr = sb.alloc([128, 8, E], F32, "wr")
        P.dma("sp", wr[:], self.din(f"rw{l}", [D, E]).rearrange("(kc p) n -> p kc n", p=128), writes=["wr"])
        rbrow = sb.alloc([1, E], F32, "rbrow")
        P.dma("sp", rbrow[:], self.din(f"rb{l}", [1, E]), writes=["rbrow"])
        bd = sb.alloc([E, D], F32, "bd")
        P.dma("sp", bd[:], self.din(f"bd{l}", [E, D]), writes=["bd"])
        bgu = sb.alloc([128, E, 16], F32, "bgu")
        P.dma("sp", bgu[:], self.din(f"bguT{l}", [128, E, 16]), writes=["bgu"])
        wguD = self.din(f"wgu{l}", [E, 8, 128, 8 * 256])
        wdD = self.din(f"wd{l}", [E, 8, 128, 8 * 128])
        if final:
            fnc = sb.alloc([128, 8], F32, "fnc")
            P.dma("sp", fnc[:], self.din("fnT", [128, 8]), writes=["fnc"])
        XTv = XT.rearrange("(kc p) t -> p kc t", p=128)
        OTv = OT.rearrange("(kc p) t -> p kc t", p=128)
        XOv = XOUT.rearrange("(kc p) t -> p kc t", p=128)
        x = sb.alloc([128, 8, NTG], F32, "xg")
        hT = sb.alloc([128, 8, NTG], BF16, "hTg")
        h32 = sb.alloc([128, 8, NTG], F32, "h32acc")
        actT = sb.alloc([128, 8, NTG], BF16, "actT")
        oT = actT
        sq = [sb.alloc([128, 512], BF16, "sqm") for _ in range(2)]
        rstd = sb.alloc([128, 512], F32, "rstdm")
        tmp = [sb.alloc([128, 512], F32, "tmpm") for _ in range(2)]
        lg = sb.alloc([128, E], F32, "lg")
        m8 = sb.alloc([128, 8], F32, "m8")
        negm = sb.alloc([128, 1], F32, "negm")
        msk = sb.alloc([128, E], F32, "msk")
        ee = sb.alloc([128, E], F32, "ee")
        ssum = sb.alloc([128, 1], F32, "gsum")
        gts = sb.alloc([128, E], F32, "gts")
        gT = sb.alloc([E, NTG], F32, "gT")
        gTh = sb.alloc([E, NTG], BF16, "gTh")
        gTl = sb.alloc([E, NTG], BF16, "gTl")
        gTr = sb.alloc([E, NTG], F32, "gTr")
        gbs = [sb.alloc([128, 512], F32, "gbs") for _ in range(2)]
        wgu = [sb.alloc([128, 8, 256], BF16, "wgu") for _ in range(4)]
        wdn = [sb.alloc([128, 8, 128], BF16, "wdn") for _ in range(4)]
        gc = [sb.alloc([128, 512], F32, "gc") for _ in range(2)]
        sg = [sb.alloc([128, 512], F32, "sg") for _ in range(2)]
        uc = [sb.alloc([128, 512], F32, "uc") for _ in range(2)]
        t1 = [sb.alloc([128, 512], F32, "t1") for _ in range(2)]
        wcnt = 0
        dcnt = 0
        ecnt = 0
        for tg in range(T // NTG):
            gl = [2 * tg, 2 * tg + 1]
            tsl = slice(tg * NTG, (tg + 1) * NTG)
            P.dma("sp", x[:], XTv[:, :, tsl], reads=[("XT", g) for g in gl], writes=["xg"])
            P.dma("sp", oT[:], OTv[:, :, tsl], reads=[("O", h, g) for h in range(8) for g in gl], writes=["actT"])
            for s in range(2):
                ss = slice(s * 512, (s + 1) * 512)
                for j in range(8):
                    pb = (s * 8 + j) % 2
                    ps = self.banks[pb]
                    for kc in range(8):
                        self.mm(ps[:], wo[:, kc, j * 128:(j + 1) * 128], oT[:, kc, ss], kc == 0, kc == 7,
                                ["wo", "actT"], [f"pb{pb}"])
                    self.stt("dve", x[:, j, ss], ps[:], mods["G1"][:, j:j + 1], x[:, j, ss], ALU.mult, ALU.add,
                             [f"pb{pb}", mods["kmod"], "xg"], ["xg"])
            if getattr(self, "debug", False):
                if tg == 0:
                    self.dbg_x1 = self.dout("d_x1", [D, T])
                P.dma("sp", self.dbg_x1.rearrange("(kc p) t -> p kc t", p=128)[:, :, tsl], x[:], reads=["xg"], out=True)
            for s in range(2):
                ss = slice(s * 512, (s + 1) * 512)
                self.rstd_of(x[:, :, ss], ["xg"], 512, sq, rstd[:], "m", 2)
                for kc in range(8):
                    tm = tmp[kc % 2]
                    self.stt("dve", tm[:], x[:, kc, ss], mods["A2"][:, kc:kc + 1], rstd[:], ALU.mult, ALU.mult,
                             ["xg", mods["kA2"], "rstdm"], [f"tmpm{kc % 2}"])
                    self.act(h32[:, kc, ss], tm[:], AF.Identity, [f"tmpm{kc % 2}", mods["kmod"]], ["h32"],
                             bias=mods["B2"][:, kc:kc + 1])
                    self.copy("pool", hT[:, kc, ss], h32[:, kc, ss], ["h32"], ["hTg"])
            for tt_ in range(NTG // 128):
                ts_ = slice(tt_ * 128, (tt_ + 1) * 128)
                ps = self.banks[3]
                for kc in range(8):
                    self.mm(ps[:, 0:E], h32[:, kc, ts_], wr[:, kc, :], kc == 0, False, ["h32", "wr"], ["pb3"])
                self.mm(ps[:, 0:E], C["onesf"][0:1, :], rbrow[:], False, True, ["c:onesf", "rbrow"], ["pb3"])
                self.copy("dve", lg[:], ps[:, 0:E], ["pb3"], ["lg"])
                P.op("dve", lambda e: e.max(out=m8[:], in_=lg[:]), ["lg"], ["m8"])
                self.ts("dve", negm[:], m8[:, 0:1], -1.0, None, ALU.mult, None, ["m8"], ["negm"])
                self.ts("dve", msk[:], lg[:], m8[:, 3:4], None, ALU.is_ge, None, ["lg", "m8"], ["msk"])
                self.act(ee[:], lg[:], AF.Exp, ["lg", "negm"], ["ee"], bias=negm[:, 0:1])
                self.tt("dve", ee[:], ee[:], msk[:], ALU.mult, ["ee", "msk"], ["ee"])
                P.op("dve", lambda e: e.reduce_sum(out=ssum[:], in_=ee[:], axis=mybir.AxisListType.X), ["ee"], ["gsum"])
                P.op("dve", lambda e: e.reciprocal(out=ssum[:], in_=ssum[:]), ["gsum"], ["gsum"])
                self.ts("dve", gts[:], ee[:], ssum[:, 0:1], None, ALU.mult, None, ["ee", "gsum"], ["gts"])
                ps2 = self.banks[4]
                self.mm(ps2[0:E, 0:128], gts[:], C["ident"][:], True, True, ["gts", "c:ident"], ["pb4"])
                self.copy("dve", gT[:, ts_], ps2[0:E, 0:128], ["pb4"], ["gT"])
            self.copy("dve", gTh[:], gT[:], ["gT"], ["gTh"])
            self.tt("dve", gTr[:], gT[:], gTh[:], ALU.subtract, ["gT", "gTh"], ["gTr"])
            self.copy("dve", gTl[:], gTr[:], ["gTr"], ["gTl"])
            acc = h32
            for s in range(2):
                ss = slice(s * 512, (s + 1) * 512)
                for dj in range(8):
                    pb = dj % 2
                    ps = self.banks[pb]
                    self.mm(ps[:], bd[:, dj * 128:(dj + 1) * 128], gT[:, ss], True, True, ["bd", "gT"], [f"pb{pb}"])
                    self.copy("dve", acc[:, dj, ss], ps[:], [f"pb{pb}"], ["h32"])
            for e in range(E):
                for s in range(2):
                    ss = slice(s * 512, (s + 1) * 512)
                    ps = self.banks[6]
                    self.mm(ps[:], C["sel"][:, e * 128:(e + 1) * 128], gTh[:, ss], True, False, ["c:sel", "gTh"], ["pb6"])
                    self.mm(ps[:], C["sel"][:, e * 128:(e + 1) * 128], gTl[:, ss], False, True, ["c:sel", "gTl"], ["pb6"])
                    self.act(gbs[s][:], ps[:], AF.Identity, ["pb6"], [f"gbs{s}"])
                for j in range(8):
                    w = wgu[wcnt % 4]
                    wk = f"wgu{wcnt % 4}"
                    wcnt += 1
                    P.dma("pool", w[:], wguD[e, j].rearrange("p (kc c) -> p kc c", kc=8), writes=[wk])
                    for s in range(2):
                        ss = slice(s * 512, (s + 1) * 512)
                        i2 = ecnt % 2
                        ecnt += 1
                        gb_, ub_ = self.banks[i2], self.banks[2 + i2]
                        for kc in range(8):
                            self.mm(gb_[:], w[:, kc, 0:128], hT[:, kc, ss], kc == 0, kc == 7, [wk, "hTg"], [f"pb{i2}"])
                        for kc in range(8):
                            self.mm(ub_[:], w[:, kc, 128:256], hT[:, kc, ss], kc == 0, kc == 7, [wk, "hTg"],
                                    [f"pb{2 + i2}"])
                        self.ts("dve", gc[i2][:], gb_[:], bgu[:, e, j:j + 1], 7.0, ALU.add, ALU.min,
                                [f"pb{i2}", "bgu"], [f"gc{i2}"])
                        self.act(sg[i2][:], gc[i2][:], AF.Sigmoid, [f"gc{i2}"], [f"sg{i2}"], scale=1.702)
                        self.ts("dve", uc[i2][:], ub_[:], bgu[:, e, 8 + j:9 + j], 7.0, ALU.add, ALU.min,
                                [f"pb{2 + i2}", "bgu"], [f"uc{i2}"])
                        self.ts("pool", uc[i2][:], uc[i2][:], -7.0, 1.0, ALU.max, ALU.add, [f"uc{i2}"], [f"uc{i2}"])
                        self.tt("pool", t1[i2][:], gc[i2][:], gbs[s][:], ALU.mult, [f"gc{i2}", f"gbs{s}"], [f"t1{i2}"])
                        self.tt("pool", uc[i2][:], uc[i2][:], t1[i2][:], ALU.mult, [f"uc{i2}", f"t1{i2}"], [f"uc{i2}"])
                        self.tt("dve", actT[:, j, ss], uc[i2][:], sg[i2][:], ALU.mult, [f"uc{i2}", f"sg{i2}"], ["actT"])
                for dj in range(8):
                    w = wdn[dcnt % 4]
                    wk = f"wdn{dcnt % 4}"
                    dcnt += 1
                    P.dma("pool", w[:], wdD[e, dj].rearrange("p (j c) -> p j c", j=8), writes=[wk])
                    for s in range(2):
                        ss = slice(s * 512, (s + 1) * 512)
                        pb = 4 + (dj * 2 + s) % 2
                        ps = self.banks[pb]
                        for j in range(8):
                            self.mm(ps[:], w[:, j, :], actT[:, j, ss], j == 0, j == 7, [wk, "actT"], [f"pb{pb}"])
                        self.tt("dve", acc[:, dj, ss], acc[:, dj, ss], ps[:], ALU.add, ["h32", f"pb{pb}"], ["h32"])
            if getattr(self, "debug", False):
                if tg == 0:
                    self.dbg_m = self.dout("d_m", [D, T])
                    self.dbg_g = self.dout("d_g", [E, T])
                P.dma("sp", self.dbg_m.rearrange("(kc p) t -> p kc t", p=128)[:, :, tsl], acc[:], reads=["h32"], out=True)
                P.dma("sp", self.dbg_g[:, tsl], gT[:], reads=["gT"], out=True)
            for j in range(8):
                self.stt("dve", x[:, j, :], acc[:, j, :], mods["G2"][:, j:j + 1], x[:, j, :], ALU.mult, ALU.add,
                         ["h32", mods["kmod"], "xg"], ["xg"])
            if final:
                for s in range(2):
                    ss = slice(s * 512, (s + 1) * 512)
                    self.rstd_of(x[:, :, ss], ["xg"], 512, sq, rstd[:], "m", 2)
                    for kc in range(8):
                        self.stt("dve", x[:, kc, ss], x[:, kc, ss], fnc[:, kc:kc + 1], rstd[:], ALU.mult, ALU.mult,
                                 ["xg", "fnc", "rstdm"], ["xg"])
            P.dma("sp", XOv[:, :, tsl], x[:], reads=["xg"], writes=[("XT", g) for g in gl], out=True)
        sb.release(m0)


def _cols(v, n):
    return np.ascontiguousarray(np.asarray(v, np.float32).reshape(n, 128).T)


def core_consts(inputs, core):
    b, hf = core // 2, core % 2
    p = np.arange(128)
    i = np.arange(512)
    k = {}
    k["k_ident"] = np.eye(128, dtype=np.float32)
    k["k_onesf"] = np.ones((128, 128), np.float32)
    k["k_onesb"] = np.ones((128, 128), NPBF)
    k["k_identb"] = np.eye(128, dtype=np.float32).astype(NPBF)
    k["k_negtri"] = (-(p[:, None] >= p[None, :]).astype(np.float32)).astype(NPBF)
    k["k_negones"] = (-np.ones((128, 128), np.float32)).astype(NPBF)
    sel = np.zeros((E, E, 128), np.float32)
    for e in range(E):
        sel[e, e, :] = 1.0
    k["k_sel"] = sel.reshape(E, E * 128).astype(NPBF)
    mA = np.zeros((128, 4, 512), np.float32)
    mB = np.zeros((128, 4, 512), np.float32)
    for d in range(4):
        kp = 128 * d + p[:, None]
        mA[:, d, :] = np.where(kp <= i[None, :], 0.0, NEG)
        mB[:, d, :] = np.where(kp < i[None, :], 0.0, NEG)
    k["k_maskA"] = mA.reshape(128, 2048).astype(NPBF)
    k["k_maskB"] = mB.reshape(128, 2048).astype(NPBF)
    k["k_flagcol"] = np.full((128, 1), 0.0 if hf == 1 else NEG, np.float32)
    k["k_zcol"] = np.zeros((128, 1), np.float32)
    k["k_cT"] = _cols(inputs["c"][b], 8)
    return k


def alibi_tables(hf):
    qp = hf * T + np.arange(T)
    kp = np.concatenate([(1 - hf) * T + np.arange(T), hf * T + np.arange(T)])
    qa = np.zeros((8, 4, T), np.float32)
    ka = np.zeros((8, 4, 2 * T), np.float32)
    for h in range(8):
        s = 2.0 ** (-(h + 1))
        qa[h, 0] = -s * 256.0 * (qp // 256)
        qa[h, 1] = -s * (qp % 256)
        qa[h, 2] = 1.0
        qa[h, 3] = 1.0
        ka[h, 0] = 1.0
        ka[h, 1] = 1.0
        ka[h, 2] = s * 256.0 * (kp // 256)
        ka[h, 3] = s * (kp % 256)
    return qa.astype(NPBF), ka.astype(NPBF)


def layer_shared(inputs, l, cache):
    if l in cache:
        return cache[l]
    f = np.float32
    w = {}
    w[f"ada_w{l}"] = np.ascontiguousarray(inputs["ada_w"][l], f)
    w[f"ada_bT{l}"] = _cols(inputs["ada_b"][l], 48)
    w[f"nmT{l}"] = np.concatenate([_cols(inputs["norm_mix"][l], 8), _cols(inputs["norm_moe"][l], 8)], axis=1)
    if l < NA:
        w[f"wqkv{l}"] = np.ascontiguousarray(inputs["a_wqkv"][l], f)
        w[f"wo{l}"] = np.ascontiguousarray(inputs["a_wo"][l], f)
        w[f"lamrep{l}"] = np.ascontiguousarray(np.tile(np.asarray(inputs["a_lambda"][l], f).reshape(1, 256), (128, 1)))
        w[f"sublnT{l}"] = np.asarray(inputs["a_subln"][l], f).reshape(128, 1).copy()
    else:
        w[f"wq{l}"] = np.ascontiguousarray(inputs["b_wq"][l - NA], f)
        w[f"wo{l}"] = np.ascontiguousarray(inputs["b_wo"][l - NA], f)
    if l == NA:
        w["kvw"] = np.ascontiguousarray(inputs["kv_w"], f)
        w["kv_ada_w"] = np.ascontiguousarray(inputs["kv_ada_w"], f)
        w["kv_ada_bT"] = _cols(inputs["kv_ada_b"], 16)
        w["kvnT"] = _cols(inputs["kv_norm"], 8)
    w[f"rw{l}"] = np.ascontiguousarray(inputs["router_w"][l], f)
    w[f"rb{l}"] = np.asarray(inputs["router_b"][l], f).reshape(1, E).copy()
    w[f"bd{l}"] = np.ascontiguousarray(inputs["b_down"][l], f)
    w[f"bguT{l}"] = np.ascontiguousarray(np.asarray(inputs["b_gate_up"][l], f).reshape(E, 16, 128).transpose(2, 0, 1))
    gu = np.asarray(inputs["w_gate_up"][l], f).reshape(E, 8, 128, 2, 8, 128)
    w[f"wgu{l}"] = np.ascontiguousarray(gu.transpose(0, 4, 2, 1, 3, 5)).reshape(E, 8, 128, 2048)
    dn = np.asarray(inputs["w_down"][l], f).reshape(E, 8, 128, 8, 128)
    w[f"wd{l}"] = np.ascontiguousarray(dn.transpose(0, 3, 2, 1, 4)).reshape(E, 8, 128, 1024)
    if l == DEPTH - 1:
        w["fnT"] = _cols(inputs["final_norm"], 8)
    cache[l] = w
    return w


def build_proj(l):
    nc = bass.Bass("TRN2", target_bir_lowering=False)
    B = Builder(nc)
    B.load_consts()
    XT = B.din("XT", [D, T])
    mods = B.layer_mods(l)
    outs = {"Q": B.dout("Q", [D, T], BF16)}
    if l < NA:
        outs["K"] = B.dout("K", [D, T], BF16)
        outs["V"] = B.dout("V", [T, D], BF16)
    elif l == NA:
        outs["KB"] = B.dout("KB", [D, T], BF16)
        outs["VB"] = B.dout("VB", [T, D], BF16)
    B.phase_proj(l, XT, mods, outs)
    for o in B.P.ops:
        if o is not None and o.is_dma and any(isinstance(k, tuple) and k[0] in ("Q", "K", "V", "KB", "VB") for k in o.writes):
            o.is_out = True
    B.P.emit()
    return nc, B


def build_res(l, debug=False):
    nc = bass.Bass("TRN2", target_bir_lowering=False)
    B = Builder(nc)
    B.debug = debug
    B.load_consts()
    XT = B.din("XT", [D, T])
    mods = B.layer_mods(l)
    QT = B.din("Q", [D, T], BF16)
    Ko = B.din("Ko", [D, T], BF16)
    Kp = B.din("Kp", [D, T], BF16)
    Vo = B.din("Vo", [T, D], BF16)
    Vp = B.din("Vp", [T, D], BF16)
    OT = B.dint("OT", [D, T], BF16)
    XO = B.dout("XO", [D, T])
    if l < NA:
        qa = B.din("qaug", [8, 4, T], BF16)
        ka = B.din("kaug", [8, 4, 2 * T], BF16)
        B.phase_att_a(l, QT, Ko, Kp, Vo, Vp, OT, [], qa, ka)
    else:
        B.phase_att_b(l, QT, Ko, Kp, Vo, Vp, OT, [], lambda hp: [], [])
    B.P.barrier()
    B.phase_res_moe(l, XT, OT, mods, XO, final=(l == DEPTH - 1))
    B.P.emit()
    return nc, B


PAIRS = [[0, 1], [2, 3], [4, 5], [6, 7]]


def build_fused():
    nc = bass.Bass("TRN2", target_bir_lowering=False)
    B = Builder(nc)
    P = B.P
    B.load_consts()
    XIN = B.din("XT", [D, T])
    XT = B.dint("XTs", [D, T])
    XO = B.dout("XO", [D, T])
    QT = B.dint("Qs", [D, T], BF16)
    OT = B.dint("OTs", [D, T], BF16)
    Kx = {n: B.dint(n + "s", [D, T], BF16) for n in ("K", "KB")}
    Vx = {n: B.dint(n + "s", [T, D], BF16) for n in ("V", "VB")}
    Kall = {n: B.dint(n + "all", [2 * D, T], BF16) for n in ("K", "KB")}
    Vall = {n: B.dint(n + "all", [2 * T, D], BF16) for n in ("V", "VB")}
    Kp = {n: B.dint(n + "p", [D, T], BF16) for n in ("K", "KB")}
    Vp = {n: B.dint(n + "p", [T, D], BF16) for n in ("V", "VB")}
    qa = B.din("qaug", [8, 4, T], BF16)
    ka = B.din("kaug", [8, 4, 2 * T], BF16)
    XIv = XIN.rearrange("(kc p) t -> p kc t", p=128)
    XTv = XT.rearrange("(kc p) t -> p kc t", p=128)
    for g in range(NG):
        P.dma("sp", XTv[:, :, g * 512:(g + 1) * 512], XIv[:, :, g * 512:(g + 1) * 512], writes=[("XT", g)])
    bypass = ALU.bypass

    _oth = {}

    def oth(e):
        if "v" not in _oth:
            _oth["v"] = (e.partition_id() + 1) % 2
        return _oth["v"]

    NCH = 4

    def exchange(kn, vn):
        kr, vr = D // NCH, T // NCH
        Kc = Kall[kn].rearrange("(i r q) t -> i (r q) t", i=NCH, r=2)
        Vc = Vall[vn].rearrange("(i r q) c -> i (r q) c", i=NCH, r=2)
        for i in range(NCH):
            kkeys = [(kn, j, g) for j in range(2 * i, 2 * i + 2) for g in range(NG)]
            vkeys = [(vn, g) for g in range(2 * i, 2 * i + 2)]
            P.cc(lambda e, i=i: e.collective_compute("AllGather", bypass, replica_groups=PAIRS,
                                                     ins=[Kx[kn][i * kr:(i + 1) * kr, :]], outs=[Kc[i]]),
                 reads=kkeys, writes=[(kn + "all", i)])
            P.cc(lambda e, i=i: e.collective_compute("AllGather", bypass, replica_groups=PAIRS,
                                                     ins=[Vx[vn][i * vr:(i + 1) * vr, :]], outs=[Vc[i]]),
                 reads=vkeys, writes=[(vn + "all", i)])
        for i in range(NCH):
            P.op("pool", lambda e, i=i: e.dma_start(out=Kp[kn][i * kr:(i + 1) * kr, :],
                                                    in_=Kc[i][bass.ds(oth(e) * kr, kr), :]),
                 reads=[(kn + "all", i)], writes=[(kn + "p", i)], dma=True)
            P.op("pool", lambda e, i=i: e.dma_start(
                out=Vp[vn][i * vr:(i + 1) * vr, :].rearrange("(n p) c -> p n c", p=128),
                in_=Vc[i][bass.ds(oth(e) * vr, vr), :].rearrange("(n p) c -> p n c", p=128)),
                 reads=[(vn + "all", i)], writes=[(kn + "pv", i)], dma=True)

    def prev_keys(kn):
        return [(kn + "p", i) for i in range(NCH)] + [(kn + "pv", i) for i in range(NCH)]

    for l in range(DEPTH):
        mods = B.layer_mods(l)
        outs = {"Q": QT}
        if l < NA:
            outs["K"], outs["V"] = Kx["K"], Vx["V"]
        elif l == NA:
            outs["KB"], outs["VB"] = Kx["KB"], Vx["VB"]
        B.phase_proj(l, XT, mods, outs)
        if l < NA:
            exchange("K", "V")
            B.phase_att_a(l, QT, Kx["K"], Kp["K"], Vx["V"], Vp["V"], OT, prev_keys("K"), qa, ka)
        else:
            if l == NA:
                exchange("KB", "VB")
            B.phase_att_b(l, QT, Kx["KB"], Kp["KB"], Vx["VB"], Vp["VB"], OT, prev_keys("KB"),
                          lambda hp: [("KB", hp, g) for g in range(NG)], [("VB", g) for g in range(NG)])
        last = l == DEPTH - 1
        B.phase_res_moe(l, XT, OT, mods, XO if last else XT, final=last)
    P.emit()
    return nc, B


_PROG = {}


def kernel(**inputs):
    ncores = 8
    if "fused" not in _PROG:
        _PROG["fused"] = build_fused()
    nc, B = _PROG["fused"]
    x = np.asarray(inputs["x"], np.float32)
    cache = {}
    shared = {}
    for l in range(DEPTH):
        shared.update(layer_shared(inputs, l, cache))
    names = [n for n in B.dram if n in shared]
    maps = []
    for c in range(ncores):
        m = dict(core_consts(inputs, c))
        m["XT"] = np.ascontiguousarray(x[c // 2, (c % 2) * T:(c % 2 + 1) * T, :].T)
        m["qaug"], m["kaug"] = alibi_tables(c % 2)
        for n in names:
            m[n] = shared[n]
        maps.append(m)
    res = run_bass_kernel_spmd(nc, maps, core_ids=list(range(ncores))).results
    out = np.empty((BATCH, SEQ, D), np.float32)
    for c in range(ncores):
        out[c // 2, (c % 2) * T:(c % 2 + 1) * T, :] = res[c]["XO"].T
    return out
```

```python
import math
import numpy as np
import ml_dtypes
import concourse.bass as bass
import concourse.mybir as mybir
from concourse.bass_utils import run_bass_kernel_spmd

F32 = mybir.dt.float32
BF16 = mybir.dt.bfloat16
AF = mybir.ActivationFunctionType
ALU = mybir.AluOpType
NPBF = ml_dtypes.bfloat16

D = 1024
SEQ = 8192
BATCH = 4
DEPTH = 4
NA = 2
T = 4096
NG = 8
E = 32
EPS = 1e-6
NEG = -32768.0
ENGS = ("pe", "dve", "act", "pool", "sp")


class Op:
    __slots__ = ("eng", "fn", "reads", "writes", "is_dma", "deps", "signal", "sem", "semval",
                 "idx", "is_out", "prev_on_sem", "bar", "is_cc")

    def __init__(self, eng, fn, reads, writes, is_dma, is_out):
        self.eng = eng
        self.fn = fn
        self.reads = reads
        self.writes = writes
        self.is_dma = is_dma
        self.deps = []
        self.signal = False
        self.sem = None
        self.semval = 0
        self.is_out = is_out
        self.prev_on_sem = 0
        self.is_cc = False


class Prog:
    def __init__(self, nc, n_dma_sems=20):
        self.nc = nc
        self.ops = []
        self.state = {}
        self.n_dma_sems = n_dma_sems

    def op(self, eng, fn, reads=(), writes=(), dma=False, out=False):
        o = Op(eng, fn, tuple(reads), tuple(writes), dma, out)
        o.idx = len(self.ops)
        deps = set()
        for k in o.reads:
            st = self.state.get(k)
            if st is not None and st[0] is not None:
                deps.add(st[0])
        for k in o.writes:
            st = self.state.get(k)
            if st is not None:
                if st[0] is not None:
                    deps.add(st[0])
                for r in st[1]:
                    deps.add(r)
        for k in o.reads:
            if isinstance(k, str) and k.startswith("c:"):
                continue
            st = self.state.setdefault(k, [None, []])
            st[1].append(o)
        for k in o.writes:
            self.state[k] = [o, []]
        deps.discard(o)
        o.deps = sorted(deps, key=lambda d: d.idx)
        self.ops.append(o)
        return o

    def dma(self, eng, out_ap, in_ap, reads=(), writes=(), out=False):
        return self.op(eng, lambda e: e.dma_start(out=out_ap, in_=in_ap), reads, writes, dma=True, out=out)

    def cc(self, fn, reads=(), writes=()):
        o = self.op("pool", fn, reads, writes, dma=True)
        o.is_cc = True
        return o

    def barrier(self):
        self.ops.append(None)

    def emit(self):
        nc = self.nc
        ops = self.ops
        real = [o for o in ops if o is not None]
        for o in real:
            for d in o.deps:
                if d.is_dma:
                    continue
                if d.eng == o.eng and d.eng == "pe" and not o.is_dma:
                    continue
                d.signal = True
        last = {}
        for o in ops:
            if o is None:
                for e, lo in last.items():
                    lo.signal = True
            elif not o.is_dma:
                last[o.eng] = o
        esem = {e: nc.alloc_semaphore(f"s_{e}") for e in ENGS}
        dq = ("sp", "act", "pool")
        dsem = {e: [nc.alloc_semaphore(f"d_{e}_{i}") for i in range(self.n_dma_sems)] for e in dq}
        dcount = {e: [0] * self.n_dma_sems for e in dq}
        rr = {e: 0 for e in dq}
        tick = {e: 0 for e in ENGS}
        bars = []
        ccs = []
        per = {e: [] for e in ENGS}
        nbar = 0
        for o in ops:
            if o is None:
                w = {}
                for e in ENGS:
                    if tick[e] > 0:
                        w[esem[e]] = tick[e]
                for e in dq:
                    for i in range(self.n_dma_sems):
                        if dcount[e][i] > 0:
                            w[dsem[e][i]] = dcount[e][i] * 16
                for co in ccs:
                    w[co.sem] = 1
                bars.append(w)
                nbar += 1
                continue
            e = o.eng
            o.bar = nbar
            per[e].append(o)
            if o.is_cc:
                o.sem = nc.alloc_semaphore(f"cc_{o.idx}")
                o.semval = 1
                ccs.append(o)
            elif o.is_dma:
                k = rr[e]
                rr[e] = (k + 1) % self.n_dma_sems
                o.prev_on_sem = dcount[e][k] * 16
                dcount[e][k] += 1
                o.sem = dsem[e][k]
                o.semval = dcount[e][k] * 16
            elif o.signal:
                tick[e] += 1
                o.sem = esem[e]
                o.semval = tick[e]
        out_waits = {}
        for o in real:
            if o.is_dma and o.is_out:
                out_waits[o.sem] = max(out_waits.get(o.sem, 0), o.semval)
        self.stats = {e: len(per[e]) for e in ENGS}

        def run(e, engobj):
            seen = {}
            curbar = 0
            for o in per[e]:
                waits = {}
                if o.bar > curbar:
                    curbar = o.bar
                    waits.update(bars[curbar - 1])
                for d in o.deps:
                    if (not d.is_dma) and d.eng == e and e == "pe" and not o.is_dma:
                        continue
                    if d.sem is None:
                        continue
                    if waits.get(d.sem, 0) < d.semval:
                        waits[d.sem] = d.semval
                if o.is_dma and o.prev_on_sem > 0 and waits.get(o.sem, 0) < o.prev_on_sem:
                    waits[o.sem] = o.prev_on_sem
                for s, v in waits.items():
                    if seen.get(s, 0) >= v:
                        continue
                    seen[s] = v
                    engobj.wait_ge(s, v)
                ins = o.fn(engobj)
                if o.is_cc:
                    ins.then_inc(o.sem)
                elif o.is_dma:
                    ins.then_inc(o.sem, 16)
                elif o.signal:
                    ins.then_inc(o.sem, 1)
            if e == "sp":
                for s, v in out_waits.items():
                    engobj.wait_ge(s, v)

        with nc.Block() as block:
            @block.tensor
            def _(t):
                run("pe", t)

            @block.vector
            def _(v):
                run("dve", v)

            @block.scalar
            def _(s):
                run("act", s)

            @block.gpsimd
            def _(g):
                run("pool", g)

            @block.sync
            def _(s):
                run("sp", s)


class SbufAlloc:
    def __init__(self, nc, base=16512, limit=229312, prog=None):
        self.nc = nc
        self.prog = prog
        self.off = base
        self.limit = limit
        self.n = 0

    def mark(self):
        return self.off

    def release(self, m):
        self.off = m
        if self.prog is not None:
            self.prog.barrier()

    def alloc(self, shape, dtype, name="t"):
        esz = 4 if dtype == F32 else 2
        nbytes = int(np.prod(shape[1:])) * esz
        nbytes = (nbytes + 63) // 64 * 64
        assert self.off + nbytes <= self.limit, f"SBUF overflow {name} {self.off}+{nbytes}"
        self.n += 1
        t = self.nc.alloc_sbuf_tensor_at(f"{name}_{self.n}", list(shape), dtype, offset=self.off)
        self.off += nbytes
        return t


class Builder:
    def __init__(self, nc):
        self.nc = nc
        self.P = Prog(nc)
        self.sb = SbufAlloc(nc, prog=self.P)
        self.banks = [nc.alloc_psum_tensor(f"pb{i}", [128, 512], F32) for i in range(8)]
        self.uid = 0
        self.dram = {}

    def din(self, name, shape, dtype=F32):
        if name in self.dram:
            return self.dram[name]
        t = self.nc.dram_tensor(name, list(shape), dtype, kind="ExternalInput").ap()
        self.dram[name] = t
        return t

    def dout(self, name, shape, dtype=F32):
        t = self.nc.dram_tensor(name, list(shape), dtype, kind="ExternalOutput").ap()
        self.dram[name] = t
        return t

    def dint(self, name, shape, dtype=F32):
        t = self.nc.dram_tensor(name, list(shape), dtype).ap()
        self.dram[name] = t
        return t

    def mm(self, out, lhsT, rhs, start, stop, reads, writes):
        self.P.op("pe", lambda e: e.matmul(out, lhsT, rhs, start=start, stop=stop), reads, writes)

    def act(self, out, in_, func, reads, writes, bias=0.0, scale=1.0):
        self.P.op("act", lambda e: e.activation(out=out, in_=in_, func=func, bias=bias, scale=scale),
                  reads, writes)

    def tt(self, eng, out, in0, in1, op, reads, writes):
        self.P.op(eng, lambda e: e.tensor_tensor(out=out, in0=in0, in1=in1, op=op), reads, writes)

    def ts(self, eng, out, in0, s1, s2, op0, op1, reads, writes):
        if s2 is None:
            self.P.op(eng, lambda e: e.tensor_single_scalar(out=out, in_=in0, scalar=s1, op=op0), reads, writes)
        else:
            self.P.op(eng, lambda e: e.tensor_scalar(out=out, in0=in0, scalar1=s1, scalar2=s2, op0=op0, op1=op1),
                      reads, writes)

    def stt(self, eng, out, in0, scalar, in1, op0, op1, reads, writes):
        self.P.op(eng, lambda e: e.scalar_tensor_tensor(out=out, in0=in0, scalar=scalar, in1=in1, op0=op0, op1=op1),
                  reads, writes)

    def copy(self, eng, out, in_, reads, writes):
        self.P.op(eng, lambda e: e.tensor_copy(out=out, in_=in_), reads, writes)

    def load_consts(self):
        sb, P = self.sb, self.P
        C = {}
        specs = [("ident", [128, 128], F32), ("onesf", [128, 128], F32), ("onesb", [128, 128], BF16),
                 ("negtri", [128, 128], BF16), ("negones", [128, 128], BF16),
                 ("identb", [128, 128], BF16),
                 ("flagcol", [128, 1], F32), ("cT", [128, 8], F32), ("zcol", [128, 1], F32)]
        for name, shape, dt in specs:
            d = self.din("k_" + name, shape, dt)
            t = sb.alloc(shape, dt, name)
            P.dma("sp", t[:], d, writes=["c:" + name])
            C[name] = t
        self.C = C
        sig = sb.alloc([128, 8], F32, "csig")
        cact = sb.alloc([128, 8], F32, "cact")
        self.act(sig[:], C["cT"][:], AF.Sigmoid, ["c:cT"], ["csig"])
        self.tt("dve", cact[:], C["cT"][:], sig[:], ALU.mult, ["c:cT", "csig"], ["c:cact"])
        C["cact"] = cact

    def mod_cols(self, w_ap, bT_ap, ncols, tag):
        sb, P, C = self.sb, self.P, self.C
        nch = ncols // 128
        out = sb.alloc([128, nch], F32, "mod" + tag)
        bt = sb.alloc([128, nch], F32, "modb" + tag)
        P.dma("sp", bt[:], bT_ap, writes=["modb" + tag])
        m = sb.mark()
        slabw = 768 if ncols % 768 == 0 else 512
        nslab = ncols // slabw
        slabs = [sb.alloc([128, 8, slabw], F32, "slab") for _ in range(2)]
        wv = w_ap.rearrange("(kc p) n -> p kc n", p=128)
        ps = self.banks[7]
        for s in range(nslab):
            sl = slabs[s % 2]
            key = f"slab{s % 2}"
            P.dma("sp", sl[:], wv[:, :, s * slabw:(s + 1) * slabw], writes=[key])
            for n in range(slabw // 128):
                nn = s * (slabw // 128) + n
                for kc in range(8):
                    self.mm(ps[:, nn:nn + 1], sl[:, kc, n * 128:(n + 1) * 128], C["cact"][:, kc:kc + 1],
                            kc == 0, kc == 7, [key, "c:cact"], ["pb7"])
        self.tt("dve", out[:], ps[:, 0:nch], bt[:], ALU.add, ["pb7", "modb" + tag], ["mod" + tag])
        sb.release(m)
        return out, "mod" + tag

    def layer_mods(self, l):
        sb, P = self.sb, self.P
        ada_w = self.din(f"ada_w{l}", [D, 6 * D])
        ada_bT = self.din(f"ada_bT{l}", [128, 48])
        nmT = self.din(f"nmT{l}", [128, 16])
        mod, mk = self.mod_cols(ada_w, ada_bT, 6 * D, f"L{l}")
        nm = sb.alloc([128, 16], F32, "nm")
        P.dma("sp", nm[:], nmT, writes=[f"nm{l}"])
        A = sb.alloc([128, 16], F32, "Acols")
        self.stt("dve", A[:, 0:8], mod[:, 8:16], 1.0, nm[:, 0:8], ALU.add, ALU.mult, [mk, f"nm{l}"], [f"A1_{l}"])
        self.stt("dve", A[:, 8:16], mod[:, 32:40], 1.0, nm[:, 8:16], ALU.add, ALU.mult, [mk, f"nm{l}"], [f"A2_{l}"])
        return dict(A1=A[:, 0:8], B1=mod[:, 0:8], G1=mod[:, 16:24], A2=A[:, 8:16], B2=mod[:, 24:32],
                    G2=mod[:, 40:48], kA1=f"A1_{l}", kA2=f"A2_{l}", kmod=mk)

    def rstd_of(self, x, xkeys, n, sq, rstd, tag, bank, dim=1024.0, nk=8):
        C = self.C
        ps = self.banks[bank]
        for kc in range(nk):
            q = sq[kc % 2]
            self.act(q[:, 0:n], x[:, kc, :], AF.Square, xkeys, [f"sq{tag}{kc % 2}"])
            self.mm(ps[:, 0:n], C["onesb"][:], q[:, 0:n], kc == 0, kc == nk - 1,
                    [f"sq{tag}{kc % 2}", "c:onesb"], [f"pb{bank}"])
        self.ts("dve", rstd, ps[:, 0:n], 1.0 / dim, EPS, ALU.mult, ALU.add, [f"pb{bank}"], ["rstd" + tag])
        self.act(rstd, rstd, AF.Ln, ["rstd" + tag], ["rstd" + tag])
        self.act(rstd, rstd, AF.Exp, ["rstd" + tag], ["rstd" + tag], scale=-0.5)

    def phase_proj(self, l, XT, mods, outs):
        sb, P, C = self.sb, self.P, self.C
        m0 = sb.mark()
        isA = l < NA
        wts = {}
        if isA:
            w = self.din(f"wqkv{l}", [D, 3 * D]).rearrange("(kc p) n -> p kc n", p=128)
            for i, nm in enumerate(("Q", "K", "V")):
                t = sb.alloc([128, 8, D], BF16, "w" + nm)
                P.dma("pool", t[:], w[:, :, i * D:(i + 1) * D], writes=["w" + nm])
                wts[nm] = t
        else:
            w = self.din(f"wq{l}", [D, D]).rearrange("(kc p) n -> p kc n", p=128)
            t = sb.alloc([128, 8, D], BF16, "wQ")
            P.dma("pool", t[:], w, writes=["wQ"])
            wts["Q"] = t
            if "KB" in outs:
                w = self.din("kvw", [D, 2 * D]).rearrange("(kc p) n -> p kc n", p=128)
                for i, nm in enumerate(("KB", "VB")):
                    t = sb.alloc([128, 8, D], BF16, "w" + nm)
                    P.dma("pool", t[:], w[:, :, i * D:(i + 1) * D], writes=["w" + nm])
                    wts[nm] = t
                kvmod, kvk = self.mod_cols(self.din("kv_ada_w", [D, 2 * D]), self.din("kv_ada_bT", [128, 16]),
                                           2 * D, "KV")
                kvn = sb.alloc([128, 8], F32, "kvn")
                P.dma("sp", kvn[:], self.din("kvnT", [128, 8]), writes=["kvn"])
                Akv = sb.alloc([128, 8], F32, "Akv")
                self.stt("dve", Akv[:], kvmod[:, 8:16], 1.0, kvn[:], ALU.add, ALU.mult, [kvk, "kvn"], ["Akv"])
        XTv = XT.rearrange("(kc p) t -> p kc t", p=128)
        xs = [sb.alloc([128, 8, 512], F32, "x") for _ in range(2)]
        sqs = [sb.alloc([128, 512], BF16, "sq") for _ in range(2)]
        rstd = sb.alloc([128, 512], F32, "rstd")
        tmp = [sb.alloc([128, 512], F32, "tmp") for _ in range(2)]
        hT = [sb.alloc([128, 8, 512], BF16, "hT") for _ in range(2)]
        hK = sb.alloc([128, 8, 512], BF16, "hK") if "KB" in outs else None
        ev = [sb.alloc([128, 512], BF16, "ev") for _ in range(4)]
        vst = [sb.alloc([128, 4, D], BF16, "vst") for _ in range(2)]
        evi = 0
        pbi = 0
        for g in range(NG):
            x = xs[g % 2]
            xk = f"x{g % 2}"
            P.dma("sp", x[:], XTv[:, :, g * 512:(g + 1) * 512], reads=[("XT", g)], writes=[xk])
            self.rstd_of(x, [xk], 512, sqs, rstd[:], "p", 6)
            h = hT[g % 2]
            hk = f"hT{g % 2}"
            for kc in range(8):
                tm = tmp[kc % 2]
                self.stt("dve", tm[:], x[:, kc, :], mods["A1"][:, kc:kc + 1], rstd[:], ALU.mult, ALU.mult,
                         [xk, mods["kA1"], "rstdp"], [f"tmp{kc % 2}"])
                self.act(h[:, kc, :], tm[:], AF.Identity, [f"tmp{kc % 2}", mods["kmod"]], [hk],
                         bias=mods["B1"][:, kc:kc + 1])
            if hK is not None:
                for kc in range(8):
                    tm = tmp[kc % 2]
                    self.stt("dve", tm[:], x[:, kc, :], Akv[:, kc:kc + 1], rstd[:], ALU.mult, ALU.mult,
                             [xk, "Akv", "rstdp"], [f"tmp{kc % 2}"])
                    self.act(hK[:, kc, :], tm[:], AF.Identity, [f"tmp{kc % 2}", kvk], ["hK"],
                             bias=kvmod[:, kc:kc + 1])
            if getattr(self, "debug", False) and g == 0:
                P.dma("sp", self.dout("d_rstd", [128, 512]), rstd[:], reads=["rstdp"], out=True)
                P.dma("sp", self.dout("d_hT", [128, 8, 512], BF16), h[:], reads=[hk], out=True)
                P.dma("sp", self.dout("d_x", [128, 8, 512]), x[:], reads=[xk], out=True)
            for nm, src, srck, scale in (("Q", h, hk, 0.125), ("K", h, hk, 1.0), ("KB", hK, "hK", 1.0)):
                if nm not in outs or nm not in wts:
                    continue
                for j in range(8):
                    pb = pbi % 4
                    pbi += 1
                    ps = self.banks[pb]
                    for kc in range(8):
                        self.mm(ps[:], wts[nm][:, kc, j * 128:(j + 1) * 128], src[:, kc, :], kc == 0, kc == 7,
                                ["w" + nm, srck], [f"pb{pb}"])
                    e = ev[evi % 4]
                    ek = f"ev{evi % 4}"
                    evi += 1
                    if j % 2 == 0:
                        self.act(e[:], ps[:], AF.Identity, [f"pb{pb}"], [ek], scale=scale)
                    else:
                        self.ts("dve", e[:], ps[:], scale, None, ALU.mult, None, [f"pb{pb}"], [ek])
                    P.dma("sp", outs[nm][j * 128:(j + 1) * 128, g * 512:(g + 1) * 512], e[:], reads=[ek],
                          writes=[(nm, j, g)])
            for nm, src, srck in (("V", h, hk), ("VB", hK, "hK")):
                if nm not in outs or nm not in wts:
                    continue
                vs = vst[g % 2]
                vk = f"vst{g % 2}"
                for tt_ in range(4):
                    for half in range(2):
                        pb = pbi % 4
                        pbi += 1
                        ps = self.banks[pb]
                        for kc in range(8):
                            self.mm(ps[:], src[:, kc, tt_ * 128:(tt_ + 1) * 128],
                                    wts[nm][:, kc, half * 512:(half + 1) * 512], kc == 0, kc == 7,
                                    ["w" + nm, srck], [f"pb{pb}"])
                        if half == 0:
                            self.act(vs[:, tt_, 0:512], ps[:], AF.Identity, [f"pb{pb}"], [vk])
                        else:
                            self.copy("dve", vs[:, tt_, 512:1024], ps[:], [f"pb{pb}"], [vk])
                P.dma("sp", outs[nm].rearrange("(n p) c -> p n c", p=128)[:, g * 4:(g + 1) * 4, :], vs[:],
                      reads=[vk], writes=[(nm, g)])
        sb.release(m0)

    def phase_att_a(self, l, QT, KTo, KTp, Vo, Vp, OT, kprev_ready, qaugD, kaugD):
        sb, P, C = self.sb, self.P, self.C
        m0 = sb.mark()
        mt = sb.alloc([128, 4 * 512], BF16, "maskA")
        P.dma("sp", mt[:], self.din("k_maskA", [128, 4 * 512], BF16), writes=["c:maskA"])
        C["maskA"] = mt
        lam_init = 0.8 - 0.6 * math.exp(-0.3 * l)
        lamr = sb.alloc([128, 256], F32, "lamr")
        P.dma("sp", lamr[:], self.din(f"lamrep{l}", [128, 256]), writes=["lamr"])
        lprod = sb.alloc([128, 2, 64], F32, "lprod")
        lsum = sb.alloc([128, 2], F32, "lsum")
        lexp = sb.alloc([128, 2], F32, "lexp")
        neglam = sb.alloc([128, 1], F32, "neglam")
        self.tt("dve", lprod[:, 0, :], lamr[:, 0:64], lamr[:, 64:128], ALU.mult, ["lamr"], ["lprod"])
        self.tt("dve", lprod[:, 1, :], lamr[:, 128:192], lamr[:, 192:256], ALU.mult, ["lamr"], ["lprod"])
        P.op("dve", lambda e: e.reduce_sum(out=lsum[:], in_=lprod[:], axis=mybir.AxisListType.X), ["lprod"], ["lsum"])
        self.act(lexp[:], lsum[:], AF.Exp, ["lsum"], ["lexp"])
        self.tt("dve", neglam[:], lexp[:, 1:2], lexp[:, 0:1], ALU.subtract, ["lexp"], ["neglam"])
        self.ts("dve", neglam[:], neglam[:], -lam_init, None, ALU.add, None, ["neglam"], ["neglam"])
        subc = sb.alloc([128, 1], F32, "subc")
        P.dma("sp", subc[:], self.din(f"sublnT{l}", [128, 1]), writes=["subc"])
        self.ts("dve", subc[:], subc[:], 1.0 - lam_init, None, ALU.mult, None, ["subc"], ["subc"])

        kaug = [[sb.alloc([68, 2 * T], BF16, "kaug") for r in range(2)] for _ in range(2)]
        qaug = [[sb.alloc([68, T], BF16, "qaug") for r in range(2)] for _ in range(2)]
        vh = [sb.alloc([128, 64, 128], BF16, "vh") for _ in range(2)]
        pbuf = [sb.alloc([128, 512], BF16, "pbuf") for _ in range(3)]
        rl = sb.alloc([128, 512], F32, "rl")
        o0 = sb.alloc([128, 512], F32, "o0")
        o1 = sb.alloc([128, 512], F32, "o1")
        osq = sb.alloc([128, 512], F32, "osq")
        orstd = sb.alloc([128, 512], F32, "orstd")
        onb = [sb.alloc([128, 512], BF16, "onb") for _ in range(2)]
        Vov = Vo.rearrange("(n p) c -> p n c", p=128)
        Vpv = Vp.rearrange("(n p) c -> p n c", p=128)
        sbank = 0
        pcount = 0
        ocount = 0
        for h in range(8):
            hb = h % 2
            for r in range(2):
                row0 = h * 128 + r * 64
                ka, qa = kaug[hb][r], qaug[hb][r]
                P.dma("sp", ka[0:64, 0:T], KTp[row0:row0 + 64, :], reads=kprev_ready, writes=[f"ka{hb}{r}"])
                P.dma("sp", ka[0:64, T:2 * T], KTo[row0:row0 + 64, :],
                      reads=[("K", h, g) for g in range(NG)], writes=[f"ka{hb}{r}"])
                P.dma("sp", ka[64:68, :], kaugD[h], writes=[f"ka{hb}{r}"])
                P.dma("sp", qa[0:64, :], QT[row0:row0 + 64, :], reads=[("Q", h, g) for g in range(NG)],
                      writes=[f"qa{hb}{r}"])
                P.dma("sp", qa[64:68, :], qaugD[h], writes=[f"qa{hb}{r}"])
            v = vh[hb]
            P.dma("sp", v[:, 0:32, :], Vpv[:, :, h * 128:(h + 1) * 128], reads=kprev_ready, writes=[f"vh{hb}"])
            P.dma("sp", v[:, 32:64, :], Vov[:, :, h * 128:(h + 1) * 128], reads=[("V", g) for g in range(NG)],
                  writes=[f"vh{hb}"])
            for g in range(NG):
                for r in range(2):
                    ka, qa = kaug[hb][r], qaug[hb][r]
                    kak, qak = f"ka{hb}{r}", f"qa{hb}{r}"
                    ob, lb = 4 + r, 6
                    blocks = [(kb, None, True) for kb in range(32)]
                    blocks += [(32 + j, None, False) for j in range(4 * g)]
                    blocks += [(32 + 4 * g + d, d, False) for d in range(4)]
                    nb = len(blocks)
                    pend = []

                    def do_pv(item, first, last, v=v, hb=hb, ob=ob, lb=lb):
                        kb, pk, pt = item
                        self.mm(self.banks[ob][:], v[:, kb, :], pt[:], first, last, [f"vh{hb}", pk], [f"pb{ob}"])
                        self.mm(self.banks[lb][:], C["onesb"][:], pt[:], first, last, ["c:onesb", pk], [f"pb{lb}"])

                    done = 0
                    for bi, (kb, d, isprev) in enumerate(blocks):
                        sbk = sbank % 3
                        sbank += 1
                        ps = self.banks[sbk]
                        self.mm(ps[:], ka[:, kb * 128:(kb + 1) * 128], qa[:, g * 512:(g + 1) * 512], True, d is None,
                                [kak, qak], [f"pb{sbk}"])
                        if d is not None:
                            self.mm(ps[:], C["identb"][:], C["maskA"][:, d * 512:(d + 1) * 512], False, True,
                                    ["c:identb", "c:maskA"], [f"pb{sbk}"])
                        pt = pbuf[pcount % 3]
                        pk = f"pbuf{pcount % 3}"
                        pcount += 1
                        self.act(pt[:], ps[:], AF.Exp, [f"pb{sbk}", "c:flagcol"], [pk],
                                 bias=(C["flagcol"][:, 0:1] if isprev else C["zcol"][:, 0:1]))
                        pend.append((kb, pk, pt))
                        if len(pend) > 1:
                            do_pv(pend.pop(0), done == 0, False)
                            done += 1
                    do_pv(pend.pop(0), done == 0, True)
                    P.op("dve", lambda e: e.reciprocal(out=rl[:], in_=self.banks[lb][:]), [f"pb{lb}"], ["rl"])
                    if r == 0:
                        self.tt("dve", o0[:], self.banks[ob][:], rl[:], ALU.mult, [f"pb{ob}", "rl"], ["o0"])
                    else:
                        self.tt("dve", o1[:], self.banks[ob][:], rl[:], ALU.mult, [f"pb{ob}", "rl"], ["o1"])
                self.stt("dve", o0[:], o1[:], neglam[:, 0:1], o0[:], ALU.mult, ALU.add, ["o1", "neglam", "o0"], ["o0"])
                self.tt("pool", osq[:], o0[:], o0[:], ALU.mult, ["o0"], ["osq"])
                ps = self.banks[7]
                self.mm(ps[:], C["onesf"][:], osq[:], True, True, ["c:onesf", "osq"], ["pb7"])
                self.ts("dve", orstd[:], ps[:], 1.0 / 128.0, EPS, ALU.mult, ALU.add, ["pb7"], ["orstd"])
                self.act(orstd[:], orstd[:], AF.Ln, ["orstd"], ["orstd"])
                self.act(orstd[:], orstd[:], AF.Exp, ["orstd"], ["orstd"], scale=-0.5)
                ob_ = onb[ocount % 2]
                obk = f"onb{ocount % 2}"
                ocount += 1
                self.stt("dve", ob_[:], o0[:], subc[:, 0:1], orstd[:], ALU.mult, ALU.mult, ["o0", "subc", "orstd"], [obk])
                P.dma("sp", OT[h * 128:(h + 1) * 128, g * 512:(g + 1) * 512], ob_[:], reads=[obk], writes=[("O", h, g)])
        sb.release(m0)

    def phase_att_b(self, l, QT, KTo, KTp, Vo, Vp, OT, kprev_ready, kown_keys, vown_keys):
        sb, P, C = self.sb, self.P, self.C
        m0 = sb.mark()
        mt = sb.alloc([128, 4 * 512], BF16, "maskB")
        P.dma("sp", mt[:], self.din("k_maskB", [128, 4 * 512], BF16), writes=["c:maskB"])
        C["maskB"] = mt
        kt = [sb.alloc([128, 2 * T], BF16, "ktb") for _ in range(2)]
        qt = [sb.alloc([128, T], BF16, "qtb") for _ in range(2)]
        vh = [sb.alloc([128, 64, 128], BF16, "vhb") for _ in range(2)]
        ebuf = [sb.alloc([128, 512], F32, "ebuf") for _ in range(2)]
        spb = [sb.alloc([128, 512], BF16, "spb") for _ in range(4)]
        abuf = [sb.alloc([128, 512], BF16, "abuf") for _ in range(4)]
        ssum = sb.alloc([128, 512], F32, "ssum")
        ssb = [sb.alloc([128, 512], BF16, "ssb") for _ in range(2)]
        ost = [sb.alloc([128, 512], BF16, "ost") for _ in range(2)]
        Vov = Vo.rearrange("(n p) c -> p n c", p=128)
        Vpv = Vp.rearrange("(n p) c -> p n c", p=128)
        ssum2 = [ssum, sb.alloc([128, 512], F32, "ssum1")]
        cnt = 0
        for hp in range(8):
            hb = hp % 2
            k_, q_, v = kt[hb], qt[hb], vh[hb]
            kk, qk, vk = f"ktb{hb}", f"qtb{hb}", f"vhb{hb}"
            P.dma("sp", k_[:, 0:T], KTp[hp * 128:(hp + 1) * 128, :], reads=kprev_ready, writes=[kk])
            P.dma("sp", k_[:, T:2 * T], KTo[hp * 128:(hp + 1) * 128, :], reads=[kk_ for kk_ in kown_keys(hp)],
                  writes=[kk])
            P.dma("sp", q_[:], QT[hp * 128:(hp + 1) * 128, :], reads=[("Q", hp, g) for g in range(NG)], writes=[qk])
            P.dma("sp", v[:, 0:32, :], Vpv[:, :, hp * 128:(hp + 1) * 128], reads=kprev_ready, writes=[vk])
            P.dma("sp", v[:, 32:64, :], Vov[:, :, hp * 128:(hp + 1) * 128], reads=vown_keys, writes=[vk])
            for g in range(NG):
                os_ = ost[(hp * NG + g) % 2]
                osk = f"ost{(hp * NG + g) % 2}"
                blocks = [(32 + 4 * g + d, d, False) for d in (3, 2, 1, 0)]
                blocks += [(32 + j, None, False) for j in range(4 * g - 1, -1, -1)]
                blocks += [(kb, None, True) for kb in range(31, -1, -1)]
                nb = len(blocks)
                for bi, (kb, d, isprev) in enumerate(blocks):
                    bias = C["flagcol"][:, 0:1] if isprev else C["zcol"][:, 0:1]
                    mk = C["maskB"][:, d * 512:(d + 1) * 512] if d is not None else None
                    par = bi % 2
                    st = []
                    for hh in range(2):
                        r0 = hh * 64
                        kblk = k_[r0:r0 + 64, kb * 128:(kb + 1) * 128]
                        qblk = q_[r0:r0 + 64, g * 512:(g + 1) * 512]
                        zb = hh
                        zp = self.banks[zb]
                        self.mm(zp[:], kblk, qblk, True, d is None, [kk, qk], [f"pb{zb}"])
                        if d is not None:
                            self.mm(zp[:], C["identb"][:], mk, False, True, ["c:identb", "c:maskB"], [f"pb{zb}"])
                        st.append((kblk, qblk))
                    for hh in range(2):
                        zb = hh
                        eb, ek = ebuf[hh], f"ebuf{hh}"
                        self.act(eb[:], self.banks[zb][:], AF.Exp, [f"pb{zb}", "c:flagcol"], [ek], bias=bias)
                        sp_, spk = spb[hh * 2 + par], f"spb{hh * 2 + par}"
                        self.act(sp_[:], eb[:], AF.Ln, [ek], [spk], bias=1.0)
                    for hh in range(2):
                        kblk, qblk = st[hh]
                        rb = 2 + hh
                        rp = self.banks[rb]
                        sp_, spk = spb[hh * 2 + par], f"spb{hh * 2 + par}"
                        self.mm(rp[:], kblk, qblk, True, False, [kk, qk], [f"pb{rb}"])
                        if d is not None:
                            self.mm(rp[:], C["identb"][:], mk, False, False, ["c:identb", "c:maskB"], [f"pb{rb}"])
                        if bi > 0:
                            self.mm(rp[:], C["negones"][:], ssb[hh][:], False, False, ["c:negones", f"ssb{hh}"],
                                    [f"pb{rb}"])
                        self.mm(rp[:], C["negtri"][:], sp_[:], False, True, ["c:negtri", spk], [f"pb{rb}"])
                    for hh in range(2):
                        rb = 2 + hh
                        at, ak = abuf[hh * 2 + par], f"abuf{hh * 2 + par}"
                        self.act(at[:], self.banks[rb][:], AF.Exp, [f"pb{rb}", "c:flagcol"], [ak], bias=bias)
                    for hh in range(2):
                        r0 = hh * 64
                        ob = 4 + hh
                        at, ak = abuf[hh * 2 + par], f"abuf{hh * 2 + par}"
                        sp_, spk = spb[hh * 2 + par], f"spb{hh * 2 + par}"
                        self.mm(self.banks[ob][0:64, :], v[:, kb, r0:r0 + 64], at[:], bi == 0, bi == nb - 1,
                                [vk, ak], [f"pb{ob}"])
                        if bi < nb - 1:
                            if bi == 0:
                                self.copy("pool", ssum2[hh][:], sp_[:], [spk], [f"ssum{hh}"])
                            else:
                                self.tt("pool", ssum2[hh][:], ssum2[hh][:], sp_[:], ALU.add, [f"ssum{hh}", spk],
                                        [f"ssum{hh}"])
                            self.copy("dve", ssb[hh][:], ssum2[hh][:], [f"ssum{hh}"], [f"ssb{hh}"])
                for hh in range(2):
                    ob = 4 + hh
                    self.copy("dve", os_[hh * 64:(hh + 1) * 64, :], self.banks[ob][0:64, :], [f"pb{ob}"], [osk])
                P.dma("sp", OT[hp * 128:(hp + 1) * 128, g * 512:(g + 1) * 512], os_[:], reads=[osk],
                      writes=[("O", hp, g)])
        sb.release(m0)

    def phase_res_moe(self, l, XT, OT, mods, XOUT, final=False):
        sb, P, C = self.sb, self.P, self.C
        m0 = sb.mark()
        NTG = 1024
        NTT = NTG // 128
        wo = sb.alloc([128, 8, D], BF16, "wo")
        P.dma("pool", wo[:], self.din(f"wo{l}", [D, D]).rearrange("(kc p) n -> p kc n", p=128), writes=["wo"])
        wr = sb.alloc([128, 8, E], F32, "wr")
        P.dma("sp", wr[:], self.din(f"rw{l}", [D, E]).rearrange("(kc p) n -> p kc n", p=128), writes=["wr"])
        rbrow = sb.alloc([1, E], F32, "rbrow")
        P.dma("sp", rbrow[:], self.din(f"rb{l}", [1, E]), writes=["rbrow"])
        bd = sb.alloc([E, D], F32, "bd")
        P.dma("sp", bd[:], self.din(f"bd{l}", [E, D]), writes=["bd"])
        bgu = sb.alloc([128, E, 16], F32, "bgu")
        P.dma("sp", bgu[:], self.din(f"bguT{l}", [128, E, 16]), writes=["bgu"])
        self.ts("dve", bgu[:, :, 8:16], bgu[:, :, 8:16], 1.0, None, ALU.add, None, ["bgu"], ["bgu"])
        wguD = self.din(f"wgu{l}", [E, 8, 128, 8 * 256])
        wdD = self.din(f"wd{l}", [E, D, D])
        if final:
            fnc = sb.alloc([128, 8], F32, "fnc")
            P.dma("sp", fnc[:], self.din("fnT", [128, 8]), writes=["fnc"])
        XTv = XT.rearrange("(kc p) t -> p kc t", p=128)
        OTv = OT.rearrange("(kc p) t -> p kc t", p=128)
        XOv = XOUT.rearrange("(kc p) t -> p kc t", p=128)
        x = sb.alloc([128, 8, NTG], F32, "xg")
        hT = sb.alloc([128, 8, NTG], BF16, "hTg")
        h32 = sb.alloc([128, 8, NTG], F32, "h32acc")
        acc = h32
        actT = sb.alloc([128, 8, NTG], BF16, "actT")
        oT = actT
        sq = [sb.alloc([128, 512], BF16, "sqm") for _ in range(2)]
        rstd = sb.alloc([128, 512], F32, "rstdm")
        tmp = [sb.alloc([128, 512], F32, "tmpm") for _ in range(2)]
        lg = sb.alloc([128, E], F32, "lg")
        m8 = sb.alloc([128, 8], F32, "m8")
        negm = sb.alloc([128, 1], F32, "negm")
        msk = sb.alloc([128, E], F32, "msk")
        ee = sb.alloc([128, E], F32, "ee")
        ssum = sb.alloc([128, 1], F32, "gsum")
        gts = sb.alloc([128, NTT, E], F32, "gts")
        gT = sb.alloc([E, NTG], F32, "gT")
        wgu = [sb.alloc([128, 8, 256], BF16, "wgu") for _ in range(4)]
        wdn = [sb.alloc([128, 8, 512], BF16, "wdn") for _ in range(3)]
        gc = [sb.alloc([128, 512], F32, "gc") for _ in range(2)]
        sg = [sb.alloc([128, 512], F32, "sg") for _ in range(2)]
        ub = [sb.alloc([128, 512], F32, "ub") for _ in range(2)]
        p1 = [sb.alloc([128, 512], F32, "p1") for _ in range(2)]
        wcnt = 0
        dcnt = 0
        ecnt = 0
        ycnt = 0
        for tg in range(T // NTG):
            gl = [2 * tg, 2 * tg + 1]
            tsl = slice(tg * NTG, (tg + 1) * NTG)
            P.dma("sp", x[:], XTv[:, :, tsl], reads=[("XT", g) for g in gl], writes=["xg"])
            P.dma("sp", oT[:], OTv[:, :, tsl], reads=[("O", h, g) for h in range(8) for g in gl], writes=["actT"])
            for s in range(2):
                ss = slice(s * 512, (s + 1) * 512)
                for j in range(8):
                    pb = (s * 8 + j) % 2
                    ps = self.banks[pb]
                    for kc in range(8):
                        self.mm(ps[:], wo[:, kc, j * 128:(j + 1) * 128], oT[:, kc, ss], kc == 0, kc == 7,
                                ["wo", "actT"], [f"pb{pb}"])
                    self.stt("dve", x[:, j, ss], ps[:], mods["G1"][:, j:j + 1], x[:, j, ss], ALU.mult, ALU.add,
                             [f"pb{pb}", mods["kmod"], "xg"], ["xg"])
            if getattr(self, "debug", False):
                if tg == 0:
                    self.dbg_x1 = self.dout("d_x1", [D, T])
                P.dma("sp", self.dbg_x1.rearrange("(kc p) t -> p kc t", p=128)[:, :, tsl], x[:], reads=["xg"], out=True)
            for s in range(2):
                ss = slice(s * 512, (s + 1) * 512)
                self.rstd_of(x[:, :, ss], ["xg"], 512, sq, rstd[:], "m", 2)
                for kc in range(8):
                    tm = tmp[kc % 2]
                    self.stt("dve", tm[:], x[:, kc, ss], mods["A2"][:, kc:kc + 1], rstd[:], ALU.mult, ALU.mult,
                             ["xg", mods["kA2"], "rstdm"], [f"tmpm{kc % 2}"])
                    self.act(h32[:, kc, ss], tm[:], AF.Identity, [f"tmpm{kc % 2}", mods["kmod"]], ["h32"],
                             bias=mods["B2"][:, kc:kc + 1])
                    self.copy("dve", hT[:, kc, ss], h32[:, kc, ss], ["h32"], ["hTg"])
            for tt_ in range(NTT):
                ts_ = slice(tt_ * 128, (tt_ + 1) * 128)
                ps = self.banks[3]
                for kc in range(8):
                    self.mm(ps[:, 0:E], h32[:, kc, ts_], wr[:, kc, :], kc == 0, False, ["h32", "wr"], ["pb3"])
                self.mm(ps[:, 0:E], C["onesf"][0:1, :], rbrow[:], False, True, ["c:onesf", "rbrow"], ["pb3"])
                self.copy("dve", lg[:], ps[:, 0:E], ["pb3"], ["lg"])
                P.op("dve", lambda e: e.max(out=m8[:], in_=lg[:]), ["lg"], ["m8"])
                self.ts("dve", negm[:], m8[:, 0:1], -1.0, None, ALU.mult, None, ["m8"], ["negm"])
                self.ts("dve", msk[:], lg[:], m8[:, 3:4], None, ALU.is_ge, None, ["lg", "m8"], ["msk"])
                self.act(ee[:], lg[:], AF.Exp, ["lg", "negm"], ["ee"], bias=negm[:, 0:1])
                self.tt("dve", ee[:], ee[:], msk[:], ALU.mult, ["ee", "msk"], ["ee"])
                P.op("dve", lambda e: e.reduce_sum(out=ssum[:], in_=ee[:], axis=mybir.AxisListType.X), ["ee"], ["gsum"])
                P.op("dve", lambda e: e.reciprocal(out=ssum[:], in_=ssum[:]), ["gsum"], ["gsum"])
                self.ts("dve", gts[:, tt_, :], ee[:], ssum[:, 0:1], None, ALU.mult, None, ["ee", "gsum"], ["gts"])
                ps2 = self.banks[4]
                self.mm(ps2[0:E, 0:128], gts[:, tt_, :], C["ident"][:], True, True, ["gts", "c:ident"], ["pb4"])
                self.copy("dve", gT[:, ts_], ps2[0:E, 0:128], ["pb4"], ["gT"])
            for tt_ in range(NTT):
                ts_ = slice(tt_ * 128, (tt_ + 1) * 128)
                for half in range(2):
                    pb = 4 + ycnt % 2
                    ycnt += 1
                    ps = self.banks[pb]
                    self.mm(ps[:], gT[:, ts_], bd[:, half * 512:(half + 1) * 512], True, True, ["gT", "bd"], [f"pb{pb}"])
                    self.copy("dve", acc[:, tt_, half * 512:(half + 1) * 512], ps[:], [f"pb{pb}"], ["h32"])
            for e in range(E):
                for j in range(8):
                    w = wgu[wcnt % 4]
                    wk = f"wgu{wcnt % 4}"
                    wcnt += 1
                    P.dma("pool", w[:], wguD[e, j].rearrange("p (kc c) -> p kc c", kc=8), writes=[wk])
                    for s in range(2):
                        ss = slice(s * 512, (s + 1) * 512)
                        i2 = ecnt % 2
                        ecnt += 1
                        gb_, ub_ = self.banks[i2], self.banks[2 + i2]
                        for kc in range(8):
                            self.mm(gb_[:], w[:, kc, 0:128], hT[:, kc, ss], kc == 0, kc == 7, [wk, "hTg"], [f"pb{i2}"])
                        for kc in range(8):
                            self.mm(ub_[:], w[:, kc, 128:256], hT[:, kc, ss], kc == 0, kc == 7, [wk, "hTg"],
                                    [f"pb{2 + i2}"])
                        self.ts("dve", gc[i2][:], gb_[:], bgu[:, e, j:j + 1], 7.0, ALU.add, ALU.min,
                                [f"pb{i2}", "bgu"], [f"gc{i2}"])
                        self.act(sg[i2][:], gc[i2][:], AF.Sigmoid, [f"gc{i2}"], [f"sg{i2}"], scale=1.702)
                        self.act(ub[i2][:], ub_[:], AF.Identity, [f"pb{2 + i2}", "bgu"], [f"ub{i2}"],
                                 bias=bgu[:, e, 8 + j:9 + j])
                        self.ts("dve", ub[i2][:], ub[i2][:], -6.0, 8.0, ALU.max, ALU.min, [f"ub{i2}"], [f"ub{i2}"])
                        self.tt("dve", p1[i2][:], gc[i2][:], sg[i2][:], ALU.mult, [f"gc{i2}", f"sg{i2}"], [f"p1{i2}"])
                        self.tt("dve", actT[:, j, ss], p1[i2][:], ub[i2][:], ALU.mult, [f"p1{i2}", f"ub{i2}"], ["actT"])
                wdv = wdD[e].rearrange("(j p) d -> p j d", p=128)
                for half in range(2):
                    w = wdn[dcnt % 3]
                    wk = f"wdn{dcnt % 3}"
                    dcnt += 1
                    P.dma("pool", w[:], wdv[:, :, half * 512:(half + 1) * 512], writes=[wk])
                    for tt_ in range(NTT):
                        ts_ = slice(tt_ * 128, (tt_ + 1) * 128)
                        pb = 4 + ycnt % 2
                        ycnt += 1
                        ps = self.banks[pb]
                        for j in range(8):
                            self.mm(ps[:], actT[:, j, ts_], w[:, j, :], j == 0, j == 7, [wk, "actT"], [f"pb{pb}"])
                        a_ = acc[:, tt_, half * 512:(half + 1) * 512]
                        self.stt("dve", a_, ps[:], gts[:, tt_, e:e + 1], a_, ALU.mult, ALU.add,
                                 [f"pb{pb}", "gts", "h32"], ["h32"])
            if getattr(self, "debug", False):
                if tg == 0:
                    self.dbg_m = self.dout("d_m", [T, D])
                P.dma("sp", self.dbg_m.rearrange("(n p) d -> p n d", p=128)[:, tg * NTT:(tg + 1) * NTT, :], acc[:],
                      reads=["h32"], out=True)
            for dj in range(8):
                for q4 in range(NTT // 4):
                    pb = 6 + (dj * 2 + q4) % 2
                    ps = self.banks[pb]
                    for t4 in range(4):
                        tt_ = q4 * 4 + t4
                        self.mm(ps[:, t4 * 128:(t4 + 1) * 128], acc[:, tt_, dj * 128:(dj + 1) * 128], C["ident"][:],
                                True, True, ["h32", "c:ident"], [f"pb{pb}"])
                    xs_ = x[:, dj, q4 * 512:(q4 + 1) * 512]
                    self.stt("dve", xs_, ps[:], mods["G2"][:, dj:dj + 1], xs_, ALU.mult, ALU.add,
                             [f"pb{pb}", mods["kmod"], "xg"], ["xg"])
            if final:
                for s in range(2):
                    ss = slice(s * 512, (s + 1) * 512)
                    self.rstd_of(x[:, :, ss], ["xg"], 512, sq, rstd[:], "m", 2)
                    for kc in range(8):
                        self.stt("dve", x[:, kc, ss], x[:, kc, ss], fnc[:, kc:kc + 1], rstd[:], ALU.mult, ALU.mult,
                                 ["xg", "fnc", "rstdm"], ["xg"])
            P.dma("sp", XOv[:, :, tsl], x[:], reads=["xg"], writes=[("XT", g) for g in gl], out=True)
        sb.release(m0)


def _cols(v, n):
    return np.ascontiguousarray(np.asarray(v, np.float32).reshape(n, 128).T)


def core_consts(inputs, core):
    b, hf = core // 2, core % 2
    p = np.arange(128)
    i = np.arange(512)
    k = {}
    k["k_ident"] = np.eye(128, dtype=np.float32)
    k["k_onesf"] = np.ones((128, 128), np.float32)
    k["k_onesb"] = np.ones((128, 128), NPBF)
    k["k_identb"] = np.eye(128, dtype=np.float32).astype(NPBF)
    k["k_negtri"] = (-(p[:, None] >= p[None, :]).astype(np.float32)).astype(NPBF)
    k["k_negones"] = (-np.ones((128, 128), np.float32)).astype(NPBF)
    sel = np.zeros((E, E, 128), np.float32)
    for e in range(E):
        sel[e, e, :] = 1.0
    k["k_sel"] = sel.reshape(E, E * 128).astype(NPBF)
    mA = np.zeros((128, 4, 512), np.float32)
    mB = np.zeros((128, 4, 512), np.float32)
    for d in range(4):
        kp = 128 * d + p[:, None]
        mA[:, d, :] = np.where(kp <= i[None, :], 0.0, NEG)
        mB[:, d, :] = np.where(kp < i[None, :], 0.0, NEG)
    k["k_maskA"] = mA.reshape(128, 2048).astype(NPBF)
    k["k_maskB"] = mB.reshape(128, 2048).astype(NPBF)
    k["k_flagcol"] = np.full((128, 1), 0.0 if hf == 1 else NEG, np.float32)
    k["k_zcol"] = np.zeros((128, 1), np.float32)
    k["k_cT"] = _cols(inputs["c"][b], 8)
    return k


def alibi_tables(hf):
    qp = hf * T + np.arange(T)
    kp = np.concatenate([(1 - hf) * T + np.arange(T), hf * T + np.arange(T)])
    qa = np.zeros((8, 4, T), np.float32)
    ka = np.zeros((8, 4, 2 * T), np.float32)
    for h in range(8):
        s = 2.0 ** (-(h + 1))
        qa[h, 0] = -s * 256.0 * (qp // 256)
        qa[h, 1] = -s * (qp % 256)
        qa[h, 2] = 1.0
        qa[h, 3] = 1.0
        ka[h, 0] = 1.0
        ka[h, 1] = 1.0
        ka[h, 2] = s * 256.0 * (kp // 256)
        ka[h, 3] = s * (kp % 256)
    return qa.astype(NPBF), ka.astype(NPBF)


def layer_shared(inputs, l, cache):
    if l in cache:
        return cache[l]
    f = np.float32
    w = {}
    w[f"ada_w{l}"] = np.ascontiguousarray(inputs["ada_w"][l], f)
    w[f"ada_bT{l}"] = _cols(inputs["ada_b"][l], 48)
    w[f"nmT{l}"] = np.concatenate([_cols(inputs["norm_mix"][l], 8), _cols(inputs["norm_moe"][l], 8)], axis=1)
    if l < NA:
        w[f"wqkv{l}"] = np.ascontiguousarray(inputs["a_wqkv"][l], f)
        w[f"wo{l}"] = np.ascontiguousarray(inputs["a_wo"][l], f)
        w[f"lamrep{l}"] = np.ascontiguousarray(np.tile(np.asarray(inputs["a_lambda"][l], f).reshape(1, 256), (128, 1)))
        w[f"sublnT{l}"] = np.asarray(inputs["a_subln"][l], f).reshape(128, 1).copy()
    else:
        w[f"wq{l}"] = np.ascontiguousarray(inputs["b_wq"][l - NA], f)
        w[f"wo{l}"] = np.ascontiguousarray(inputs["b_wo"][l - NA], f)
    if l == NA:
        w["kvw"] = np.ascontiguousarray(inputs["kv_w"], f)
        w["kv_ada_w"] = np.ascontiguousarray(inputs["kv_ada_w"], f)
        w["kv_ada_bT"] = _cols(inputs["kv_ada_b"], 16)
        w["kvnT"] = _cols(inputs["kv_norm"], 8)
    w[f"rw{l}"] = np.ascontiguousarray(inputs["router_w"][l], f)
    w[f"rb{l}"] = np.asarray(inputs["router_b"][l], f).reshape(1, E).copy()
    w[f"bd{l}"] = np.ascontiguousarray(inputs["b_down"][l], f)
    w[f"bguT{l}"] = np.ascontiguousarray(np.asarray(inputs["b_gate_up"][l], f).reshape(E, 16, 128).transpose(2, 0, 1))
    gu = np.asarray(inputs["w_gate_up"][l], f).reshape(E, 8, 128, 2, 8, 128)
    w[f"wgu{l}"] = np.ascontiguousarray(gu.transpose(0, 4, 2, 1, 3, 5)).reshape(E, 8, 128, 2048)
    w[f"wd{l}"] = np.ascontiguousarray(inputs["w_down"][l], f)
    if l == DEPTH - 1:
        w["fnT"] = _cols(inputs["final_norm"], 8)
    cache[l] = w
    return w


def build_proj(l):
    nc = bass.Bass("TRN2", target_bir_lowering=False)
    B = Builder(nc)
    B.load_consts()
    XT = B.din("XT", [D, T])
    mods = B.layer_mods(l)
    outs = {"Q": B.dout("Q", [D, T], BF16)}
    if l < NA:
        outs["K"] = B.dout("K", [D, T], BF16)
        outs["V"] = B.dout("V", [T, D], BF16)
    elif l == NA:
        outs["KB"] = B.dout("KB", [D, T], BF16)
        outs["VB"] = B.dout("VB", [T, D], BF16)
    B.phase_proj(l, XT, mods, outs)
    for o in B.P.ops:
        if o is not None and o.is_dma and any(isinstance(k, tuple) and k[0] in ("Q", "K", "V", "KB", "VB") for k in o.writes):
            o.is_out = True
    B.P.emit()
    return nc, B


def build_res(l, debug=False):
    nc = bass.Bass("TRN2", target_bir_lowering=False)
    B = Builder(nc)
    B.debug = debug
    B.load_consts()
    XT = B.din("XT", [D, T])
    mods = B.layer_mods(l)
    QT = B.din("Q", [D, T], BF16)
    Ko = B.din("Ko", [D, T], BF16)
    Kp = B.din("Kp", [D, T], BF16)
    Vo = B.din("Vo", [T, D], BF16)
    Vp = B.din("Vp", [T, D], BF16)
    OT = B.dint("OT", [D, T], BF16)
    XO = B.dout("XO", [D, T])
    if l < NA:
        qa = B.din("qaug", [8, 4, T], BF16)
        ka = B.din("kaug", [8, 4, 2 * T], BF16)
        B.phase_att_a(l, QT, Ko, Kp, Vo, Vp, OT, [], qa, ka)
    else:
        B.phase_att_b(l, QT, Ko, Kp, Vo, Vp, OT, [], lambda hp: [], [])
    B.P.barrier()
    B.phase_res_moe(l, XT, OT, mods, XO, final=(l == DEPTH - 1))
    B.P.emit()
    return nc, B


PAIRS = [[0, 1], [2, 3], [4, 5], [6, 7]]


def build_fused():
    nc = bass.Bass("TRN2", target_bir_lowering=False)
    B = Builder(nc)
    P = B.P
    B.load_consts()
    XIN = B.din("XT", [D, T])
    XT = B.dint("XTs", [D, T])
    XO = B.dout("XO", [D, T])
    QT = B.dint("Qs", [D, T], BF16)
    OT = B.dint("OTs", [D, T], BF16)
    Kx = {n: B.dint(n + "s", [D, T], BF16) for n in ("K", "KB")}
    Vx = {n: B.dint(n + "s", [T, D], BF16) for n in ("V", "VB")}
    Kall = {n: B.dint(n + "all", [2 * D, T], BF16) for n in ("K", "KB")}
    Vall = {n: B.dint(n + "all", [2 * T, D], BF16) for n in ("V", "VB")}
    Kp = {n: B.dint(n + "p", [D, T], BF16) for n in ("K", "KB")}
    Vp = {n: B.dint(n + "p", [T, D], BF16) for n in ("V", "VB")}
    qa = B.din("qaug", [8, 4, T], BF16)
    ka = B.din("kaug", [8, 4, 2 * T], BF16)
    XIv = XIN.rearrange("(kc p) t -> p kc t", p=128)
    XTv = XT.rearrange("(kc p) t -> p kc t", p=128)
    for g in range(NG):
        P.dma("sp", XTv[:, :, g * 512:(g + 1) * 512], XIv[:, :, g * 512:(g + 1) * 512], writes=[("XT", g)])
    bypass = ALU.bypass

    _oth = {}

    def oth(e):
        if "v" not in _oth:
            _oth["v"] = (e.partition_id() + 1) % 2
        return _oth["v"]

    NCH = 4

    def exchange(kn, vn):
        kr, vr = D // NCH, T // NCH
        Kc = Kall[kn].rearrange("(i r q) t -> i (r q) t", i=NCH, r=2)
        Vc = Vall[vn].rearrange("(i r q) c -> i (r q) c", i=NCH, r=2)
        for i in range(NCH):
            kkeys = [(kn, j, g) for j in range(2 * i, 2 * i + 2) for g in range(NG)]
            vkeys = [(vn, g) for g in range(2 * i, 2 * i + 2)]
            P.cc(lambda e, i=i: e.collective_compute("AllGather", bypass, replica_groups=PAIRS,
                                                     ins=[Kx[kn][i * kr:(i + 1) * kr, :]], outs=[Kc[i]]),
                 reads=kkeys, writes=[(kn + "all", i)])
            P.cc(lambda e, i=i: e.collective_compute("AllGather", bypass, replica_groups=PAIRS,
                                                     ins=[Vx[vn][i * vr:(i + 1) * vr, :]], outs=[Vc[i]]),
                 reads=vkeys, writes=[(vn + "all", i)])
        for i in range(NCH):
            P.op("pool", lambda e, i=i: e.dma_start(out=Kp[kn][i * kr:(i + 1) * kr, :],
                                                    in_=Kc[i][bass.ds(oth(e) * kr, kr), :]),
                 reads=[(kn + "all", i)], writes=[(kn + "p", i)], dma=True)
            P.op("pool", lambda e, i=i: e.dma_start(
                out=Vp[vn][i * vr:(i + 1) * vr, :].rearrange("(n p) c -> p n c", p=128),
                in_=Vc[i][bass.ds(oth(e) * vr, vr), :].rearrange("(n p) c -> p n c", p=128)),
                 reads=[(vn + "all", i)], writes=[(kn + "pv", i)], dma=True)

    def prev_keys(kn):
        return [(kn + "p", i) for i in range(NCH)] + [(kn + "pv", i) for i in range(NCH)]

    for l in range(DEPTH):
        mods = B.layer_mods(l)
        outs = {"Q": QT}
        if l < NA:
            outs["K"], outs["V"] = Kx["K"], Vx["V"]
        elif l == NA:
            outs["KB"], outs["VB"] = Kx["KB"], Vx["VB"]
        B.phase_proj(l, XT, mods, outs)
        if l < NA:
            exchange("K", "V")
            B.phase_att_a(l, QT, Kx["K"], Kp["K"], Vx["V"], Vp["V"], OT, prev_keys("K"), qa, ka)
        else:
            if l == NA:
                exchange("KB", "VB")
            B.phase_att_b(l, QT, Kx["KB"], Kp["KB"], Vx["VB"], Vp["VB"], OT, prev_keys("KB"),
                          lambda hp: [("KB", hp, g) for g in range(NG)], [("VB", g) for g in range(NG)])
        last = l == DEPTH - 1
        B.phase_res_moe(l, XT, OT, mods, XO if last else XT, final=last)
    P.emit()
    return nc, B


_PROG = {}


def kernel(**inputs):
    ncores = 8
    if "fused" not in _PROG:
        _PROG["fused"] = build_fused()
    nc, B = _PROG["fused"]
    x = np.asarray(inputs["x"], np.float32)
    cache = {}
    shared = {}
    for l in range(DEPTH):
        shared.update(layer_shared(inputs, l, cache))
    names = [n for n in B.dram if n in shared]
    maps = []
    for c in range(ncores):
        m = dict(core_consts(inputs, c))
        m["XT"] = np.ascontiguousarray(x[c // 2, (c % 2) * T:(c % 2 + 1) * T, :].T)
        m["qaug"], m["kaug"] = alibi_tables(c % 2)
        for n in names:
            m[n] = shared[n]
        maps.append(m)
    res = run_bass_kernel_spmd(nc, maps, core_ids=list(range(ncores))).results
    out = np.empty((BATCH, SEQ, D), np.float32)
    for c in range(ncores):
        out[c // 2, (c % 2) * T:(c % 2 + 1) * T, :] = res[c]["XO"].T
    return out
```

```python
import math
import numpy as np
import ml_dtypes
import concourse.bass as bass
import concourse.mybir as mybir
from concourse.bass_utils import run_bass_kernel_spmd

F32 = mybir.dt.float32
BF16 = mybir.dt.bfloat16
AF = mybir.ActivationFunctionType
ALU = mybir.AluOpType
NPBF = ml_dtypes.bfloat16

D = 1024
SEQ = 8192
BATCH = 4
DEPTH = 4
NA = 2
T = 4096
NG = 8
E = 32
EPS = 1e-6
NEG = -32768.0
ENGS = ("pe", "dve", "act", "pool", "sp")


class Op:
    __slots__ = ("eng", "fn", "reads", "writes", "is_dma", "deps", "signal", "sem", "semval",
                 "idx", "is_out", "prev_on_sem", "bar", "is_cc")

    def __init__(self, eng, fn, reads, writes, is_dma, is_out):
        self.eng = eng
        self.fn = fn
        self.reads = reads
        self.writes = writes
        self.is_dma = is_dma
        self.deps = []
        self.signal = False
        self.sem = None
        self.semval = 0
        self.is_out = is_out
        self.prev_on_sem = 0
        self.is_cc = False


class Prog:
    def __init__(self, nc, n_dma_sems=20):
        self.nc = nc
        self.ops = []
        self.state = {}
        self.n_dma_sems = n_dma_sems

    def op(self, eng, fn, reads=(), writes=(), dma=False, out=False):
        o = Op(eng, fn, tuple(reads), tuple(writes), dma, out)
        o.idx = len(self.ops)
        deps = set()
        for k in o.reads:
            st = self.state.get(k)
            if st is not None and st[0] is not None:
                deps.add(st[0])
        for k in o.writes:
            st = self.state.get(k)
            if st is not None:
                if st[0] is not None:
                    deps.add(st[0])
                for r in st[1]:
                    deps.add(r)
        for k in o.reads:
            if isinstance(k, str) and k.startswith("c:"):
                continue
            st = self.state.setdefault(k, [None, []])
            st[1].append(o)
        for k in o.writes:
            self.state[k] = [o, []]
        deps.discard(o)
        o.deps = sorted(deps, key=lambda d: d.idx)
        self.ops.append(o)
        return o

    def dma(self, eng, out_ap, in_ap, reads=(), writes=(), out=False):
        return self.op(eng, lambda e: e.dma_start(out=out_ap, in_=in_ap), reads, writes, dma=True, out=out)

    def cc(self, fn, reads=(), writes=()):
        o = self.op("pool", fn, reads, writes, dma=True)
        o.is_cc = True
        return o

    def barrier(self):
        self.ops.append(None)

    def emit(self):
        nc = self.nc
        ops = self.ops
        real = [o for o in ops if o is not None]
        for o in real:
            for d in o.deps:
                if d.is_dma:
                    continue
                if d.eng == o.eng and d.eng == "pe" and not o.is_dma:
                    continue
                d.signal = True
        last = {}
        for o in ops:
            if o is None:
                for e, lo in last.items():
                    lo.signal = True
            elif not o.is_dma:
                last[o.eng] = o
        esem = {e: nc.alloc_semaphore(f"s_{e}") for e in ENGS}
        dq = ("sp", "act", "pool")
        dsem = {e: [nc.alloc_semaphore(f"d_{e}_{i}") for i in range(self.n_dma_sems)] for e in dq}
        dcount = {e: [0] * self.n_dma_sems for e in dq}
        rr = {e: 0 for e in dq}
        tick = {e: 0 for e in ENGS}
        bars = []
        ccs = []
        per = {e: [] for e in ENGS}
        nbar = 0
        for o in ops:
            if o is None:
                w = {}
                for e in ENGS:
                    if tick[e] > 0:
                        w[esem[e]] = tick[e]
                for e in dq:
                    for i in range(self.n_dma_sems):
                        if dcount[e][i] > 0:
                            w[dsem[e][i]] = dcount[e][i] * 16
                for co in ccs:
                    w[co.sem] = 1
                bars.append(w)
                nbar += 1
                continue
            e = o.eng
            o.bar = nbar
            per[e].append(o)
            if o.is_cc:
                o.sem = nc.alloc_semaphore(f"cc_{o.idx}")
                o.semval = 1
                ccs.append(o)
            elif o.is_dma:
                k = rr[e]
                rr[e] = (k + 1) % self.n_dma_sems
                o.prev_on_sem = dcount[e][k] * 16
                dcount[e][k] += 1
                o.sem = dsem[e][k]
                o.semval = dcount[e][k] * 16
            elif o.signal:
                tick[e] += 1
                o.sem = esem[e]
                o.semval = tick[e]
        out_waits = {}
        for o in real:
            if o.is_dma and o.is_out:
                out_waits[o.sem] = max(out_waits.get(o.sem, 0), o.semval)
        self.stats = {e: len(per[e]) for e in ENGS}

        def run(e, engobj):
            seen = {}
            curbar = 0
            for o in per[e]:
                waits = {}
                if o.bar > curbar:
                    curbar = o.bar
                    waits.update(bars[curbar - 1])
                for d in o.deps:
                    if (not d.is_dma) and d.eng == e and e == "pe" and not o.is_dma:
                        continue
                    if d.sem is None:
                        continue
                    if waits.get(d.sem, 0) < d.semval:
                        waits[d.sem] = d.semval
                if o.is_dma and o.prev_on_sem > 0 and waits.get(o.sem, 0) < o.prev_on_sem:
                    waits[o.sem] = o.prev_on_sem
                for s, v in waits.items():
                    if seen.get(s, 0) >= v:
                        continue
                    seen[s] = v
                    engobj.wait_ge(s, v)
                ins = o.fn(engobj)
                if o.is_cc:
                    ins.then_inc(o.sem)
                elif o.is_dma:
                    ins.then_inc(o.sem, 16)
                elif o.signal:
                    ins.then_inc(o.sem, 1)
            if e == "sp":
                for s, v in out_waits.items():
                    engobj.wait_ge(s, v)

        with nc.Block() as block:
            @block.tensor
            def _(t):
                run("pe", t)

            @block.vector
            def _(v):
                run("dve", v)

            @block.scalar
            def _(s):
                run("act", s)

            @block.gpsimd
            def _(g):
                run("pool", g)

            @block.sync
            def _(s):
                run("sp", s)


class SbufAlloc:
    def __init__(self, nc, base=16512, limit=229312, prog=None):
        self.nc = nc
        self.prog = prog
        self.off = base
        self.limit = limit
        self.n = 0

    def mark(self):
        return self.off

    def release(self, m):
        self.off = m
        if self.prog is not None:
            self.prog.barrier()

    def alloc(self, shape, dtype, name="t"):
        esz = 4 if dtype == F32 else 2
        nbytes = int(np.prod(shape[1:])) * esz
        nbytes = (nbytes + 63) // 64 * 64
        assert self.off + nbytes <= self.limit, f"SBUF overflow {name} {self.off}+{nbytes}"
        self.n += 1
        t = self.nc.alloc_sbuf_tensor_at(f"{name}_{self.n}", list(shape), dtype, offset=self.off)
        self.off += nbytes
        return t


class Builder:
    def __init__(self, nc):
        self.nc = nc
        self.P = Prog(nc)
        self.sb = SbufAlloc(nc, prog=self.P)
        self.psum = nc.alloc_psum_tensor("psum_all", [128, 4096], F32)
        self.banks = [self.psum[:, i * 512:(i + 1) * 512] for i in range(8)]
        self.uid = 0
        self.dram = {}

    def din(self, name, shape, dtype=F32):
        if name in self.dram:
            return self.dram[name]
        t = self.nc.dram_tensor(name, list(shape), dtype, kind="ExternalInput").ap()
        self.dram[name] = t
        return t

    def dout(self, name, shape, dtype=F32):
        t = self.nc.dram_tensor(name, list(shape), dtype, kind="ExternalOutput").ap()
        self.dram[name] = t
        return t

    def dint(self, name, shape, dtype=F32):
        t = self.nc.dram_tensor(name, list(shape), dtype).ap()
        self.dram[name] = t
        return t

    def mm(self, out, lhsT, rhs, start, stop, reads, writes):
        self.P.op("pe", lambda e: e.matmul(out, lhsT, rhs, start=start, stop=stop), reads, writes)

    def act(self, out, in_, func, reads, writes, bias=0.0, scale=1.0):
        self.P.op("act", lambda e: e.activation(out=out, in_=in_, func=func, bias=bias, scale=scale),
                  reads, writes)

    def tt(self, eng, out, in0, in1, op, reads, writes):
        self.P.op(eng, lambda e: e.tensor_tensor(out=out, in0=in0, in1=in1, op=op), reads, writes)

    def ts(self, eng, out, in0, s1, s2, op0, op1, reads, writes):
        if s2 is None:
            self.P.op(eng, lambda e: e.tensor_single_scalar(out=out, in_=in0, scalar=s1, op=op0), reads, writes)
        else:
            self.P.op(eng, lambda e: e.tensor_scalar(out=out, in0=in0, scalar1=s1, scalar2=s2, op0=op0, op1=op1),
                      reads, writes)

    def stt(self, eng, out, in0, scalar, in1, op0, op1, reads, writes):
        self.P.op(eng, lambda e: e.scalar_tensor_tensor(out=out, in0=in0, scalar=scalar, in1=in1, op0=op0, op1=op1),
                  reads, writes)

    def copy(self, eng, out, in_, reads, writes):
        self.P.op(eng, lambda e: e.tensor_copy(out=out, in_=in_), reads, writes)

    def load_consts(self):
        sb, P = self.sb, self.P
        C = {}
        specs = [("ident", [128, 128], F32), ("onesf", [128, 128], F32), ("onesb", [128, 128], BF16),
                 ("negtri", [128, 128], BF16), ("negones", [128, 128], BF16),
                 ("identb", [128, 128], BF16),
                 ("flagcol", [128, 1], F32), ("cT", [128, 8], F32), ("zcol", [128, 1], F32)]
        for name, shape, dt in specs:
            d = self.din("k_" + name, shape, dt)
            t = sb.alloc(shape, dt, name)
            P.dma("sp", t[:], d, writes=["c:" + name])
            C[name] = t
        self.C = C
        sig = sb.alloc([128, 8], F32, "csig")
        cact = sb.alloc([128, 8], F32, "cact")
        self.act(sig[:], C["cT"][:], AF.Sigmoid, ["c:cT"], ["csig"])
        self.tt("dve", cact[:], C["cT"][:], sig[:], ALU.mult, ["c:cT", "csig"], ["c:cact"])
        C["cact"] = cact

    def mod_cols(self, w_ap, bT_ap, ncols, tag):
        sb, P, C = self.sb, self.P, self.C
        nch = ncols // 128
        out = sb.alloc([128, nch], F32, "mod" + tag)
        bt = sb.alloc([128, nch], F32, "modb" + tag)
        P.dma("sp", bt[:], bT_ap, writes=["modb" + tag])
        m = sb.mark()
        slabw = 768 if ncols % 768 == 0 else 512
        nslab = ncols // slabw
        slabs = [sb.alloc([128, 8, slabw], F32, "slab") for _ in range(2)]
        wv = w_ap.rearrange("(kc p) n -> p kc n", p=128)
        ps = self.banks[7]
        for s in range(nslab):
            sl = slabs[s % 2]
            key = f"slab{s % 2}"
            P.dma("sp", sl[:], wv[:, :, s * slabw:(s + 1) * slabw], writes=[key])
            for n in range(slabw // 128):
                nn = s * (slabw // 128) + n
                for kc in range(8):
                    self.mm(ps[:, nn:nn + 1], sl[:, kc, n * 128:(n + 1) * 128], C["cact"][:, kc:kc + 1],
                            kc == 0, kc == 7, [key, "c:cact"], ["pb7"])
        self.tt("dve", out[:], ps[:, 0:nch], bt[:], ALU.add, ["pb7", "modb" + tag], ["mod" + tag])
        sb.release(m)
        return out, "mod" + tag

    def layer_mods(self, l):
        sb, P = self.sb, self.P
        ada_w = self.din(f"ada_w{l}", [D, 6 * D])
        ada_bT = self.din(f"ada_bT{l}", [128, 48])
        nmT = self.din(f"nmT{l}", [128, 16])
        mod, mk = self.mod_cols(ada_w, ada_bT, 6 * D, f"L{l}")
        nm = sb.alloc([128, 16], F32, "nm")
        P.dma("sp", nm[:], nmT, writes=[f"nm{l}"])
        A = sb.alloc([128, 16], F32, "Acols")
        self.stt("dve", A[:, 0:8], mod[:, 8:16], 1.0, nm[:, 0:8], ALU.add, ALU.mult, [mk, f"nm{l}"], [f"A1_{l}"])
        self.stt("dve", A[:, 8:16], mod[:, 32:40], 1.0, nm[:, 8:16], ALU.add, ALU.mult, [mk, f"nm{l}"], [f"A2_{l}"])
        return dict(A1=A[:, 0:8], B1=mod[:, 0:8], G1=mod[:, 16:24], A2=A[:, 8:16], B2=mod[:, 24:32],
                    G2=mod[:, 40:48], kA1=f"A1_{l}", kA2=f"A2_{l}", kmod=mk)

    def rstd_of(self, x, xkeys, n, sq, rstd, tag, bank, dim=1024.0, nk=8):
        C = self.C
        ps = self.banks[bank]
        for kc in range(nk):
            q = sq[kc % 2]
            self.act(q[:, 0:n], x[:, kc, :], AF.Square, xkeys, [f"sq{tag}{kc % 2}"])
            self.mm(ps[:, 0:n], C["onesb"][:], q[:, 0:n], kc == 0, kc == nk - 1,
                    [f"sq{tag}{kc % 2}", "c:onesb"], [f"pb{bank}"])
        self.ts("dve", rstd, ps[:, 0:n], 1.0 / dim, EPS, ALU.mult, ALU.add, [f"pb{bank}"], ["rstd" + tag])
        self.act(rstd, rstd, AF.Ln, ["rstd" + tag], ["rstd" + tag])
        self.act(rstd, rstd, AF.Exp, ["rstd" + tag], ["rstd" + tag], scale=-0.5)

    def phase_proj(self, l, XT, mods, outs):
        sb, P, C = self.sb, self.P, self.C
        m0 = sb.mark()
        isA = l < NA
        wts = {}
        if isA:
            w = self.din(f"wqkv{l}", [D, 3 * D]).rearrange("(kc p) n -> p kc n", p=128)
            for i, nm in enumerate(("Q", "K", "V")):
                t = sb.alloc([128, 8, D], BF16, "w" + nm)
                P.dma("pool", t[:], w[:, :, i * D:(i + 1) * D], writes=["w" + nm])
                wts[nm] = t
        else:
            w = self.din(f"wq{l}", [D, D]).rearrange("(kc p) n -> p kc n", p=128)
            t = sb.alloc([128, 8, D], BF16, "wQ")
            P.dma("pool", t[:], w, writes=["wQ"])
            wts["Q"] = t
            if "KB" in outs:
                w = self.din("kvw", [D, 2 * D]).rearrange("(kc p) n -> p kc n", p=128)
                for i, nm in enumerate(("KB", "VB")):
                    t = sb.alloc([128, 8, D], BF16, "w" + nm)
                    P.dma("pool", t[:], w[:, :, i * D:(i + 1) * D], writes=["w" + nm])
                    wts[nm] = t
                kvmod, kvk = self.mod_cols(self.din("kv_ada_w", [D, 2 * D]), self.din("kv_ada_bT", [128, 16]),
                                           2 * D, "KV")
                kvn = sb.alloc([128, 8], F32, "kvn")
                P.dma("sp", kvn[:], self.din("kvnT", [128, 8]), writes=["kvn"])
                Akv = sb.alloc([128, 8], F32, "Akv")
                self.stt("dve", Akv[:], kvmod[:, 8:16], 1.0, kvn[:], ALU.add, ALU.mult, [kvk, "kvn"], ["Akv"])
        XTv = XT.rearrange("(kc p) t -> p kc t", p=128)
        xs = [sb.alloc([128, 8, 512], F32, "x") for _ in range(2)]
        sqs = [sb.alloc([128, 512], BF16, "sq") for _ in range(2)]
        rstd = sb.alloc([128, 512], F32, "rstd")
        tmp = [sb.alloc([128, 512], F32, "tmp") for _ in range(2)]
        hT = [sb.alloc([128, 8, 512], BF16, "hT") for _ in range(2)]
        hK = sb.alloc([128, 8, 512], BF16, "hK") if "KB" in outs else None
        ev = [sb.alloc([128, 512], BF16, "ev") for _ in range(4)]
        vst = [sb.alloc([128, 4, D], BF16, "vst") for _ in range(2)]
        evi = 0
        pbi = 0
        for g in range(NG):
            x = xs[g % 2]
            xk = f"x{g % 2}"
            P.dma("sp", x[:], XTv[:, :, g * 512:(g + 1) * 512], reads=[("XT", g)], writes=[xk])
            self.rstd_of(x, [xk], 512, sqs, rstd[:], "p", 6)
            h = hT[g % 2]
            hk = f"hT{g % 2}"
            for kc in range(8):
                tm = tmp[kc % 2]
                self.stt("dve", tm[:], x[:, kc, :], mods["A1"][:, kc:kc + 1], rstd[:], ALU.mult, ALU.mult,
                         [xk, mods["kA1"], "rstdp"], [f"tmp{kc % 2}"])
                self.act(h[:, kc, :], tm[:], AF.Identity, [f"tmp{kc % 2}", mods["kmod"]], [hk],
                         bias=mods["B1"][:, kc:kc + 1])
            if hK is not None:
                for kc in range(8):
                    tm = tmp[kc % 2]
                    self.stt("dve", tm[:], x[:, kc, :], Akv[:, kc:kc + 1], rstd[:], ALU.mult, ALU.mult,
                             [xk, "Akv", "rstdp"], [f"tmp{kc % 2}"])
                    self.act(hK[:, kc, :], tm[:], AF.Identity, [f"tmp{kc % 2}", kvk], ["hK"],
                             bias=kvmod[:, kc:kc + 1])
            if getattr(self, "debug", False) and g == 0:
                P.dma("sp", self.dout("d_rstd", [128, 512]), rstd[:], reads=["rstdp"], out=True)
                P.dma("sp", self.dout("d_hT", [128, 8, 512], BF16), h[:], reads=[hk], out=True)
                P.dma("sp", self.dout("d_x", [128, 8, 512]), x[:], reads=[xk], out=True)
            for nm, src, srck, scale in (("Q", h, hk, 0.125), ("K", h, hk, 1.0), ("KB", hK, "hK", 1.0)):
                if nm not in outs or nm not in wts:
                    continue
                for j in range(8):
                    pb = pbi % 4
                    pbi += 1
                    ps = self.banks[pb]
                    for kc in range(8):
                        self.mm(ps[:], wts[nm][:, kc, j * 128:(j + 1) * 128], src[:, kc, :], kc == 0, kc == 7,
                                ["w" + nm, srck], [f"pb{pb}"])
                    e = ev[evi % 4]
                    ek = f"ev{evi % 4}"
                    evi += 1
                    if j % 2 == 0:
                        self.act(e[:], ps[:], AF.Identity, [f"pb{pb}"], [ek], scale=scale)
                    else:
                        self.ts("dve", e[:], ps[:], scale, None, ALU.mult, None, [f"pb{pb}"], [ek])
                    P.dma("sp", outs[nm][j * 128:(j + 1) * 128, g * 512:(g + 1) * 512], e[:], reads=[ek],
                          writes=[(nm, j, g)])
            for nm, src, srck in (("V", h, hk), ("VB", hK, "hK")):
                if nm not in outs or nm not in wts:
                    continue
                vs = vst[g % 2]
                vk = f"vst{g % 2}"
                for tt_ in range(4):
                    for half in range(2):
                        pb = pbi % 4
                        pbi += 1
                        ps = self.banks[pb]
                        for kc in range(8):
                            self.mm(ps[:], src[:, kc, tt_ * 128:(tt_ + 1) * 128],
                                    wts[nm][:, kc, half * 512:(half + 1) * 512], kc == 0, kc == 7,
                                    ["w" + nm, srck], [f"pb{pb}"])
                        if half == 0:
                            self.act(vs[:, tt_, 0:512], ps[:], AF.Identity, [f"pb{pb}"], [vk])
                        else:
                            self.copy("dve", vs[:, tt_, 512:1024], ps[:], [f"pb{pb}"], [vk])
                P.dma("sp", outs[nm].rearrange("(n p) c -> p n c", p=128)[:, g * 4:(g + 1) * 4, :], vs[:],
                      reads=[vk], writes=[(nm, g)])
        sb.release(m0)

    def phase_att_a(self, l, QT, KTo, KTp, Vo, Vp, OT, kprev_ready, qaugD, kaugD):
        sb, P, C = self.sb, self.P, self.C
        m0 = sb.mark()
        mt = sb.alloc([128, 4 * 512], BF16, "maskA")
        P.dma("sp", mt[:], self.din("k_maskA", [128, 4 * 512], BF16), writes=["c:maskA"])
        C["maskA"] = mt
        lam_init = 0.8 - 0.6 * math.exp(-0.3 * l)
        lamr = sb.alloc([128, 256], F32, "lamr")
        P.dma("sp", lamr[:], self.din(f"lamrep{l}", [128, 256]), writes=["lamr"])
        lprod = sb.alloc([128, 2, 64], F32, "lprod")
        lsum = sb.alloc([128, 2], F32, "lsum")
        lexp = sb.alloc([128, 2], F32, "lexp")
        neglam = sb.alloc([128, 1], F32, "neglam")
        self.tt("dve", lprod[:, 0, :], lamr[:, 0:64], lamr[:, 64:128], ALU.mult, ["lamr"], ["lprod"])
        self.tt("dve", lprod[:, 1, :], lamr[:, 128:192], lamr[:, 192:256], ALU.mult, ["lamr"], ["lprod"])
        P.op("dve", lambda e: e.reduce_sum(out=lsum[:], in_=lprod[:], axis=mybir.AxisListType.X), ["lprod"], ["lsum"])
        self.act(lexp[:], lsum[:], AF.Exp, ["lsum"], ["lexp"])
        self.tt("dve", neglam[:], lexp[:, 1:2], lexp[:, 0:1], ALU.subtract, ["lexp"], ["neglam"])
        self.ts("dve", neglam[:], neglam[:], -lam_init, None, ALU.add, None, ["neglam"], ["neglam"])
        subc = sb.alloc([128, 1], F32, "subc")
        P.dma("sp", subc[:], self.din(f"sublnT{l}", [128, 1]), writes=["subc"])
        self.ts("dve", subc[:], subc[:], 1.0 - lam_init, None, ALU.mult, None, ["subc"], ["subc"])

        kaug = [[sb.alloc([68, 2 * T], BF16, "kaug") for r in range(2)] for _ in range(2)]
        qaug = [[sb.alloc([68, T], BF16, "qaug") for r in range(2)] for _ in range(2)]
        vh = [sb.alloc([128, 64, 128], BF16, "vh") for _ in range(2)]
        pw = [sb.alloc([128, 1024], BF16, "pw") for _ in range(3)]
        pacc = sb.alloc([128, 1024], F32, "pacc")
        rl = sb.alloc([128, 1024], F32, "rl")
        o0 = sb.alloc([128, 512], F32, "o0")
        o1 = sb.alloc([128, 512], F32, "o1")
        osq = sb.alloc([128, 512], F32, "osq")
        orstd = sb.alloc([128, 512], F32, "orstd")
        onb = [sb.alloc([128, 512], BF16, "onb") for _ in range(2)]
        Vov = Vo.rearrange("(n p) c -> p n c", p=128)
        Vpv = Vp.rearrange("(n p) c -> p n c", p=128)
        Sw = [self.psum[:, 0:1024], self.psum[:, 1024:2048]]
        Lw = self.psum[:, 3072:4096]
        ocount = 0
        scount = 0
        pcount = 0
        for h in range(8):
            hb = h % 2
            for r in range(2):
                row0 = h * 128 + r * 64
                ka, qa = kaug[hb][r], qaug[hb][r]
                P.dma("sp", ka[0:64, 0:T], KTp[row0:row0 + 64, :], reads=kprev_ready, writes=[f"ka{hb}{r}"])
                P.dma("sp", ka[0:64, T:2 * T], KTo[row0:row0 + 64, :],
                      reads=[("K", h, g) for g in range(NG)], writes=[f"ka{hb}{r}"])
                P.dma("sp", ka[64:68, :], kaugD[h], writes=[f"ka{hb}{r}"])
                P.dma("sp", qa[0:64, :], QT[row0:row0 + 64, :], reads=[("Q", h, g) for g in range(NG)],
                      writes=[f"qa{hb}{r}"])
                P.dma("sp", qa[64:68, :], qaugD[h], writes=[f"qa{hb}{r}"])
            v = vh[hb]
            P.dma("sp", v[:, 0:32, :], Vpv[:, :, h * 128:(h + 1) * 128], reads=kprev_ready, writes=[f"vh{hb}"])
            P.dma("sp", v[:, 32:64, :], Vov[:, :, h * 128:(h + 1) * 128], reads=[("V", g) for g in range(NG)],
                  writes=[f"vh{hb}"])
            for g in range(NG):
                blocks = [(kb, None, True) for kb in range(32)]
                blocks += [(32 + j, None, False) for j in range(4 * g)]
                blocks += [(32 + 4 * g + d, d, False) for d in range(4)]
                nb = len(blocks)
                state = {}

                def s_stage(bi):
                    nonlocal scount, pcount
                    kb, d, isprev = blocks[bi]
                    si = scount % 2
                    scount += 1
                    S = Sw[si]
                    for r in range(2):
                        ka, qa = kaug[hb][r], qaug[hb][r]
                        o_ = S[:, r * 512:(r + 1) * 512]
                        self.mm(o_, ka[:, kb * 128:(kb + 1) * 128], qa[:, g * 512:(g + 1) * 512], True, d is None,
                                [f"ka{hb}{r}", f"qa{hb}{r}"], [f"Sw{si}"])
                        if d is not None:
                            self.mm(o_, C["identb"][:], C["maskA"][:, d * 512:(d + 1) * 512], False, True,
                                    ["c:identb", "c:maskA"], [f"Sw{si}"])
                    pi = pcount % 3
                    pcount += 1
                    self.act(pw[pi][:], S, AF.Exp, [f"Sw{si}", "c:flagcol"], [f"pw{pi}"],
                             bias=(C["flagcol"][:, 0:1] if isprev else C["zcol"][:, 0:1]))
                    state[bi] = pi

                def pv_stage(bi):
                    kb, d, isprev = blocks[bi]
                    pi = state.pop(bi)
                    for r in range(2):
                        self.mm(self.banks[4 + r], v[:, kb, :], pw[pi][:, r * 512:(r + 1) * 512], bi == 0, bi == nb - 1,
                                [f"vh{hb}", f"pw{pi}"], [f"pb{4 + r}"])
                    if bi == 0:
                        self.copy("dve", pacc[:], pw[pi][:], [f"pw{pi}"], ["pacc"])
                    else:
                        self.tt("dve", pacc[:], pacc[:], pw[pi][:], ALU.add, ["pacc", f"pw{pi}"], ["pacc"])

                s_stage(0)
                for bi in range(nb):
                    if bi + 1 < nb:
                        s_stage(bi + 1)
                    pv_stage(bi)
                for r in range(2):
                    self.mm(Lw[:, r * 512:(r + 1) * 512], C["onesf"][:], pacc[:, r * 512:(r + 1) * 512], True, True,
                            ["c:onesf", "pacc"], ["Lw"])
                P.op("dve", lambda e: e.reciprocal(out=rl[:], in_=Lw), ["Lw"], ["rl"])
                self.tt("dve", o0[:], self.banks[4], rl[:, 0:512], ALU.mult, ["pb4", "rl"], ["o0"])
                self.tt("dve", o1[:], self.banks[5], rl[:, 512:1024], ALU.mult, ["pb5", "rl"], ["o1"])
                self.stt("dve", o0[:], o1[:], neglam[:, 0:1], o0[:], ALU.mult, ALU.add, ["o1", "neglam", "o0"], ["o0"])
                self.tt("dve", osq[:], o0[:], o0[:], ALU.mult, ["o0"], ["osq"])
                ps = self.banks[6]
                self.mm(ps, C["onesf"][:], osq[:], True, True, ["c:onesf", "osq"], ["Lw"])
                self.ts("dve", orstd[:], ps, 1.0 / 128.0, EPS, ALU.mult, ALU.add, ["Lw"], ["orstd"])
                self.act(orstd[:], orstd[:], AF.Ln, ["orstd"], ["orstd"])
                self.act(orstd[:], orstd[:], AF.Exp, ["orstd"], ["orstd"], scale=-0.5)
                ob_ = onb[ocount % 2]
                obk = f"onb{ocount % 2}"
                ocount += 1
                self.stt("dve", ob_[:], o0[:], subc[:, 0:1], orstd[:], ALU.mult, ALU.mult, ["o0", "subc", "orstd"], [obk])
                P.dma("sp", OT[h * 128:(h + 1) * 128, g * 512:(g + 1) * 512], ob_[:], reads=[obk], writes=[("O", h, g)])
        sb.release(m0)

    def phase_att_b(self, l, QT, KTo, KTp, Vo, Vp, OT, kprev_ready, kown_keys, vown_keys):
        sb, P, C = self.sb, self.P, self.C
        m0 = sb.mark()
        mt = sb.alloc([128, 4 * 512], BF16, "maskB")
        P.dma("sp", mt[:], self.din("k_maskB", [128, 4 * 512], BF16), writes=["c:maskB"])
        C["maskB"] = mt
        kt = [sb.alloc([128, 2 * T], BF16, "ktb") for _ in range(2)]
        qt = [sb.alloc([128, T], BF16, "qtb") for _ in range(2)]
        vh = [sb.alloc([128, 64, 128], BF16, "vhb") for _ in range(2)]
        ebw = sb.alloc([128, 1024], F32, "ebw")
        spw = [sb.alloc([128, 1024], BF16, "spw") for _ in range(2)]
        aw = [sb.alloc([128, 1024], BF16, "aw") for _ in range(2)]
        ssum = sb.alloc([128, 1024], F32, "ssum")
        ssbw = [sb.alloc([128, 1024], BF16, "ssbw") for _ in range(2)]
        ost = [sb.alloc([128, 512], BF16, "ost") for _ in range(2)]
        Vov = Vo.rearrange("(n p) c -> p n c", p=128)
        Vpv = Vp.rearrange("(n p) c -> p n c", p=128)
        Zw = self.psum[:, 0:1024]
        Rw = self.psum[:, 1024:2048]
        for hp in range(8):
            hb = hp % 2
            k_, q_, v = kt[hb], qt[hb], vh[hb]
            kk, qk, vk = f"ktb{hb}", f"qtb{hb}", f"vhb{hb}"
            P.dma("sp", k_[:, 0:T], KTp[hp * 128:(hp + 1) * 128, :], reads=kprev_ready, writes=[kk])
            P.dma("sp", k_[:, T:2 * T], KTo[hp * 128:(hp + 1) * 128, :], reads=[kk_ for kk_ in kown_keys(hp)],
                  writes=[kk])
            P.dma("sp", q_[:], QT[hp * 128:(hp + 1) * 128, :], reads=[("Q", hp, g) for g in range(NG)], writes=[qk])
            P.dma("sp", v[:, 0:32, :], Vpv[:, :, hp * 128:(hp + 1) * 128], reads=kprev_ready, writes=[vk])
            P.dma("sp", v[:, 32:64, :], Vov[:, :, hp * 128:(hp + 1) * 128], reads=vown_keys, writes=[vk])
            for g in range(NG):
                os_ = ost[(hp * NG + g) % 2]
                osk = f"ost{(hp * NG + g) % 2}"
                blocks = [(32 + 4 * g + d, d, False) for d in (3, 2, 1, 0)]
                blocks += [(32 + j, None, False) for j in range(4 * g - 1, -1, -1)]
                blocks += [(kb, None, True) for kb in range(31, -1, -1)]
                nb = len(blocks)

                def ops_of(bi):
                    kb, d, isprev = blocks[bi]
                    bias = C["flagcol"][:, 0:1] if isprev else C["zcol"][:, 0:1]
                    mk = C["maskB"][:, d * 512:(d + 1) * 512] if d is not None else None
                    return kb, d, bias, mk, bi % 2

                def qk_into(dst, dkey, hh, kb, d, mk, last):
                    r0 = hh * 64
                    kblk = k_[r0:r0 + 64, kb * 128:(kb + 1) * 128]
                    qblk = q_[r0:r0 + 64, g * 512:(g + 1) * 512]
                    o_ = dst[:, hh * 512:(hh + 1) * 512]
                    self.mm(o_, kblk, qblk, True, last and d is None, [kk, qk], [dkey])
                    if d is not None:
                        self.mm(o_, C["identb"][:], mk, False, last, ["c:identb", "c:maskB"], [dkey])

                def stage1(bi):
                    kb, d, bias, mk, par = ops_of(bi)
                    for hh in range(2):
                        qk_into(Zw, "Zw", hh, kb, d, mk, True)
                    self.act(ebw[:], Zw, AF.Exp, ["Zw", "c:flagcol"], ["ebw"], bias=bias)
                    self.act(spw[par][:], ebw[:], AF.Ln, ["ebw"], [f"spw{par}"], bias=1.0)

                def stage2(bi):
                    kb, d, bias, mk, par = ops_of(bi)
                    for hh in range(2):
                        hs = slice(hh * 512, (hh + 1) * 512)
                        qk_into(Rw, "Rw", hh, kb, d, mk, False)
                        if bi > 0:
                            self.mm(Rw[:, hs], C["negones"][:], ssbw[(bi - 1) % 2][:, hs], False, False,
                                    ["c:negones", f"ssbw{(bi - 1) % 2}"], ["Rw"])
                        self.mm(Rw[:, hs], C["negtri"][:], spw[par][:, hs], False, True, ["c:negtri", f"spw{par}"], ["Rw"])
                    self.act(aw[par][:], Rw, AF.Exp, ["Rw", "c:flagcol"], [f"aw{par}"], bias=bias)
                    if bi < nb - 1:
                        if bi == 0:
                            self.copy("dve", ssum[:], spw[par][:], [f"spw{par}"], ["ssum"])
                        else:
                            self.tt("dve", ssum[:], ssum[:], spw[par][:], ALU.add, ["ssum", f"spw{par}"], ["ssum"])
                        self.copy("dve", ssbw[par][:], ssum[:], ["ssum"], [f"ssbw{par}"])

                def stage3(bi):
                    kb, d, bias, mk, par = ops_of(bi)
                    for hh in range(2):
                        ob = 4 + hh
                        self.mm(self.banks[ob][0:64, :], v[:, kb, hh * 64:(hh + 1) * 64], aw[par][:, hh * 512:(hh + 1) * 512],
                                bi == 0, bi == nb - 1, [vk, f"aw{par}"], [f"pb{ob}"])

                stage1(0)
                for bi in range(nb):
                    if bi + 1 < nb:
                        stage1(bi + 1)
                    stage2(bi)
                    if bi >= 1:
                        stage3(bi - 1)
                stage3(nb - 1)
                for hh in range(2):
                    ob = 4 + hh
                    self.copy("dve", os_[hh * 64:(hh + 1) * 64, :], self.banks[ob][0:64, :], [f"pb{ob}"], [osk])
                P.dma("sp", OT[hp * 128:(hp + 1) * 128, g * 512:(g + 1) * 512], os_[:], reads=[osk],
                      writes=[("O", hp, g)])
        sb.release(m0)

    def phase_res_moe(self, l, XT, OT, mods, XOUT, final=False):
        sb, P, C = self.sb, self.P, self.C
        m0 = sb.mark()
        NTG = 1024
        NTT = NTG // 128
        wo = sb.alloc([128, 8, D], BF16, "wo")
        P.dma("pool", wo[:], self.din(f"wo{l}", [D, D]).rearrange("(kc p) n -> p kc n", p=128), writes=["wo"])
        wr = sb.alloc([128, 8, E], F32, "wr")
        P.dma("sp", wr[:], self.din(f"rw{l}", [D, E]).rearrange("(kc p) n -> p kc n", p=128), writes=["wr"])
        rbrow = sb.alloc([1, E], F32, "rbrow")
        P.dma("sp", rbrow[:], self.din(f"rb{l}", [1, E]), writes=["rbrow"])
        bd = sb.alloc([E, D], F32, "bd")
        P.dma("sp", bd[:], self.din(f"bd{l}", [E, D]), writes=["bd"])
        bgu = sb.alloc([128, E, 16], F32, "bgu")
        P.dma("sp", bgu[:], self.din(f"bguT{l}", [128, E, 16]), writes=["bgu"])
        self.ts("dve", bgu[:, :, 8:16], bgu[:, :, 8:16], 1.0, None, ALU.add, None, ["bgu"], ["bgu"])
        wguD = self.din(f"wgu{l}", [E, 8, 128, 8 * 256])
        wdD = self.din(f"wd{l}", [E, D, D])
        if final:
            fnc = sb.alloc([128, 8], F32, "fnc")
            P.dma("sp", fnc[:], self.din("fnT", [128, 8]), writes=["fnc"])
        XTv = XT.rearrange("(kc p) t -> p kc t", p=128)
        OTv = OT.rearrange("(kc p) t -> p kc t", p=128)
        XOv = XOUT.rearrange("(kc p) t -> p kc t", p=128)
        x = sb.alloc([128, 8, NTG], F32, "xg")
        hT = sb.alloc([128, 8, NTG], BF16, "hTg")
        h32 = sb.alloc([128, 8, NTG], F32, "h32acc")
        acc = h32
        actT = sb.alloc([128, 8, NTG], BF16, "actT")
        oT = actT
        sq = [sb.alloc([128, 512], BF16, "sqm") for _ in range(2)]
        rstd = sb.alloc([128, 512], F32, "rstdm")
        tmp = [sb.alloc([128, 512], F32, "tmpm") for _ in range(2)]
        lg = sb.alloc([128, E], F32, "lg")
        m8 = sb.alloc([128, 8], F32, "m8")
        negm = sb.alloc([128, 1], F32, "negm")
        msk = sb.alloc([128, E], F32, "msk")
        ee = sb.alloc([128, E], F32, "ee")
        ssum = sb.alloc([128, 1], F32, "gsum")
        gts = sb.alloc([128, NTT, E], F32, "gts")
        gT = sb.alloc([E, NTG], F32, "gT")
        wgu = [sb.alloc([128, 8, 256], BF16, "wgu") for _ in range(4)]
        wdn = [sb.alloc([128, 8, 512], BF16, "wdn") for _ in range(3)]
        gc = [sb.alloc([128, 512], F32, "gc") for _ in range(2)]
        sg = [sb.alloc([128, 512], F32, "sg") for _ in range(2)]
        ub = [sb.alloc([128, 512], F32, "ub") for _ in range(2)]
        p1 = [sb.alloc([128, 512], F32, "p1") for _ in range(2)]
        wcnt = 0
        dcnt = 0
        ecnt = 0
        ycnt = 0
        for tg in range(T // NTG):
            gl = [2 * tg, 2 * tg + 1]
            tsl = slice(tg * NTG, (tg + 1) * NTG)
            P.dma("sp", x[:], XTv[:, :, tsl], reads=[("XT", g) for g in gl], writes=["xg"])
            P.dma("sp", oT[:], OTv[:, :, tsl], reads=[("O", h, g) for h in range(8) for g in gl], writes=["actT"])
            for s in range(2):
                ss = slice(s * 512, (s + 1) * 512)
                for j in range(8):
                    pb = (s * 8 + j) % 2
                    ps = self.banks[pb]
                    for kc in range(8):
                        self.mm(ps[:], wo[:, kc, j * 128:(j + 1) * 128], oT[:, kc, ss], kc == 0, kc == 7,
                                ["wo", "actT"], [f"pb{pb}"])
                    self.stt("dve", x[:, j, ss], ps[:], mods["G1"][:, j:j + 1], x[:, j, ss], ALU.mult, ALU.add,
                             [f"pb{pb}", mods["kmod"], "xg"], ["xg"])
            if getattr(self, "debug", False):
                if tg == 0:
                    self.dbg_x1 = self.dout("d_x1", [D, T])
                P.dma("sp", self.dbg_x1.rearrange("(kc p) t -> p kc t", p=128)[:, :, tsl], x[:], reads=["xg"], out=True)
            for s in range(2):
                ss = slice(s * 512, (s + 1) * 512)
                self.rstd_of(x[:, :, ss], ["xg"], 512, sq, rstd[:], "m", 2)
                for kc in range(8):
                    tm = tmp[kc % 2]
                    self.stt("dve", tm[:], x[:, kc, ss], mods["A2"][:, kc:kc + 1], rstd[:], ALU.mult, ALU.mult,
                             ["xg", mods["kA2"], "rstdm"], [f"tmpm{kc % 2}"])
                    self.act(h32[:, kc, ss], tm[:], AF.Identity, [f"tmpm{kc % 2}", mods["kmod"]], ["h32"],
                             bias=mods["B2"][:, kc:kc + 1])
                    self.copy("dve", hT[:, kc, ss], h32[:, kc, ss], ["h32"], ["hTg"])
            for tt_ in range(NTT):
                ts_ = slice(tt_ * 128, (tt_ + 1) * 128)
                ps = self.banks[3]
                for kc in range(8):
                    self.mm(ps[:, 0:E], h32[:, kc, ts_], wr[:, kc, :], kc == 0, False, ["h32", "wr"], ["pb3"])
                self.mm(ps[:, 0:E], C["onesf"][0:1, :], rbrow[:], False, True, ["c:onesf", "rbrow"], ["pb3"])
                self.copy("dve", lg[:], ps[:, 0:E], ["pb3"], ["lg"])
                P.op("dve", lambda e: e.max(out=m8[:], in_=lg[:]), ["lg"], ["m8"])
                self.ts("dve", negm[:], m8[:, 0:1], -1.0, None, ALU.mult, None, ["m8"], ["negm"])
                self.ts("dve", msk[:], lg[:], m8[:, 3:4], None, ALU.is_ge, None, ["lg", "m8"], ["msk"])
                self.act(ee[:], lg[:], AF.Exp, ["lg", "negm"], ["ee"], bias=negm[:, 0:1])
                self.tt("dve", ee[:], ee[:], msk[:], ALU.mult, ["ee", "msk"], ["ee"])
                P.op("dve", lambda e: e.reduce_sum(out=ssum[:], in_=ee[:], axis=mybir.AxisListType.X), ["ee"], ["gsum"])
                P.op("dve", lambda e: e.reciprocal(out=ssum[:], in_=ssum[:]), ["gsum"], ["gsum"])
                self.ts("dve", gts[:, tt_, :], ee[:], ssum[:, 0:1], None, ALU.mult, None, ["ee", "gsum"], ["gts"])
                ps2 = self.banks[4]
                self.mm(ps2[0:E, 0:128], gts[:, tt_, :], C["ident"][:], True, True, ["gts", "c:ident"], ["pb4"])
                self.copy("dve", gT[:, ts_], ps2[0:E, 0:128], ["pb4"], ["gT"])
            for tt_ in range(NTT):
                ts_ = slice(tt_ * 128, (tt_ + 1) * 128)
                for half in range(2):
                    pb = 4 + ycnt % 2
                    ycnt += 1
                    ps = self.banks[pb]
                    self.mm(ps[:], gT[:, ts_], bd[:, half * 512:(half + 1) * 512], True, True, ["gT", "bd"], [f"pb{pb}"])
                    self.copy("dve", acc[:, tt_, half * 512:(half + 1) * 512], ps[:], [f"pb{pb}"], ["h32"])
            for e in range(E):
                for j in range(8):
                    w = wgu[wcnt % 4]
                    wk = f"wgu{wcnt % 4}"
                    wcnt += 1
                    P.dma("pool", w[:], wguD[e, j].rearrange("p (kc c) -> p kc c", kc=8), writes=[wk])
                    for s in range(2):
                        ss = slice(s * 512, (s + 1) * 512)
                        i2 = ecnt % 2
                        ecnt += 1
                        gb_, ub_ = self.banks[i2], self.banks[2 + i2]
                        for kc in range(8):
                            self.mm(gb_[:], w[:, kc, 0:128], hT[:, kc, ss], kc == 0, kc == 7, [wk, "hTg"], [f"pb{i2}"])
                        for kc in range(8):
                            self.mm(ub_[:], w[:, kc, 128:256], hT[:, kc, ss], kc == 0, kc == 7, [wk, "hTg"],
                                    [f"pb{2 + i2}"])
                        self.ts("dve", gc[i2][:], gb_[:], bgu[:, e, j:j + 1], 7.0, ALU.add, ALU.min,
                                [f"pb{i2}", "bgu"], [f"gc{i2}"])
                        self.act(sg[i2][:], gc[i2][:], AF.Sigmoid, [f"gc{i2}"], [f"sg{i2}"], scale=1.702)
                        self.act(ub[i2][:], ub_[:], AF.Identity, [f"pb{2 + i2}", "bgu"], [f"ub{i2}"],
                                 bias=bgu[:, e, 8 + j:9 + j])
                        self.ts("dve", ub[i2][:], ub[i2][:], -6.0, 8.0, ALU.max, ALU.min, [f"ub{i2}"], [f"ub{i2}"])
                        self.tt("dve", p1[i2][:], gc[i2][:], sg[i2][:], ALU.mult, [f"gc{i2}", f"sg{i2}"], [f"p1{i2}"])
                        self.tt("dve", actT[:, j, ss], p1[i2][:], ub[i2][:], ALU.mult, [f"p1{i2}", f"ub{i2}"], ["actT"])
                wdv = wdD[e].rearrange("(j p) d -> p j d", p=128)
                for half in range(2):
                    w = wdn[dcnt % 3]
                    wk = f"wdn{dcnt % 3}"
                    dcnt += 1
                    P.dma("pool", w[:], wdv[:, :, half * 512:(half + 1) * 512], writes=[wk])
                    for tt_ in range(NTT):
                        ts_ = slice(tt_ * 128, (tt_ + 1) * 128)
                        pb = 4 + ycnt % 2
                        ycnt += 1
                        ps = self.banks[pb]
                        for j in range(8):
                            self.mm(ps[:], actT[:, j, ts_], w[:, j, :], j == 0, j == 7, [wk, "actT"], [f"pb{pb}"])
                        a_ = acc[:, tt_, half * 512:(half + 1) * 512]
                        self.stt("dve", a_, ps[:], gts[:, tt_, e:e + 1], a_, ALU.mult, ALU.add,
                                 [f"pb{pb}", "gts", "h32"], ["h32"])
            if getattr(self, "debug", False):
                if tg == 0:
                    self.dbg_m = self.dout("d_m", [T, D])
                P.dma("sp", self.dbg_m.rearrange("(n p) d -> p n d", p=128)[:, tg * NTT:(tg + 1) * NTT, :], acc[:],
                      reads=["h32"], out=True)
            for dj in range(8):
                for q4 in range(NTT // 4):
                    pb = 6 + (dj * 2 + q4) % 2
                    ps = self.banks[pb]
                    for t4 in range(4):
                        tt_ = q4 * 4 + t4
                        self.mm(ps[:, t4 * 128:(t4 + 1) * 128], acc[:, tt_, dj * 128:(dj + 1) * 128], C["ident"][:],
                                True, True, ["h32", "c:ident"], [f"pb{pb}"])
                    xs_ = x[:, dj, q4 * 512:(q4 + 1) * 512]
                    self.stt("dve", xs_, ps[:], mods["G2"][:, dj:dj + 1], xs_, ALU.mult, ALU.add,
                             [f"pb{pb}", mods["kmod"], "xg"], ["xg"])
            if final:
                for s in range(2):
                    ss = slice(s * 512, (s + 1) * 512)
                    self.rstd_of(x[:, :, ss], ["xg"], 512, sq, rstd[:], "m", 2)
                    for kc in range(8):
                        self.stt("dve", x[:, kc, ss], x[:, kc, ss], fnc[:, kc:kc + 1], rstd[:], ALU.mult, ALU.mult,
                                 ["xg", "fnc", "rstdm"], ["xg"])
            P.dma("sp", XOv[:, :, tsl], x[:], reads=["xg"], writes=[("XT", g) for g in gl], out=True)
        sb.release(m0)


def _cols(v, n):
    return np.ascontiguousarray(np.asarray(v, np.float32).reshape(n, 128).T)


def core_consts(inputs, core):
    b, hf = core // 2, core % 2
    p = np.arange(128)
    i = np.arange(512)
    k = {}
    k["k_ident"] = np.eye(128, dtype=np.float32)
    k["k_onesf"] = np.ones((128, 128), np.float32)
    k["k_onesb"] = np.ones((128, 128), NPBF)
    k["k_identb"] = np.eye(128, dtype=np.float32).astype(NPBF)
    k["k_negtri"] = (-(p[:, None] >= p[None, :]).astype(np.float32)).astype(NPBF)
    k["k_negones"] = (-np.ones((128, 128), np.float32)).astype(NPBF)
    sel = np.zeros((E, E, 128), np.float32)
    for e in range(E):
        sel[e, e, :] = 1.0
    k["k_sel"] = sel.reshape(E, E * 128).astype(NPBF)
    mA = np.zeros((128, 4, 512), np.float32)
    mB = np.zeros((128, 4, 512), np.float32)
    for d in range(4):
        kp = 128 * d + p[:, None]
        mA[:, d, :] = np.where(kp <= i[None, :], 0.0, NEG)
        mB[:, d, :] = np.where(kp < i[None, :], 0.0, NEG)
    k["k_maskA"] = mA.reshape(128, 2048).astype(NPBF)
    k["k_maskB"] = mB.reshape(128, 2048).astype(NPBF)
    k["k_flagcol"] = np.full((128, 1), 0.0 if hf == 1 else NEG, np.float32)
    k["k_zcol"] = np.zeros((128, 1), np.float32)
    k["k_cT"] = _cols(inputs["c"][b], 8)
    return k


def alibi_tables(hf):
    qp = hf * T + np.arange(T)
    kp = np.concatenate([(1 - hf) * T + np.arange(T), hf * T + np.arange(T)])
    qa = np.zeros((8, 4, T), np.float32)
    ka = np.zeros((8, 4, 2 * T), np.float32)
    for h in range(8):
        s = 2.0 ** (-(h + 1))
        qa[h, 0] = -s * 256.0 * (qp // 256)
        qa[h, 1] = -s * (qp % 256)
        qa[h, 2] = 1.0
        qa[h, 3] = 1.0
        ka[h, 0] = 1.0
        ka[h, 1] = 1.0
        ka[h, 2] = s * 256.0 * (kp // 256)
        ka[h, 3] = s * (kp % 256)
    return qa.astype(NPBF), ka.astype(NPBF)


def layer_shared(inputs, l, cache):
    if l in cache:
        return cache[l]
    f = np.float32
    w = {}
    w[f"ada_w{l}"] = np.ascontiguousarray(inputs["ada_w"][l], f)
    w[f"ada_bT{l}"] = _cols(inputs["ada_b"][l], 48)
    w[f"nmT{l}"] = np.concatenate([_cols(inputs["norm_mix"][l], 8), _cols(inputs["norm_moe"][l], 8)], axis=1)
    if l < NA:
        w[f"wqkv{l}"] = np.ascontiguousarray(inputs["a_wqkv"][l], f)
        w[f"wo{l}"] = np.ascontiguousarray(inputs["a_wo"][l], f)
        w[f"lamrep{l}"] = np.ascontiguousarray(np.tile(np.asarray(inputs["a_lambda"][l], f).reshape(1, 256), (128, 1)))
        w[f"sublnT{l}"] = np.asarray(inputs["a_subln"][l], f).reshape(128, 1).copy()
    else:
        w[f"wq{l}"] = np.ascontiguousarray(inputs["b_wq"][l - NA], f)
        w[f"wo{l}"] = np.ascontiguousarray(inputs["b_wo"][l - NA], f)
    if l == NA:
        w["kvw"] = np.ascontiguousarray(inputs["kv_w"], f)
        w["kv_ada_w"] = np.ascontiguousarray(inputs["kv_ada_w"], f)
        w["kv_ada_bT"] = _cols(inputs["kv_ada_b"], 16)
        w["kvnT"] = _cols(inputs["kv_norm"], 8)
    w[f"rw{l}"] = np.ascontiguousarray(inputs["router_w"][l], f)
    w[f"rb{l}"] = np.asarray(inputs["router_b"][l], f).reshape(1, E).copy()
    w[f"bd{l}"] = np.ascontiguousarray(inputs["b_down"][l], f)
    w[f"bguT{l}"] = np.ascontiguousarray(np.asarray(inputs["b_gate_up"][l], f).reshape(E, 16, 128).transpose(2, 0, 1))
    gu = np.asarray(inputs["w_gate_up"][l], f).reshape(E, 8, 128, 2, 8, 128)
    w[f"wgu{l}"] = np.ascontiguousarray(gu.transpose(0, 4, 2, 1, 3, 5)).reshape(E, 8, 128, 2048)
    w[f"wd{l}"] = np.ascontiguousarray(inputs["w_down"][l], f)
    if l == DEPTH - 1:
        w["fnT"] = _cols(inputs["final_norm"], 8)
    cache[l] = w
    return w


def build_proj(l):
    nc = bass.Bass("TRN2", target_bir_lowering=False)
    B = Builder(nc)
    B.load_consts()
    XT = B.din("XT", [D, T])
    mods = B.layer_mods(l)
    outs = {"Q": B.dout("Q", [D, T], BF16)}
    if l < NA:
        outs["K"] = B.dout("K", [D, T], BF16)
        outs["V"] = B.dout("V", [T, D], BF16)
    elif l == NA:
        outs["KB"] = B.dout("KB", [D, T], BF16)
        outs["VB"] = B.dout("VB", [T, D], BF16)
    B.phase_proj(l, XT, mods, outs)
    for o in B.P.ops:
        if o is not None and o.is_dma and any(isinstance(k, tuple) and k[0] in ("Q", "K", "V", "KB", "VB") for k in o.writes):
            o.is_out = True
    B.P.emit()
    return nc, B


def build_res(l, debug=False):
    nc = bass.Bass("TRN2", target_bir_lowering=False)
    B = Builder(nc)
    B.debug = debug
    B.load_consts()
    XT = B.din("XT", [D, T])
    mods = B.layer_mods(l)
    QT = B.din("Q", [D, T], BF16)
    Ko = B.din("Ko", [D, T], BF16)
    Kp = B.din("Kp", [D, T], BF16)
    Vo = B.din("Vo", [T, D], BF16)
    Vp = B.din("Vp", [T, D], BF16)
    OT = B.dint("OT", [D, T], BF16)
    XO = B.dout("XO", [D, T])
    if l < NA:
        qa = B.din("qaug", [8, 4, T], BF16)
        ka = B.din("kaug", [8, 4, 2 * T], BF16)
        B.phase_att_a(l, QT, Ko, Kp, Vo, Vp, OT, [], qa, ka)
    else:
        B.phase_att_b(l, QT, Ko, Kp, Vo, Vp, OT, [], lambda hp: [], [])
    B.P.barrier()
    B.phase_res_moe(l, XT, OT, mods, XO, final=(l == DEPTH - 1))
    B.P.emit()
    return nc, B


PAIRS = [[0, 1], [2, 3], [4, 5], [6, 7]]


def build_fused():
    nc = bass.Bass("TRN2", target_bir_lowering=False)
    B = Builder(nc)
    P = B.P
    B.load_consts()
    XIN = B.din("XT", [D, T])
    XT = B.dint("XTs", [D, T])
    XO = B.dout("XO", [D, T])
    QT = B.dint("Qs", [D, T], BF16)
    OT = B.dint("OTs", [D, T], BF16)
    Kx = {n: B.dint(n + "s", [D, T], BF16) for n in ("K", "KB")}
    Vx = {n: B.dint(n + "s", [T, D], BF16) for n in ("V", "VB")}
    Kall = {n: B.dint(n + "all", [2 * D, T], BF16) for n in ("K", "KB")}
    Vall = {n: B.dint(n + "all", [2 * T, D], BF16) for n in ("V", "VB")}
    Kp = {n: B.dint(n + "p", [D, T], BF16) for n in ("K", "KB")}
    Vp = {n: B.dint(n + "p", [T, D], BF16) for n in ("V", "VB")}
    qa = B.din("qaug", [8, 4, T], BF16)
    ka = B.din("kaug", [8, 4, 2 * T], BF16)
    XIv = XIN.rearrange("(kc p) t -> p kc t", p=128)
    XTv = XT.rearrange("(kc p) t -> p kc t", p=128)
    for g in range(NG):
        P.dma("sp", XTv[:, :, g * 512:(g + 1) * 512], XIv[:, :, g * 512:(g + 1) * 512], writes=[("XT", g)])
    bypass = ALU.bypass

    _oth = {}

    def oth(e):
        if "v" not in _oth:
            _oth["v"] = (e.partition_id() + 1) % 2
        return _oth["v"]

    NCH = 4

    def exchange(kn, vn):
        kr, vr = D // NCH, T // NCH
        Kc = Kall[kn].rearrange("(i r q) t -> i (r q) t", i=NCH, r=2)
        Vc = Vall[vn].rearrange("(i r q) c -> i (r q) c", i=NCH, r=2)
        for i in range(NCH):
            kkeys = [(kn, j, g) for j in range(2 * i, 2 * i + 2) for g in range(NG)]
            vkeys = [(vn, g) for g in range(2 * i, 2 * i + 2)]
            P.cc(lambda e, i=i: e.collective_compute("AllGather", bypass, replica_groups=PAIRS,
                                                     ins=[Kx[kn][i * kr:(i + 1) * kr, :]], outs=[Kc[i]]),
                 reads=kkeys, writes=[(kn + "all", i)])
            P.cc(lambda e, i=i: e.collective_compute("AllGather", bypass, replica_groups=PAIRS,
                                                     ins=[Vx[vn][i * vr:(i + 1) * vr, :]], outs=[Vc[i]]),
                 reads=vkeys, writes=[(vn + "all", i)])
        for i in range(NCH):
            P.op("pool", lambda e, i=i: e.dma_start(out=Kp[kn][i * kr:(i + 1) * kr, :],
                                                    in_=Kc[i][bass.ds(oth(e) * kr, kr), :]),
                 reads=[(kn + "all", i)], writes=[(kn + "p", i)], dma=True)
            P.op("pool", lambda e, i=i: e.dma_start(
                out=Vp[vn][i * vr:(i + 1) * vr, :].rearrange("(n p) c -> p n c", p=128),
                in_=Vc[i][bass.ds(oth(e) * vr, vr), :].rearrange("(n p) c -> p n c", p=128)),
                 reads=[(vn + "all", i)], writes=[(kn + "pv", i)], dma=True)

    def prev_keys(kn):
        return [(kn + "p", i) for i in range(NCH)] + [(kn + "pv", i) for i in range(NCH)]

    for l in range(DEPTH):
        mods = B.layer_mods(l)
        outs = {"Q": QT}
        if l < NA:
            outs["K"], outs["V"] = Kx["K"], Vx["V"]
        elif l == NA:
            outs["KB"], outs["VB"] = Kx["KB"], Vx["VB"]
        B.phase_proj(l, XT, mods, outs)
        if l < NA:
            exchange("K", "V")
            B.phase_att_a(l, QT, Kx["K"], Kp["K"], Vx["V"], Vp["V"], OT, prev_keys("K"), qa, ka)
        else:
            if l == NA:
                exchange("KB", "VB")
            B.phase_att_b(l, QT, Kx["KB"], Kp["KB"], Vx["VB"], Vp["VB"], OT, prev_keys("KB"),
                          lambda hp: [("KB", hp, g) for g in range(NG)], [("VB", g) for g in range(NG)])
        last = l == DEPTH - 1
        B.phase_res_moe(l, XT, OT, mods, XO if last else XT, final=last)
    P.emit()
    return nc, B


_PROG = {}


def kernel(**inputs):
    ncores = 8
    if "fused" not in _PROG:
        _PROG["fused"] = build_fused()
    nc, B = _PROG["fused"]
    x = np.asarray(inputs["x"], np.float32)
    cache = {}
    shared = {}
    for l in range(DEPTH):
        shared.update(layer_shared(inputs, l, cache))
    names = [n for n in B.dram if n in shared]
    maps = []
    for c in range(ncores):
        m = dict(core_consts(inputs, c))
        m["XT"] = np.ascontiguousarray(x[c // 2, (c % 2) * T:(c % 2 + 1) * T, :].T)
        m["qaug"], m["kaug"] = alibi_tables(c % 2)
        for n in names:
            m[n] = shared[n]
        maps.append(m)
    res = run_bass_kernel_spmd(nc, maps, core_ids=list(range(ncores))).results
    out = np.empty((BATCH, SEQ, D), np.float32)
    for c in range(ncores):
        out[c // 2, (c % 2) * T:(c % 2 + 1) * T, :] = res[c]["XO"].T
    return out
```

```python
import math
import numpy as np
import ml_dtypes
import concourse.bass as bass
import concourse.mybir as mybir
from concourse.bass_utils import run_bass_kernel_spmd

F32 = mybir.dt.float32
BF16 = mybir.dt.bfloat16
AF = mybir.ActivationFunctionType
ALU = mybir.AluOpType
NPBF = ml_dtypes.bfloat16

D = 1024
SEQ = 8192
BATCH = 4
DEPTH = 4
NA = 2
T = 4096
NG = 8
E = 32
EPS = 1e-6
NEG = -32768.0
ENGS = ("pe", "dve", "act", "pool", "sp")


class Op:
    __slots__ = ("eng", "fn", "reads", "writes", "is_dma", "deps", "signal", "sem", "semval",
                 "idx", "is_out", "prev_on_sem", "bar", "is_cc")

    def __init__(self, eng, fn, reads, writes, is_dma, is_out):
        self.eng = eng
        self.fn = fn
        self.reads = reads
        self.writes = writes
        self.is_dma = is_dma
        self.deps = []
        self.signal = False
        self.sem = None
        self.semval = 0
        self.is_out = is_out
        self.prev_on_sem = 0
        self.is_cc = False


class Prog:
    def __init__(self, nc, n_dma_sems=20):
        self.nc = nc
        self.ops = []
        self.state = {}
        self.n_dma_sems = n_dma_sems

    def op(self, eng, fn, reads=(), writes=(), dma=False, out=False):
        o = Op(eng, fn, tuple(reads), tuple(writes), dma, out)
        o.idx = len(self.ops)
        deps = set()
        for k in o.reads:
            st = self.state.get(k)
            if st is not None and st[0] is not None:
                deps.add(st[0])
        for k in o.writes:
            st = self.state.get(k)
            if st is not None:
                if st[0] is not None:
                    deps.add(st[0])
                for r in st[1]:
                    deps.add(r)
        for k in o.reads:
            if isinstance(k, str) and k.startswith("c:"):
                continue
            st = self.state.setdefault(k, [None, []])
            st[1].append(o)
        for k in o.writes:
            self.state[k] = [o, []]
        deps.discard(o)
        o.deps = sorted(deps, key=lambda d: d.idx)
        self.ops.append(o)
        return o

    def dma(self, eng, out_ap, in_ap, reads=(), writes=(), out=False):
        return self.op(eng, lambda e: e.dma_start(out=out_ap, in_=in_ap), reads, writes, dma=True, out=out)

    def cc(self, fn, reads=(), writes=()):
        o = self.op("pool", fn, reads, writes, dma=True)
        o.is_cc = True
        return o

    def barrier(self):
        self.ops.append(None)

    def emit(self):
        nc = self.nc
        ops = self.ops
        real = [o for o in ops if o is not None]
        for o in real:
            for d in o.deps:
                if d.is_dma:
                    continue
                if d.eng == o.eng and d.eng == "pe" and not o.is_dma:
                    continue
                d.signal = True
        last = {}
        for o in ops:
            if o is None:
                for e, lo in last.items():
                    lo.signal = True
            elif not o.is_dma:
                last[o.eng] = o
        esem = {e: nc.alloc_semaphore(f"s_{e}") for e in ENGS}
        dq = ("sp", "act", "pool")
        dsem = {e: [nc.alloc_semaphore(f"d_{e}_{i}") for i in range(self.n_dma_sems)] for e in dq}
        dcount = {e: [0] * self.n_dma_sems for e in dq}
        rr = {e: 0 for e in dq}
        tick = {e: 0 for e in ENGS}
        bars = []
        ccs = []
        per = {e: [] for e in ENGS}
        nbar = 0
        for o in ops:
            if o is None:
                w = {}
                for e in ENGS:
                    if tick[e] > 0:
                        w[esem[e]] = tick[e]
                for e in dq:
                    for i in range(self.n_dma_sems):
                        if dcount[e][i] > 0:
                            w[dsem[e][i]] = dcount[e][i] * 16
                for co in ccs:
                    w[co.sem] = 1
                bars.append(w)
                nbar += 1
                continue
            e = o.eng
            o.bar = nbar
            per[e].append(o)
            if o.is_cc:
                o.sem = nc.alloc_semaphore(f"cc_{o.idx}")
                o.semval = 1
                ccs.append(o)
            elif o.is_dma:
                k = rr[e]
                rr[e] = (k + 1) % self.n_dma_sems
                o.prev_on_sem = dcount[e][k] * 16
                dcount[e][k] += 1
                o.sem = dsem[e][k]
                o.semval = dcount[e][k] * 16
            elif o.signal:
                tick[e] += 1
                o.sem = esem[e]
                o.semval = tick[e]
        out_waits = {}
        for o in real:
            if o.is_dma and o.is_out:
                out_waits[o.sem] = max(out_waits.get(o.sem, 0), o.semval)
        self.stats = {e: len(per[e]) for e in ENGS}

        def run(e, engobj):
            seen = {}
            curbar = 0
            for o in per[e]:
                waits = {}
                if o.bar > curbar:
                    curbar = o.bar
                    waits.update(bars[curbar - 1])
                for d in o.deps:
                    if (not d.is_dma) and d.eng == e and e == "pe" and not o.is_dma:
                        continue
                    if d.sem is None:
                        continue
                    if waits.get(d.sem, 0) < d.semval:
                        waits[d.sem] = d.semval
                if o.is_dma and o.prev_on_sem > 0 and waits.get(o.sem, 0) < o.prev_on_sem:
                    waits[o.sem] = o.prev_on_sem
                for s, v in waits.items():
                    if seen.get(s, 0) >= v:
                        continue
                    seen[s] = v
                    engobj.wait_ge(s, v)
                ins = o.fn(engobj)
                if o.is_cc:
                    ins.then_inc(o.sem)
                elif o.is_dma:
                    ins.then_inc(o.sem, 16)
                elif o.signal:
                    ins.then_inc(o.sem, 1)
            if e == "sp":
                for s, v in out_waits.items():
                    engobj.wait_ge(s, v)

        with nc.Block() as block:
            @block.tensor
            def _(t):
                run("pe", t)

            @block.vector
            def _(v):
                run("dve", v)

            @block.scalar
            def _(s):
                run("act", s)

            @block.gpsimd
            def _(g):
                run("pool", g)

            @block.sync
            def _(s):
                run("sp", s)


class SbufAlloc:
    def __init__(self, nc, base=16512, limit=229312, prog=None):
        self.nc = nc
        self.prog = prog
        self.off = base
        self.limit = limit
        self.n = 0

    def mark(self):
        return self.off

    def release(self, m):
        self.off = m
        if self.prog is not None:
            self.prog.barrier()

    def alloc(self, shape, dtype, name="t"):
        esz = 4 if dtype == F32 else 2
        nbytes = int(np.prod(shape[1:])) * esz
        nbytes = (nbytes + 63) // 64 * 64
        assert self.off + nbytes <= self.limit, f"SBUF overflow {name} {self.off}+{nbytes}"
        self.n += 1
        t = self.nc.alloc_sbuf_tensor_at(f"{name}_{self.n}", list(shape), dtype, offset=self.off)
        self.off += nbytes
        return t


class Builder:
    def __init__(self, nc):
        self.nc = nc
        self.P = Prog(nc)
        self.sb = SbufAlloc(nc, prog=self.P)
        self.psum = nc.alloc_psum_tensor("psum_all", [128, 4096], F32)
        self.banks = [self.psum[:, i * 512:(i + 1) * 512] for i in range(8)]
        self.uid = 0
        self.dram = {}

    def din(self, name, shape, dtype=F32):
        if name in self.dram:
            return self.dram[name]
        t = self.nc.dram_tensor(name, list(shape), dtype, kind="ExternalInput").ap()
        self.dram[name] = t
        return t

    def dout(self, name, shape, dtype=F32):
        t = self.nc.dram_tensor(name, list(shape), dtype, kind="ExternalOutput").ap()
        self.dram[name] = t
        return t

    def dint(self, name, shape, dtype=F32):
        t = self.nc.dram_tensor(name, list(shape), dtype).ap()
        self.dram[name] = t
        return t

    def mm(self, out, lhsT, rhs, start, stop, reads, writes):
        self.P.op("pe", lambda e: e.matmul(out, lhsT, rhs, start=start, stop=stop), reads, writes)

    def act(self, out, in_, func, reads, writes, bias=0.0, scale=1.0):
        self.P.op("act", lambda e: e.activation(out=out, in_=in_, func=func, bias=bias, scale=scale),
                  reads, writes)

    def tt(self, eng, out, in0, in1, op, reads, writes):
        self.P.op(eng, lambda e: e.tensor_tensor(out=out, in0=in0, in1=in1, op=op), reads, writes)

    def ts(self, eng, out, in0, s1, s2, op0, op1, reads, writes):
        if s2 is None:
            self.P.op(eng, lambda e: e.tensor_single_scalar(out=out, in_=in0, scalar=s1, op=op0), reads, writes)
        else:
            self.P.op(eng, lambda e: e.tensor_scalar(out=out, in0=in0, scalar1=s1, scalar2=s2, op0=op0, op1=op1),
                      reads, writes)

    def stt(self, eng, out, in0, scalar, in1, op0, op1, reads, writes):
        self.P.op(eng, lambda e: e.scalar_tensor_tensor(out=out, in0=in0, scalar=scalar, in1=in1, op0=op0, op1=op1),
                  reads, writes)

    def copy(self, eng, out, in_, reads, writes):
        self.P.op(eng, lambda e: e.tensor_copy(out=out, in_=in_), reads, writes)

    def load_consts(self):
        sb, P = self.sb, self.P
        C = {}
        specs = [("ident", [128, 128], F32), ("onesf", [128, 128], F32), ("onesb", [128, 128], BF16),
                 ("negtri", [128, 128], BF16), ("negones", [128, 128], BF16),
                 ("identb", [128, 128], BF16),
                 ("flagcol", [128, 1], F32), ("cT", [128, 8], F32), ("zcol", [128, 1], F32)]
        for name, shape, dt in specs:
            d = self.din("k_" + name, shape, dt)
            t = sb.alloc(shape, dt, name)
            P.dma("sp", t[:], d, writes=["c:" + name])
            C[name] = t
        self.C = C
        sig = sb.alloc([128, 8], F32, "csig")
        cact = sb.alloc([128, 8], F32, "cact")
        self.act(sig[:], C["cT"][:], AF.Sigmoid, ["c:cT"], ["csig"])
        self.tt("dve", cact[:], C["cT"][:], sig[:], ALU.mult, ["c:cT", "csig"], ["c:cact"])
        C["cact"] = cact

    def mod_cols(self, w_ap, bT_ap, ncols, tag):
        sb, P, C = self.sb, self.P, self.C
        nch = ncols // 128
        out = sb.alloc([128, nch], F32, "mod" + tag)
        bt = sb.alloc([128, nch], F32, "modb" + tag)
        P.dma("sp", bt[:], bT_ap, writes=["modb" + tag])
        m = sb.mark()
        slabw = 768 if ncols % 768 == 0 else 512
        nslab = ncols // slabw
        slabs = [sb.alloc([128, 8, slabw], F32, "slab") for _ in range(2)]
        wv = w_ap.rearrange("(kc p) n -> p kc n", p=128)
        ps = self.banks[7]
        for s in range(nslab):
            sl = slabs[s % 2]
            key = f"slab{s % 2}"
            P.dma("sp", sl[:], wv[:, :, s * slabw:(s + 1) * slabw], writes=[key])
            for n in range(slabw // 128):
                nn = s * (slabw // 128) + n
                for kc in range(8):
                    self.mm(ps[:, nn:nn + 1], sl[:, kc, n * 128:(n + 1) * 128], C["cact"][:, kc:kc + 1],
                            kc == 0, kc == 7, [key, "c:cact"], ["pb7"])
        self.tt("dve", out[:], ps[:, 0:nch], bt[:], ALU.add, ["pb7", "modb" + tag], ["mod" + tag])
        sb.release(m)
        return out, "mod" + tag

    def layer_mods(self, l):
        sb, P = self.sb, self.P
        ada_w = self.din(f"ada_w{l}", [D, 6 * D])
        ada_bT = self.din(f"ada_bT{l}", [128, 48])
        nmT = self.din(f"nmT{l}", [128, 16])
        mod, mk = self.mod_cols(ada_w, ada_bT, 6 * D, f"L{l}")
        nm = sb.alloc([128, 16], F32, "nm")
        P.dma("sp", nm[:], nmT, writes=[f"nm{l}"])
        A = sb.alloc([128, 16], F32, "Acols")
        self.stt("dve", A[:, 0:8], mod[:, 8:16], 1.0, nm[:, 0:8], ALU.add, ALU.mult, [mk, f"nm{l}"], [f"A1_{l}"])
        self.stt("dve", A[:, 8:16], mod[:, 32:40], 1.0, nm[:, 8:16], ALU.add, ALU.mult, [mk, f"nm{l}"], [f"A2_{l}"])
        return dict(A1=A[:, 0:8], B1=mod[:, 0:8], G1=mod[:, 16:24], A2=A[:, 8:16], B2=mod[:, 24:32],
                    G2=mod[:, 40:48], kA1=f"A1_{l}", kA2=f"A2_{l}", kmod=mk)

    def rstd_of(self, x, xkeys, n, sq, rstd, tag, bank, dim=1024.0, nk=8):
        C = self.C
        ps = self.banks[bank]
        for kc in range(nk):
            q = sq[kc % 2]
            self.act(q[:, 0:n], x[:, kc, :], AF.Square, xkeys, [f"sq{tag}{kc % 2}"])
            self.mm(ps[:, 0:n], C["onesb"][:], q[:, 0:n], kc == 0, kc == nk - 1,
                    [f"sq{tag}{kc % 2}", "c:onesb"], [f"pb{bank}"])
        self.ts("dve", rstd, ps[:, 0:n], 1.0 / dim, EPS, ALU.mult, ALU.add, [f"pb{bank}"], ["rstd" + tag])
        self.act(rstd, rstd, AF.Ln, ["rstd" + tag], ["rstd" + tag])
        self.act(rstd, rstd, AF.Exp, ["rstd" + tag], ["rstd" + tag], scale=-0.5)

    def phase_proj(self, l, XT, mods, outs):
        sb, P, C = self.sb, self.P, self.C
        m0 = sb.mark()
        isA = l < NA
        wts = {}
        if isA:
            w = self.din(f"wqkv{l}", [D, 3 * D]).rearrange("(kc p) n -> p kc n", p=128)
            for i, nm in enumerate(("Q", "K", "V")):
                t = sb.alloc([128, 8, D], BF16, "w" + nm)
                P.dma("pool", t[:], w[:, :, i * D:(i + 1) * D], writes=["w" + nm])
                wts[nm] = t
        else:
            w = self.din(f"wq{l}", [D, D]).rearrange("(kc p) n -> p kc n", p=128)
            t = sb.alloc([128, 8, D], BF16, "wQ")
            P.dma("pool", t[:], w, writes=["wQ"])
            wts["Q"] = t
            if "KB" in outs:
                w = self.din("kvw", [D, 2 * D]).rearrange("(kc p) n -> p kc n", p=128)
                for i, nm in enumerate(("KB", "VB")):
                    t = sb.alloc([128, 8, D], BF16, "w" + nm)
                    P.dma("pool", t[:], w[:, :, i * D:(i + 1) * D], writes=["w" + nm])
                    wts[nm] = t
                kvmod, kvk = self.mod_cols(self.din("kv_ada_w", [D, 2 * D]), self.din("kv_ada_bT", [128, 16]),
                                           2 * D, "KV")
                kvn = sb.alloc([128, 8], F32, "kvn")
                P.dma("sp", kvn[:], self.din("kvnT", [128, 8]), writes=["kvn"])
                Akv = sb.alloc([128, 8], F32, "Akv")
                self.stt("dve", Akv[:], kvmod[:, 8:16], 1.0, kvn[:], ALU.add, ALU.mult, [kvk, "kvn"], ["Akv"])
        XTv = XT.rearrange("(kc p) t -> p kc t", p=128)
        xs = [sb.alloc([128, 8, 512], F32, "x") for _ in range(2)]
        sqs = [sb.alloc([128, 512], BF16, "sq") for _ in range(2)]
        rstd = sb.alloc([128, 512], F32, "rstd")
        tmp = [sb.alloc([128, 512], F32, "tmp") for _ in range(2)]
        hT = [sb.alloc([128, 8, 512], BF16, "hT") for _ in range(2)]
        hK = sb.alloc([128, 8, 512], BF16, "hK") if "KB" in outs else None
        ev = [sb.alloc([128, 512], BF16, "ev") for _ in range(4)]
        vst = [sb.alloc([128, 4, D], BF16, "vst") for _ in range(2)]
        evi = 0
        pbi = 0
        for g in range(NG):
            x = xs[g % 2]
            xk = f"x{g % 2}"
            P.dma("sp", x[:], XTv[:, :, g * 512:(g + 1) * 512], reads=[("XT", g)], writes=[xk])
            self.rstd_of(x, [xk], 512, sqs, rstd[:], "p", 6)
            h = hT[g % 2]
            hk = f"hT{g % 2}"
            for kc in range(8):
                tm = tmp[kc % 2]
                self.stt("dve", tm[:], x[:, kc, :], mods["A1"][:, kc:kc + 1], rstd[:], ALU.mult, ALU.mult,
                         [xk, mods["kA1"], "rstdp"], [f"tmp{kc % 2}"])
                self.act(h[:, kc, :], tm[:], AF.Identity, [f"tmp{kc % 2}", mods["kmod"]], [hk],
                         bias=mods["B1"][:, kc:kc + 1])
            if hK is not None:
                for kc in range(8):
                    tm = tmp[kc % 2]
                    self.stt("dve", tm[:], x[:, kc, :], Akv[:, kc:kc + 1], rstd[:], ALU.mult, ALU.mult,
                             [xk, "Akv", "rstdp"], [f"tmp{kc % 2}"])
                    self.act(hK[:, kc, :], tm[:], AF.Identity, [f"tmp{kc % 2}", kvk], ["hK"],
                             bias=kvmod[:, kc:kc + 1])
            if getattr(self, "debug", False) and g == 0:
                P.dma("sp", self.dout("d_rstd", [128, 512]), rstd[:], reads=["rstdp"], out=True)
                P.dma("sp", self.dout("d_hT", [128, 8, 512], BF16), h[:], reads=[hk], out=True)
                P.dma("sp", self.dout("d_x", [128, 8, 512]), x[:], reads=[xk], out=True)
            for nm, src, srck, scale in (("Q", h, hk, 0.125), ("K", h, hk, 1.0), ("KB", hK, "hK", 1.0)):
                if nm not in outs or nm not in wts:
                    continue
                for j in range(8):
                    pb = pbi % 4
                    pbi += 1
                    ps = self.banks[pb]
                    for kc in range(8):
                        self.mm(ps[:], wts[nm][:, kc, j * 128:(j + 1) * 128], src[:, kc, :], kc == 0, kc == 7,
                                ["w" + nm, srck], [f"pb{pb}"])
                    e = ev[evi % 4]
                    ek = f"ev{evi % 4}"
                    evi += 1
                    if j % 2 == 0:
                        self.act(e[:], ps[:], AF.Identity, [f"pb{pb}"], [ek], scale=scale)
                    else:
                        self.ts("dve", e[:], ps[:], scale, None, ALU.mult, None, [f"pb{pb}"], [ek])
                    P.dma("sp", outs[nm][j * 128:(j + 1) * 128, g * 512:(g + 1) * 512], e[:], reads=[ek],
                          writes=[(nm, j, g)])
            for nm, src, srck in (("V", h, hk), ("VB", hK, "hK")):
                if nm not in outs or nm not in wts:
                    continue
                vs = vst[g % 2]
                vk = f"vst{g % 2}"
                for tt_ in range(4):
                    for half in range(2):
                        pb = pbi % 4
                        pbi += 1
                        ps = self.banks[pb]
                        for kc in range(8):
                            self.mm(ps[:], src[:, kc, tt_ * 128:(tt_ + 1) * 128],
                                    wts[nm][:, kc, half * 512:(half + 1) * 512], kc == 0, kc == 7,
                                    ["w" + nm, srck], [f"pb{pb}"])
                        if half == 0:
                            self.act(vs[:, tt_, 0:512], ps[:], AF.Identity, [f"pb{pb}"], [vk])
                        else:
                            self.copy("dve", vs[:, tt_, 512:1024], ps[:], [f"pb{pb}"], [vk])
                P.dma("sp", outs[nm].rearrange("(n p) c -> p n c", p=128)[:, g * 4:(g + 1) * 4, :], vs[:],
                      reads=[vk], writes=[(nm, g)])
        sb.release(m0)

    def phase_att_a(self, l, QT, KTo, KTp, Vo, Vp, OT, kprev_ready, qaugD, kaugD):
        sb, P, C = self.sb, self.P, self.C
        m0 = sb.mark()
        mt = sb.alloc([128, 4 * 512], BF16, "maskA")
        P.dma("sp", mt[:], self.din("k_maskA", [128, 4 * 512], BF16), writes=["c:maskA"])
        C["maskA"] = mt
        lam_init = 0.8 - 0.6 * math.exp(-0.3 * l)
        lamr = sb.alloc([128, 256], F32, "lamr")
        P.dma("sp", lamr[:], self.din(f"lamrep{l}", [128, 256]), writes=["lamr"])
        lprod = sb.alloc([128, 2, 64], F32, "lprod")
        lsum = sb.alloc([128, 2], F32, "lsum")
        lexp = sb.alloc([128, 2], F32, "lexp")
        neglam = sb.alloc([128, 1], F32, "neglam")
        self.tt("dve", lprod[:, 0, :], lamr[:, 0:64], lamr[:, 64:128], ALU.mult, ["lamr"], ["lprod"])
        self.tt("dve", lprod[:, 1, :], lamr[:, 128:192], lamr[:, 192:256], ALU.mult, ["lamr"], ["lprod"])
        P.op("dve", lambda e: e.reduce_sum(out=lsum[:], in_=lprod[:], axis=mybir.AxisListType.X), ["lprod"], ["lsum"])
        self.act(lexp[:], lsum[:], AF.Exp, ["lsum"], ["lexp"])
        self.tt("dve", neglam[:], lexp[:, 1:2], lexp[:, 0:1], ALU.subtract, ["lexp"], ["neglam"])
        self.ts("dve", neglam[:], neglam[:], -lam_init, None, ALU.add, None, ["neglam"], ["neglam"])
        subc = sb.alloc([128, 1], F32, "subc")
        P.dma("sp", subc[:], self.din(f"sublnT{l}", [128, 1]), writes=["subc"])
        self.ts("dve", subc[:], subc[:], 1.0 - lam_init, None, ALU.mult, None, ["subc"], ["subc"])

        kaug = [[sb.alloc([68, 2 * T], BF16, "kaug") for r in range(2)] for _ in range(2)]
        qaug = [[sb.alloc([68, T], BF16, "qaug") for r in range(2)] for _ in range(2)]
        vh = [sb.alloc([128, 64, 128], BF16, "vh") for _ in range(2)]
        pw = [sb.alloc([128, 1024], BF16, "pw") for _ in range(3)]
        pacc = sb.alloc([128, 1024], F32, "pacc")
        rl = sb.alloc([128, 1024], F32, "rl")
        o0 = sb.alloc([128, 512], F32, "o0")
        o1 = sb.alloc([128, 512], F32, "o1")
        osq = sb.alloc([128, 512], F32, "osq")
        orstd = sb.alloc([128, 512], F32, "orstd")
        onb = [sb.alloc([128, 512], BF16, "onb") for _ in range(2)]
        Vov = Vo.rearrange("(n p) c -> p n c", p=128)
        Vpv = Vp.rearrange("(n p) c -> p n c", p=128)
        Sw = [self.psum[:, 0:1024], self.psum[:, 1024:2048]]
        Lw = self.psum[:, 3072:4096]
        ocount = 0
        scount = 0
        pcount = 0
        for h in range(8):
            hb = h % 2
            for r in range(2):
                row0 = h * 128 + r * 64
                ka, qa = kaug[hb][r], qaug[hb][r]
                P.dma("sp", ka[0:64, 0:T], KTp[row0:row0 + 64, :], reads=kprev_ready, writes=[f"ka{hb}{r}"])
                P.dma("sp", ka[0:64, T:2 * T], KTo[row0:row0 + 64, :],
                      reads=[("K", h, g) for g in range(NG)], writes=[f"ka{hb}{r}"])
                P.dma("sp", ka[64:68, :], kaugD[h], writes=[f"ka{hb}{r}"])
                P.dma("sp", qa[0:64, :], QT[row0:row0 + 64, :], reads=[("Q", h, g) for g in range(NG)],
                      writes=[f"qa{hb}{r}"])
                P.dma("sp", qa[64:68, :], qaugD[h], writes=[f"qa{hb}{r}"])
            v = vh[hb]
            P.dma("sp", v[:, 0:32, :], Vpv[:, :, h * 128:(h + 1) * 128], reads=kprev_ready, writes=[f"vh{hb}"])
            P.dma("sp", v[:, 32:64, :], Vov[:, :, h * 128:(h + 1) * 128], reads=[("V", g) for g in range(NG)],
                  writes=[f"vh{hb}"])
            for g in range(NG):
                blocks = [(kb, None, True) for kb in range(32)]
                blocks += [(32 + j, None, False) for j in range(4 * g)]
                blocks += [(32 + 4 * g + d, d, False) for d in range(4)]
                nb = len(blocks)
                state = {}

                def s_stage(bi):
                    nonlocal scount, pcount
                    kb, d, isprev = blocks[bi]
                    si = scount % 2
                    scount += 1
                    S = Sw[si]
                    for r in range(2):
                        ka, qa = kaug[hb][r], qaug[hb][r]
                        o_ = S[:, r * 512:(r + 1) * 512]
                        self.mm(o_, ka[:, kb * 128:(kb + 1) * 128], qa[:, g * 512:(g + 1) * 512], True, d is None,
                                [f"ka{hb}{r}", f"qa{hb}{r}"], [f"Sw{si}"])
                        if d is not None:
                            self.mm(o_, C["identb"][:], C["maskA"][:, d * 512:(d + 1) * 512], False, True,
                                    ["c:identb", "c:maskA"], [f"Sw{si}"])
                    pi = pcount % 3
                    pcount += 1
                    self.act(pw[pi][:], S, AF.Exp, [f"Sw{si}", "c:flagcol"], [f"pw{pi}"],
                             bias=(C["flagcol"][:, 0:1] if isprev else C["zcol"][:, 0:1]))
                    state[bi] = pi

                def pv_stage(bi):
                    kb, d, isprev = blocks[bi]
                    pi = state.pop(bi)
                    for r in range(2):
                        self.mm(self.banks[4 + r], v[:, kb, :], pw[pi][:, r * 512:(r + 1) * 512], bi == 0, bi == nb - 1,
                                [f"vh{hb}", f"pw{pi}"], [f"pb{4 + r}"])
                    self.mm(self.banks[6], C["onesb"][:], pw[pi][:, 0:512], bi == 0, bi == nb - 1,
                            ["c:onesb", f"pw{pi}"], ["pb6"])
                    if bi == 0:
                        self.copy("dve", pacc[:, 0:512], pw[pi][:, 512:1024], [f"pw{pi}"], ["pacc"])
                    else:
                        self.tt("dve", pacc[:, 0:512], pacc[:, 0:512], pw[pi][:, 512:1024], ALU.add,
                                ["pacc", f"pw{pi}"], ["pacc"])

                s_stage(0)
                for bi in range(nb):
                    if bi + 1 < nb:
                        s_stage(bi + 1)
                    pv_stage(bi)
                self.mm(self.banks[7], C["onesf"][:], pacc[:, 0:512], True, True, ["c:onesf", "pacc"], ["pb7"])
                P.op("dve", lambda e: e.reciprocal(out=rl[:], in_=Lw), ["pb6", "pb7"], ["rl"])
                self.tt("dve", o0[:], self.banks[4], rl[:, 0:512], ALU.mult, ["pb4", "rl"], ["o0"])
                self.tt("dve", o1[:], self.banks[5], rl[:, 512:1024], ALU.mult, ["pb5", "rl"], ["o1"])
                self.stt("dve", o0[:], o1[:], neglam[:, 0:1], o0[:], ALU.mult, ALU.add, ["o1", "neglam", "o0"], ["o0"])
                self.tt("dve", osq[:], o0[:], o0[:], ALU.mult, ["o0"], ["osq"])
                ps = self.banks[7]
                self.mm(ps, C["onesf"][:], osq[:], True, True, ["c:onesf", "osq"], ["pb7"])
                self.ts("dve", orstd[:], ps, 1.0 / 128.0, EPS, ALU.mult, ALU.add, ["pb7"], ["orstd"])
                self.act(orstd[:], orstd[:], AF.Ln, ["orstd"], ["orstd"])
                self.act(orstd[:], orstd[:], AF.Exp, ["orstd"], ["orstd"], scale=-0.5)
                ob_ = onb[ocount % 2]
                obk = f"onb{ocount % 2}"
                ocount += 1
                self.stt("dve", ob_[:], o0[:], subc[:, 0:1], orstd[:], ALU.mult, ALU.mult, ["o0", "subc", "orstd"], [obk])
                P.dma("sp", OT[h * 128:(h + 1) * 128, g * 512:(g + 1) * 512], ob_[:], reads=[obk], writes=[("O", h, g)])
        sb.release(m0)

    def phase_att_b(self, l, QT, KTo, KTp, Vo, Vp, OT, kprev_ready, kown_keys, vown_keys):
        sb, P, C = self.sb, self.P, self.C
        m0 = sb.mark()
        mt = sb.alloc([128, 4 * 512], BF16, "maskB")
        P.dma("sp", mt[:], self.din("k_maskB", [128, 4 * 512], BF16), writes=["c:maskB"])
        C["maskB"] = mt
        kt = [sb.alloc([128, 2 * T], BF16, "ktb") for _ in range(2)]
        qt = [sb.alloc([128, T], BF16, "qtb") for _ in range(2)]
        vh = [sb.alloc([128, 64, 128], BF16, "vhb") for _ in range(2)]
        ebw = sb.alloc([128, 1024], F32, "ebw")
        spw = [sb.alloc([128, 1024], BF16, "spw") for _ in range(2)]
        aw = [sb.alloc([128, 1024], BF16, "aw") for _ in range(2)]
        ssum = sb.alloc([128, 1024], F32, "ssum")
        ssbw = [sb.alloc([128, 1024], BF16, "ssbw") for _ in range(2)]
        ost = [sb.alloc([128, 512], BF16, "ost") for _ in range(2)]
        Vov = Vo.rearrange("(n p) c -> p n c", p=128)
        Vpv = Vp.rearrange("(n p) c -> p n c", p=128)
        Zw = self.psum[:, 0:1024]
        Rw = self.psum[:, 1024:2048]
        for hp in range(8):
            hb = hp % 2
            k_, q_, v = kt[hb], qt[hb], vh[hb]
            kk, qk, vk = f"ktb{hb}", f"qtb{hb}", f"vhb{hb}"
            P.dma("sp", k_[:, 0:T], KTp[hp * 128:(hp + 1) * 128, :], reads=kprev_ready, writes=[kk])
            P.dma("sp", k_[:, T:2 * T], KTo[hp * 128:(hp + 1) * 128, :], reads=[kk_ for kk_ in kown_keys(hp)],
                  writes=[kk])
            P.dma("sp", q_[:], QT[hp * 128:(hp + 1) * 128, :], reads=[("Q", hp, g) for g in range(NG)], writes=[qk])
            P.dma("sp", v[:, 0:32, :], Vpv[:, :, hp * 128:(hp + 1) * 128], reads=kprev_ready, writes=[vk])
            P.dma("sp", v[:, 32:64, :], Vov[:, :, hp * 128:(hp + 1) * 128], reads=vown_keys, writes=[vk])
            for g in range(NG):
                os_ = ost[(hp * NG + g) % 2]
                osk = f"ost{(hp * NG + g) % 2}"
                blocks = [(32 + 4 * g + d, d, False) for d in (3, 2, 1, 0)]
                blocks += [(32 + j, None, False) for j in range(4 * g - 1, -1, -1)]
                blocks += [(kb, None, True) for kb in range(31, -1, -1)]
                nb = len(blocks)

                def ops_of(bi):
                    kb, d, isprev = blocks[bi]
                    bias = C["flagcol"][:, 0:1] if isprev else C["zcol"][:, 0:1]
                    mk = C["maskB"][:, d * 512:(d + 1) * 512] if d is not None else None
                    return kb, d, bias, mk, bi % 2

                def qk_into(dst, dkey, hh, kb, d, mk, last):
                    r0 = hh * 64
                    kblk = k_[r0:r0 + 64, kb * 128:(kb + 1) * 128]
                    qblk = q_[r0:r0 + 64, g * 512:(g + 1) * 512]
                    o_ = dst[:, hh * 512:(hh + 1) * 512]
                    self.mm(o_, kblk, qblk, True, last and d is None, [kk, qk], [dkey])
                    if d is not None:
                        self.mm(o_, C["identb"][:], mk, False, last, ["c:identb", "c:maskB"], [dkey])

                def stage1(bi):
                    kb, d, bias, mk, par = ops_of(bi)
                    for hh in range(2):
                        qk_into(Zw, "Zw", hh, kb, d, mk, True)
                    self.act(ebw[:], Zw, AF.Exp, ["Zw", "c:flagcol"], ["ebw"], bias=bias)
                    self.act(spw[par][:], ebw[:], AF.Ln, ["ebw"], [f"spw{par}"], bias=1.0)

                def stage2(bi):
                    kb, d, bias, mk, par = ops_of(bi)
                    for hh in range(2):
                        hs = slice(hh * 512, (hh + 1) * 512)
                        qk_into(Rw, "Rw", hh, kb, d, mk, False)
                        if bi > 0:
                            self.mm(Rw[:, hs], C["negones"][:], ssbw[(bi - 1) % 2][:, hs], False, False,
                                    ["c:negones", f"ssbw{(bi - 1) % 2}"], ["Rw"])
                        self.mm(Rw[:, hs], C["negtri"][:], spw[par][:, hs], False, True, ["c:negtri", f"spw{par}"], ["Rw"])
                    self.act(aw[par][:], Rw, AF.Exp, ["Rw", "c:flagcol"], [f"aw{par}"], bias=bias)
                    if bi < nb - 1:
                        if bi == 0:
                            self.copy("dve", ssum[:], spw[par][:], [f"spw{par}"], ["ssum"])
                        else:
                            self.tt("dve", ssum[:], ssum[:], spw[par][:], ALU.add, ["ssum", f"spw{par}"], ["ssum"])
                        self.copy("dve", ssbw[par][:], ssum[:], ["ssum"], [f"ssbw{par}"])

                def stage3(bi):
                    kb, d, bias, mk, par = ops_of(bi)
                    for hh in range(2):
                        ob = 4 + hh
                        self.mm(self.banks[ob][0:64, :], v[:, kb, hh * 64:(hh + 1) * 64], aw[par][:, hh * 512:(hh + 1) * 512],
                                bi == 0, bi == nb - 1, [vk, f"aw{par}"], [f"pb{ob}"])

                stage1(0)
                for bi in range(nb):
                    if bi + 1 < nb:
                        stage1(bi + 1)
                    stage2(bi)
                    if bi >= 1:
                        stage3(bi - 1)
                stage3(nb - 1)
                for hh in range(2):
                    ob = 4 + hh
                    self.copy("dve", os_[hh * 64:(hh + 1) * 64, :], self.banks[ob][0:64, :], [f"pb{ob}"], [osk])
                P.dma("sp", OT[hp * 128:(hp + 1) * 128, g * 512:(g + 1) * 512], os_[:], reads=[osk],
                      writes=[("O", hp, g)])
        sb.release(m0)

    def phase_res_moe(self, l, XT, OT, mods, XOUT, final=False):
        sb, P, C = self.sb, self.P, self.C
        m0 = sb.mark()
        NTG = 1024
        NTT = NTG // 128
        wo = sb.alloc([128, 8, D], BF16, "wo")
        P.dma("pool", wo[:], self.din(f"wo{l}", [D, D]).rearrange("(kc p) n -> p kc n", p=128), writes=["wo"])
        wr = sb.alloc([128, 8, E], F32, "wr")
        P.dma("sp", wr[:], self.din(f"rw{l}", [D, E]).rearrange("(kc p) n -> p kc n", p=128), writes=["wr"])
        rbrow = sb.alloc([1, E], F32, "rbrow")
        P.dma("sp", rbrow[:], self.din(f"rb{l}", [1, E]), writes=["rbrow"])
        bd = sb.alloc([E, D], F32, "bd")
        P.dma("sp", bd[:], self.din(f"bd{l}", [E, D]), writes=["bd"])
        bgu = sb.alloc([128, E, 16], F32, "bgu")
        P.dma("sp", bgu[:], self.din(f"bguT{l}", [128, E, 16]), writes=["bgu"])
        self.ts("dve", bgu[:, :, 8:16], bgu[:, :, 8:16], 1.0, None, ALU.add, None, ["bgu"], ["bgu"])
        wguD = self.din(f"wgu{l}", [E, 8, 128, 8 * 256])
        wdD = self.din(f"wd{l}", [E, D, D])
        if final:
            fnc = sb.alloc([128, 8], F32, "fnc")
            P.dma("sp", fnc[:], self.din("fnT", [128, 8]), writes=["fnc"])
        XTv = XT.rearrange("(kc p) t -> p kc t", p=128)
        OTv = OT.rearrange("(kc p) t -> p kc t", p=128)
        XOv = XOUT.rearrange("(kc p) t -> p kc t", p=128)
        x = sb.alloc([128, 8, NTG], F32, "xg")
        hT = sb.alloc([128, 8, NTG], BF16, "hTg")
        h32 = sb.alloc([128, 8, NTG], F32, "h32acc")
        acc = h32
        actT = sb.alloc([128, 8, NTG], BF16, "actT")
        oT = actT
        sq = [sb.alloc([128, 512], BF16, "sqm") for _ in range(2)]
        rstd = sb.alloc([128, 512], F32, "rstdm")
        tmp = [sb.alloc([128, 512], F32, "tmpm") for _ in range(2)]
        lg = sb.alloc([128, E], F32, "lg")
        m8 = sb.alloc([128, 8], F32, "m8")
        negm = sb.alloc([128, 1], F32, "negm")
        msk = sb.alloc([128, E], F32, "msk")
        ee = sb.alloc([128, E], F32, "ee")
        ssum = sb.alloc([128, 1], F32, "gsum")
        gts = sb.alloc([128, NTT, E], F32, "gts")
        gT = sb.alloc([E, NTG], F32, "gT")
        wgu = [sb.alloc([128, 8, 256], BF16, "wgu") for _ in range(4)]
        wdn = [sb.alloc([128, 8, 512], BF16, "wdn") for _ in range(3)]
        gc = [sb.alloc([128, 512], F32, "gc") for _ in range(2)]
        sg = [sb.alloc([128, 512], F32, "sg") for _ in range(2)]
        ub = [sb.alloc([128, 512], F32, "ub") for _ in range(2)]
        p1 = [sb.alloc([128, 512], F32, "p1") for _ in range(2)]
        wcnt = 0
        dcnt = 0
        ecnt = 0
        ycnt = 0
        for tg in range(T // NTG):
            gl = [2 * tg, 2 * tg + 1]
            tsl = slice(tg * NTG, (tg + 1) * NTG)
            P.dma("sp", x[:], XTv[:, :, tsl], reads=[("XT", g) for g in gl], writes=["xg"])
            P.dma("sp", oT[:], OTv[:, :, tsl], reads=[("O", h, g) for h in range(8) for g in gl], writes=["actT"])
            for s in range(2):
                ss = slice(s * 512, (s + 1) * 512)
                for j in range(8):
                    pb = (s * 8 + j) % 2
                    ps = self.banks[pb]
                    for kc in range(8):
                        self.mm(ps[:], wo[:, kc, j * 128:(j + 1) * 128], oT[:, kc, ss], kc == 0, kc == 7,
                                ["wo", "actT"], [f"pb{pb}"])
                    self.stt("dve", x[:, j, ss], ps[:], mods["G1"][:, j:j + 1], x[:, j, ss], ALU.mult, ALU.add,
                             [f"pb{pb}", mods["kmod"], "xg"], ["xg"])
            if getattr(self, "debug", False):
                if tg == 0:
                    self.dbg_x1 = self.dout("d_x1", [D, T])
                P.dma("sp", self.dbg_x1.rearrange("(kc p) t -> p kc t", p=128)[:, :, tsl], x[:], reads=["xg"], out=True)
            for s in range(2):
                ss = slice(s * 512, (s + 1) * 512)
                self.rstd_of(x[:, :, ss], ["xg"], 512, sq, rstd[:], "m", 2)
                for kc in range(8):
                    tm = tmp[kc % 2]
                    self.stt("dve", tm[:], x[:, kc, ss], mods["A2"][:, kc:kc + 1], rstd[:], ALU.mult, ALU.mult,
                             ["xg", mods["kA2"], "rstdm"], [f"tmpm{kc % 2}"])
                    self.act(h32[:, kc, ss], tm[:], AF.Identity, [f"tmpm{kc % 2}", mods["kmod"]], ["h32"],
                             bias=mods["B2"][:, kc:kc + 1])
                    self.copy("dve", hT[:, kc, ss], h32[:, kc, ss], ["h32"], ["hTg"])
            for tt_ in range(NTT):
                ts_ = slice(tt_ * 128, (tt_ + 1) * 128)
                ps = self.banks[3]
                for kc in range(8):
                    self.mm(ps[:, 0:E], h32[:, kc, ts_], wr[:, kc, :], kc == 0, False, ["h32", "wr"], ["pb3"])
                self.mm(ps[:, 0:E], C["onesf"][0:1, :], rbrow[:], False, True, ["c:onesf", "rbrow"], ["pb3"])
                self.copy("dve", lg[:], ps[:, 0:E], ["pb3"], ["lg"])
                P.op("dve", lambda e: e.max(out=m8[:], in_=lg[:]), ["lg"], ["m8"])
                self.ts("dve", negm[:], m8[:, 0:1], -1.0, None, ALU.mult, None, ["m8"], ["negm"])
                self.ts("dve", msk[:], lg[:], m8[:, 3:4], None, ALU.is_ge, None, ["lg", "m8"], ["msk"])
                self.act(ee[:], lg[:], AF.Exp, ["lg", "negm"], ["ee"], bias=negm[:, 0:1])
                self.tt("dve", ee[:], ee[:], msk[:], ALU.mult, ["ee", "msk"], ["ee"])
                P.op("dve", lambda e: e.reduce_sum(out=ssum[:], in_=ee[:], axis=mybir.AxisListType.X), ["ee"], ["gsum"])
                P.op("dve", lambda e: e.reciprocal(out=ssum[:], in_=ssum[:]), ["gsum"], ["gsum"])
                self.ts("dve", gts[:, tt_, :], ee[:], ssum[:, 0:1], None, ALU.mult, None, ["ee", "gsum"], ["gts"])
                ps2 = self.banks[4]
                self.mm(ps2[0:E, 0:128], gts[:, tt_, :], C["ident"][:], True, True, ["gts", "c:ident"], ["pb4"])
                self.copy("dve", gT[:, ts_], ps2[0:E, 0:128], ["pb4"], ["gT"])
            for tt_ in range(NTT):
                ts_ = slice(tt_ * 128, (tt_ + 1) * 128)
                for half in range(2):
                    pb = 4 + ycnt % 2
                    ycnt += 1
                    ps = self.banks[pb]
                    self.mm(ps[:], gT[:, ts_], bd[:, half * 512:(half + 1) * 512], True, True, ["gT", "bd"], [f"pb{pb}"])
                    self.copy("dve", acc[:, tt_, half * 512:(half + 1) * 512], ps[:], [f"pb{pb}"], ["h32"])
            for e in range(E):
                for j in range(8):
                    w = wgu[wcnt % 4]
                    wk = f"wgu{wcnt % 4}"
                    wcnt += 1
                    P.dma("pool", w[:], wguD[e, j].rearrange("p (kc c) -> p kc c", kc=8), writes=[wk])
                    for s in range(2):
                        ss = slice(s * 512, (s + 1) * 512)
                        i2 = ecnt % 2
                        ecnt += 1
                        gb_, ub_ = self.banks[i2], self.banks[2 + i2]
                        for kc in range(8):
                            self.mm(gb_[:], w[:, kc, 0:128], hT[:, kc, ss], kc == 0, kc == 7, [wk, "hTg"], [f"pb{i2}"])
                        for kc in range(8):
                            self.mm(ub_[:], w[:, kc, 128:256], hT[:, kc, ss], kc == 0, kc == 7, [wk, "hTg"],
                                    [f"pb{2 + i2}"])
                        self.ts("dve", gc[i2][:], gb_[:], bgu[:, e, j:j + 1], 7.0, ALU.add, ALU.min,
                                [f"pb{i2}", "bgu"], [f"gc{i2}"])
                        self.act(sg[i2][:], gc[i2][:], AF.Sigmoid, [f"gc{i2}"], [f"sg{i2}"], scale=1.702)
                        self.act(ub[i2][:], ub_[:], AF.Identity, [f"pb{2 + i2}", "bgu"], [f"ub{i2}"],
                                 bias=bgu[:, e, 8 + j:9 + j])
                        self.ts("dve", ub[i2][:], ub[i2][:], -6.0, 8.0, ALU.max, ALU.min, [f"ub{i2}"], [f"ub{i2}"])
                        self.tt("dve", p1[i2][:], gc[i2][:], sg[i2][:], ALU.mult, [f"gc{i2}", f"sg{i2}"], [f"p1{i2}"])
                        self.tt("dve", actT[:, j, ss], p1[i2][:], ub[i2][:], ALU.mult, [f"p1{i2}", f"ub{i2}"], ["actT"])
                wdv = wdD[e].rearrange("(j p) d -> p j d", p=128)
                for half in range(2):
                    w = wdn[dcnt % 3]
                    wk = f"wdn{dcnt % 3}"
                    dcnt += 1
                    P.dma("pool", w[:], wdv[:, :, half * 512:(half + 1) * 512], writes=[wk])
                    for tt_ in range(NTT):
                        ts_ = slice(tt_ * 128, (tt_ + 1) * 128)
                        pb = 4 + ycnt % 2
                        ycnt += 1
                        ps = self.banks[pb]
                        for j in range(8):
                            self.mm(ps[:], actT[:, j, ts_], w[:, j, :], j == 0, j == 7, [wk, "actT"], [f"pb{pb}"])
                        a_ = acc[:, tt_, half * 512:(half + 1) * 512]
                        self.stt("dve", a_, ps[:], gts[:, tt_, e:e + 1], a_, ALU.mult, ALU.add,
                                 [f"pb{pb}", "gts", "h32"], ["h32"])
            if getattr(self, "debug", False):
                if tg == 0:
                    self.dbg_m = self.dout("d_m", [T, D])
                P.dma("sp", self.dbg_m.rearrange("(n p) d -> p n d", p=128)[:, tg * NTT:(tg + 1) * NTT, :], acc[:],
                      reads=["h32"], out=True)
            for dj in range(8):
                for q4 in range(NTT // 4):
                    pb = 6 + (dj * 2 + q4) % 2
                    ps = self.banks[pb]
                    for t4 in range(4):
                        tt_ = q4 * 4 + t4
                        self.mm(ps[:, t4 * 128:(t4 + 1) * 128], acc[:, tt_, dj * 128:(dj + 1) * 128], C["ident"][:],
                                True, True, ["h32", "c:ident"], [f"pb{pb}"])
                    xs_ = x[:, dj, q4 * 512:(q4 + 1) * 512]
                    self.stt("dve", xs_, ps[:], mods["G2"][:, dj:dj + 1], xs_, ALU.mult, ALU.add,
                             [f"pb{pb}", mods["kmod"], "xg"], ["xg"])
            if final:
                for s in range(2):
                    ss = slice(s * 512, (s + 1) * 512)
                    self.rstd_of(x[:, :, ss], ["xg"], 512, sq, rstd[:], "m", 2)
                    for kc in range(8):
                        self.stt("dve", x[:, kc, ss], x[:, kc, ss], fnc[:, kc:kc + 1], rstd[:], ALU.mult, ALU.mult,
                                 ["xg", "fnc", "rstdm"], ["xg"])
            P.dma("sp", XOv[:, :, tsl], x[:], reads=["xg"], writes=[("XT", g) for g in gl], out=True)
        sb.release(m0)


def _cols(v, n):
    return np.ascontiguousarray(np.asarray(v, np.float32).reshape(n, 128).T)


def core_consts(inputs, core):
    b, hf = core // 2, core % 2
    p = np.arange(128)
    i = np.arange(512)
    k = {}
    k["k_ident"] = np.eye(128, dtype=np.float32)
    k["k_onesf"] = np.ones((128, 128), np.float32)
    k["k_onesb"] = np.ones((128, 128), NPBF)
    k["k_identb"] = np.eye(128, dtype=np.float32).astype(NPBF)
    k["k_negtri"] = (-(p[:, None] >= p[None, :]).astype(np.float32)).astype(NPBF)
    k["k_negones"] = (-np.ones((128, 128), np.float32)).astype(NPBF)
    sel = np.zeros((E, E, 128), np.float32)
    for e in range(E):
        sel[e, e, :] = 1.0
    k["k_sel"] = sel.reshape(E, E * 128).astype(NPBF)
    mA = np.zeros((128, 4, 512), np.float32)
    mB = np.zeros((128, 4, 512), np.float32)
    for d in range(4):
        kp = 128 * d + p[:, None]
        mA[:, d, :] = np.where(kp <= i[None, :], 0.0, NEG)
        mB[:, d, :] = np.where(kp < i[None, :], 0.0, NEG)
    k["k_maskA"] = mA.reshape(128, 2048).astype(NPBF)
    k["k_maskB"] = mB.reshape(128, 2048).astype(NPBF)
    k["k_flagcol"] = np.full((128, 1), 0.0 if hf == 1 else NEG, np.float32)
    k["k_zcol"] = np.zeros((128, 1), np.float32)
    k["k_cT"] = _cols(inputs["c"][b], 8)
    return k


def alibi_tables(hf):
    qp = hf * T + np.arange(T)
    kp = np.concatenate([(1 - hf) * T + np.arange(T), hf * T + np.arange(T)])
    qa = np.zeros((8, 4, T), np.float32)
    ka = np.zeros((8, 4, 2 * T), np.float32)
    for h in range(8):
        s = 2.0 ** (-(h + 1))
        qa[h, 0] = -s * 256.0 * (qp // 256)
        qa[h, 1] = -s * (qp % 256)
        qa[h, 2] = 1.0
        qa[h, 3] = 1.0
        ka[h, 0] = 1.0
        ka[h, 1] = 1.0
        ka[h, 2] = s * 256.0 * (kp // 256)
        ka[h, 3] = s * (kp % 256)
    return qa.astype(NPBF), ka.astype(NPBF)


def layer_shared(inputs, l, cache):
    if l in cache:
        return cache[l]
    f = np.float32
    w = {}
    w[f"ada_w{l}"] = np.ascontiguousarray(inputs["ada_w"][l], f)
    w[f"ada_bT{l}"] = _cols(inputs["ada_b"][l], 48)
    w[f"nmT{l}"] = np.concatenate([_cols(inputs["norm_mix"][l], 8), _cols(inputs["norm_moe"][l], 8)], axis=1)
    if l < NA:
        w[f"wqkv{l}"] = np.ascontiguousarray(inputs["a_wqkv"][l], f)
        w[f"wo{l}"] = np.ascontiguousarray(inputs["a_wo"][l], f)
        w[f"lamrep{l}"] = np.ascontiguousarray(np.tile(np.asarray(inputs["a_lambda"][l], f).reshape(1, 256), (128, 1)))
        w[f"sublnT{l}"] = np.asarray(inputs["a_subln"][l], f).reshape(128, 1).copy()
    else:
        w[f"wq{l}"] = np.ascontiguousarray(inputs["b_wq"][l - NA], f)
        w[f"wo{l}"] = np.ascontiguousarray(inputs["b_wo"][l - NA], f)
    if l == NA:
        w["kvw"] = np.ascontiguousarray(inputs["kv_w"], f)
        w["kv_ada_w"] = np.ascontiguousarray(inputs["kv_ada_w"], f)
        w["kv_ada_bT"] = _cols(inputs["kv_ada_b"], 16)
        w["kvnT"] = _cols(inputs["kv_norm"], 8)
    w[f"rw{l}"] = np.ascontiguousarray(inputs["router_w"][l], f)
    w[f"rb{l}"] = np.asarray(inputs["router_b"][l], f).reshape(1, E).copy()
    w[f"bd{l}"] = np.ascontiguousarray(inputs["b_down"][l], f)
    w[f"bguT{l}"] = np.ascontiguousarray(np.asarray(inputs["b_gate_up"][l], f).reshape(E, 16, 128).transpose(2, 0, 1))
    gu = np.asarray(inputs["w_gate_up"][l], f).reshape(E, 8, 128, 2, 8, 128)
    w[f"wgu{l}"] = np.ascontiguousarray(gu.transpose(0, 4, 2, 1, 3, 5)).reshape(E, 8, 128, 2048)
    w[f"wd{l}"] = np.ascontiguousarray(inputs["w_down"][l], f)
    if l == DEPTH - 1:
        w["fnT"] = _cols(inputs["final_norm"], 8)
    cache[l] = w
    return w


def build_proj(l):
    nc = bass.Bass("TRN2", target_bir_lowering=False)
    B = Builder(nc)
    B.load_consts()
    XT = B.din("XT", [D, T])
    mods = B.layer_mods(l)
    outs = {"Q": B.dout("Q", [D, T], BF16)}
    if l < NA:
        outs["K"] = B.dout("K", [D, T], BF16)
        outs["V"] = B.dout("V", [T, D], BF16)
    elif l == NA:
        outs["KB"] = B.dout("KB", [D, T], BF16)
        outs["VB"] = B.dout("VB", [T, D], BF16)
    B.phase_proj(l, XT, mods, outs)
    for o in B.P.ops:
        if o is not None and o.is_dma and any(isinstance(k, tuple) and k[0] in ("Q", "K", "V", "KB", "VB") for k in o.writes):
            o.is_out = True
    B.P.emit()
    return nc, B


def build_res(l, debug=False):
    nc = bass.Bass("TRN2", target_bir_lowering=False)
    B = Builder(nc)
    B.debug = debug
    B.load_consts()
    XT = B.din("XT", [D, T])
    mods = B.layer_mods(l)
    QT = B.din("Q", [D, T], BF16)
    Ko = B.din("Ko", [D, T], BF16)
    Kp = B.din("Kp", [D, T], BF16)
    Vo = B.din("Vo", [T, D], BF16)
    Vp = B.din("Vp", [T, D], BF16)
    OT = B.dint("OT", [D, T], BF16)
    XO = B.dout("XO", [D, T])
    if l < NA:
        qa = B.din("qaug", [8, 4, T], BF16)
        ka = B.din("kaug", [8, 4, 2 * T], BF16)
        B.phase_att_a(l, QT, Ko, Kp, Vo, Vp, OT, [], qa, ka)
    else:
        B.phase_att_b(l, QT, Ko, Kp, Vo, Vp, OT, [], lambda hp: [], [])
    B.P.barrier()
    B.phase_res_moe(l, XT, OT, mods, XO, final=(l == DEPTH - 1))
    B.P.emit()
    return nc, B


PAIRS = [[0, 1], [2, 3], [4, 5], [6, 7]]


def build_fused():
    nc = bass.Bass("TRN2", target_bir_lowering=False)
    B = Builder(nc)
    P = B.P
    B.load_consts()
    XIN = B.din("XT", [D, T])
    XT = B.dint("XTs", [D, T])
    XO = B.dout("XO", [D, T])
    QT = B.dint("Qs", [D, T], BF16)
    OT = B.dint("OTs", [D, T], BF16)
    Kx = {n: B.dint(n + "s", [D, T], BF16) for n in ("K", "KB")}
    Vx = {n: B.dint(n + "s", [T, D], BF16) for n in ("V", "VB")}
    Kall = {n: B.dint(n + "all", [2 * D, T], BF16) for n in ("K", "KB")}
    Vall = {n: B.dint(n + "all", [2 * T, D], BF16) for n in ("V", "VB")}
    Kp = {n: B.dint(n + "p", [D, T], BF16) for n in ("K", "KB")}
    Vp = {n: B.dint(n + "p", [T, D], BF16) for n in ("V", "VB")}
    qa = B.din("qaug", [8, 4, T], BF16)
    ka = B.din("kaug", [8, 4, 2 * T], BF16)
    XIv = XIN.rearrange("(kc p) t -> p kc t", p=128)
    XTv = XT.rearrange("(kc p) t -> p kc t", p=128)
    for g in range(NG):
        P.dma("sp", XTv[:, :, g * 512:(g + 1) * 512], XIv[:, :, g * 512:(g + 1) * 512], writes=[("XT", g)])
    bypass = ALU.bypass

    _oth = {}

    def oth(e):
        if "v" not in _oth:
            _oth["v"] = (e.partition_id() + 1) % 2
        return _oth["v"]

    NCH = 4

    def exchange(kn, vn):
        kr, vr = D // NCH, T // NCH
        Kc = Kall[kn].rearrange("(i r q) t -> i (r q) t", i=NCH, r=2)
        Vc = Vall[vn].rearrange("(i r q) c -> i (r q) c", i=NCH, r=2)
        for i in range(NCH):
            kkeys = [(kn, j, g) for j in range(2 * i, 2 * i + 2) for g in range(NG)]
            vkeys = [(vn, g) for g in range(2 * i, 2 * i + 2)]
            P.cc(lambda e, i=i: e.collective_compute("AllGather", bypass, replica_groups=PAIRS,
                                                     ins=[Kx[kn][i * kr:(i + 1) * kr, :]], outs=[Kc[i]]),
                 reads=kkeys, writes=[(kn + "all", i)])
            P.cc(lambda e, i=i: e.collective_compute("AllGather", bypass, replica_groups=PAIRS,
                                                     ins=[Vx[vn][i * vr:(i + 1) * vr, :]], outs=[Vc[i]]),
                 reads=vkeys, writes=[(vn + "all", i)])
        for i in range(NCH):
            P.op("pool", lambda e, i=i: e.dma_start(out=Kp[kn][i * kr:(i + 1) * kr, :],
                                                    in_=Kc[i][bass.ds(oth(e) * kr, kr), :]),
                 reads=[(kn + "all", i)], writes=[(kn + "p", i)], dma=True)
            P.op("pool", lambda e, i=i: e.dma_start(
                out=Vp[vn][i * vr:(i + 1) * vr, :].rearrange("(n p) c -> p n c", p=128),
                in_=Vc[i][bass.ds(oth(e) * vr, vr), :].rearrange("(n p) c -> p n c", p=128)),
                 reads=[(vn + "all", i)], writes=[(kn + "pv", i)], dma=True)

    def prev_keys(kn):
        return [(kn + "p", i) for i in range(NCH)] + [(kn + "pv", i) for i in range(NCH)]

    for l in range(DEPTH):
        mods = B.layer_mods(l)
        outs = {"Q": QT}
        if l < NA:
            outs["K"], outs["V"] = Kx["K"], Vx["V"]
        elif l == NA:
            outs["KB"], outs["VB"] = Kx["KB"], Vx["VB"]
        B.phase_proj(l, XT, mods, outs)
        if l < NA:
            exchange("K", "V")
            B.phase_att_a(l, QT, Kx["K"], Kp["K"], Vx["V"], Vp["V"], OT, prev_keys("K"), qa, ka)
        else:
            if l == NA:
                exchange("KB", "VB")
            B.phase_att_b(l, QT, Kx["KB"], Kp["KB"], Vx["VB"], Vp["VB"], OT, prev_keys("KB"),
                          lambda hp: [("KB", hp, g) for g in range(NG)], [("VB", g) for g in range(NG)])
        last = l == DEPTH - 1
        B.phase_res_moe(l, XT, OT, mods, XO if last else XT, final=last)
    P.emit()
    return nc, B


_PROG = {}


def kernel(**inputs):
    ncores = 8
    if "fused" not in _PROG:
        _PROG["fused"] = build_fused()
    nc, B = _PROG["fused"]
    x = np.asarray(inputs["x"], np.float32)
    cache = {}
    shared = {}
    for l in range(DEPTH):
        shared.update(layer_shared(inputs, l, cache))
    names = [n for n in B.dram if n in shared]
    maps = []
    for c in range(ncores):
        m = dict(core_consts(inputs, c))
        m["XT"] = np.ascontiguousarray(x[c // 2, (c % 2) * T:(c % 2 + 1) * T, :].T)
        m["qaug"], m["kaug"] = alibi_tables(c % 2)
        for n in names:
            m[n] = shared[n]
        maps.append(m)
    res = run_bass_kernel_spmd(nc, maps, core_ids=list(range(ncores))).results
    out = np.empty((BATCH, SEQ, D), np.float32)
    for c in range(ncores):
        out[c // 2, (c % 2) * T:(c % 2 + 1) * T, :] = res[c]["XO"].T
    return out
```
